# Optimizing a Trainium2 kernel written in Bass

```python
import math
import jax
import jax.numpy as jnp
from jax import lax
import numpy as np

D_MODEL = 1024
BATCH = 2
SEQ = 16384
DEPTH = 1

N_MEM = 256
ATTN_HEADS = 8
ATTN_HEAD_DIM = D_MODEL // 16
DILATED_PATTERNS = ((128, 1), (512, 4), (2048, 16))
RET_HEADS = 4
RET_QK_DIM = D_MODEL // 16
RET_V_DIM = D_MODEL // 8
RET_CHUNK = 128
RET_BASE_EXP = 5
ATTN_WIDTH = ATTN_HEADS * ATTN_HEAD_DIM
RET_QK_WIDTH = RET_HEADS * RET_QK_DIM
RET_V_WIDTH = RET_HEADS * RET_V_DIM
MIX_WIDTH = ATTN_WIDTH + RET_V_WIDTH
IN_PROJ_COLS = 3 * ATTN_WIDTH + 2 * RET_QK_WIDTH + 2 * RET_V_WIDTH
MEM_HEADS = 4
MEM_HEAD_DIM = D_MODEL // MEM_HEADS
N_GROUPS = 4
EXPERTS_PER_GROUP = 8
N_EXPERTS = N_GROUPS * EXPERTS_PER_GROUP
EXPERT_TOP_K = 2
EXPERT_FF = D_MODEL // 2
MOE_BLOCK = 128
NORM_EPS = 1e-6
GN_EPS = 1e-5
NEG_INF = -1e30

kernel_name = 'hybrid_dilated_retention_hmoe_encoder'


def rms_norm(x, w, eps=NORM_EPS):
    xf = x.astype(jnp.float32)
    y = xf * lax.rsqrt(jnp.mean(xf * xf, axis=-1, keepdims=True) + eps)
    return (y * w.astype(jnp.float32)).astype(x.dtype)


def split_heads(t, n_heads, head_dim):
    b, s, _ = t.shape
    return t.reshape(b, s, n_heads, head_dim).transpose(0, 2, 1, 3)


def merge_heads(t):
    b, h, s, d = t.shape
    return t.transpose(0, 2, 1, 3).reshape(b, s, h * d)


def alibi_slopes(n_heads):
    exps = jnp.arange(1, n_heads + 1, dtype=jnp.float32)
    return jnp.exp2(-8.0 * exps / n_heads)


def dilated_window_attention(q, k, v, slopes, window, dilation):
    B, H, S, hd = q.shape
    half = window // (2 * dilation)
    L = S // dilation
    nb = -(-L // half)
    Lp = nb * half

    def to_sub(t):
        return t.reshape(B, H, L, dilation, hd).transpose(0, 1, 3, 2, 4)

    qs, ks, vs = to_sub(q), to_sub(k), to_sub(v)
    qb = jnp.pad(qs, ((0, 0), (0, 0), (0, 0), (0, Lp - L), (0, 0))).reshape(B, H, dilation, nb, half, hd)
    kv_pad = ((0, 0), (0, 0), (0, 0), (half, Lp - L + half), (0, 0))
    kp = jnp.pad(ks, kv_pad).reshape(B, H, dilation, nb + 2, half, hd)
    vp = jnp.pad(vs, kv_pad).reshape(B, H, dilation, nb + 2, half, hd)

    def band(t):
        return jnp.concatenate([t[:, :, :, 0:nb], t[:, :, :, 1:nb + 1], t[:, :, :, 2:nb + 2]], axis=4)

    kb, vb = band(kp), band(vp)
    s = jnp.einsum('bhrnqd,bhrnkd->bhrnqk', qb, kb, preferred_element_type=jnp.float32)
    qi = jnp.arange(half)
    kc = jnp.arange(3 * half)
    delta = kc[None, :] - half - qi[:, None]
    key_pos = jnp.arange(nb)[:, None] * half - half + kc[None, :]
    allowed = (jnp.abs(delta) <= half)[None] & ((key_pos >= 0) & (key_pos < L))[:, None, :]
    bias = -slopes[:, None, None] * (dilation * jnp.abs(delta)).astype(jnp.float32)
    s = s + bias[None, :, None, None]
    s = jnp.where(allowed[None, None, None], s, NEG_INF)
    m = jnp.max(s, axis=-1, keepdims=True)
    p = jnp.exp(s - m)
    denom = jnp.sum(p, axis=-1, keepdims=True)
    o = jnp.einsum('bhrnqk,bhrnkd->bhrnqd', p.astype(v.dtype), vb, preferred_element_type=jnp.float32) / denom
    lse = (m + jnp.log(denom))[..., 0]

    def from_sub(t):
        t = t.reshape((B, H, dilation, Lp) + t.shape[5:])[:, :, :, :L]
        t = jnp.moveaxis(t, 2, 3)
        return t.reshape((B, H, S) + t.shape[4:])

    return from_sub(o), from_sub(lse)


def retention_chunkwise(q, k, v, log_gamma, inclusive):
    B, H, S, dk = q.shape
    dv = v.shape[-1]
    C = RET_CHUNK
    nC = S // C
    qc = q.reshape(B, H, nC, C, dk)
    kc = k.reshape(B, H, nC, C, dk)
    vc = v.reshape(B, H, nC, C, dv)
    i = jnp.arange(C)
    lg = log_gamma[:, None]
    diff = i[:, None] - i[None, :]
    mask = diff >= 0 if inclusive else diff > 0
    decay_mat = jnp.where(mask[None], jnp.exp(lg[:, :, None] * jnp.maximum(diff, 0)[None]), 0.0)
    scores = jnp.einsum('bhnid,bhnjd->bhnij', qc, kc) * decay_mat[None, :, None]
    y_intra = jnp.einsum('bhnij,bhnje->bhnie', scores, vc)
    k_dec = jnp.exp(lg * (C - 1 - i)[None])
    chunk_kv = jnp.einsum('bhnjd,hj,bhnje->bhnde', kc, k_dec, vc)
    chunk_decay = jnp.exp(log_gamma * C)[None, :, None, None]

    def step(state, kv_n):
        return state * chunk_decay + kv_n, state

    _, prev = lax.scan(step, jnp.zeros((B, H, dk, dv), q.dtype), jnp.moveaxis(chunk_kv, 2, 0))
    prev = jnp.moveaxis(prev, 0, 2)
    q_dec = jnp.exp(lg * (i + 1)[None])
    y_cross = jnp.einsum('bhnid,hi,bhnde->bhnie', qc, q_dec, prev)
    return (y_intra + y_cross).reshape(B, H, S, dv)


def head_group_norm(y, w, eps=GN_EPS):
    mu = jnp.mean(y, axis=-1, keepdims=True)
    var = jnp.mean(jnp.square(y - mu), axis=-1, keepdims=True)
    return merge_heads((y - mu) * lax.rsqrt(var + eps)) * w.astype(jnp.float32)


def parallel_mixer(h, w_in, q_norm_w, k_norm_w, decay_f, decay_b, gn_w, w_out):
    f32 = jnp.float32
    proj = h @ w_in
    a3 = 3 * ATTN_WIDTH
    cuts = [ATTN_WIDTH, 2 * ATTN_WIDTH, a3, a3 + RET_QK_WIDTH, a3 + 2 * RET_QK_WIDTH,
            a3 + 2 * RET_QK_WIDTH + RET_V_WIDTH]
    aq, ak, av, rq, rk, rv, rg = jnp.split(proj, cuts, axis=-1)
    q = rms_norm(split_heads(aq, ATTN_HEADS, ATTN_HEAD_DIM), q_norm_w) * (ATTN_HEAD_DIM ** -0.5)
    k = rms_norm(split_heads(ak, ATTN_HEADS, ATTN_HEAD_DIM), k_norm_w)
    v = split_heads(av, ATTN_HEADS, ATTN_HEAD_DIM)
    slopes = alibi_slopes(ATTN_HEADS)
    outs, lses = [], []
    for window, dilation in DILATED_PATTERNS:
        o, lse = dilated_window_attention(q, k, v, slopes, window, dilation)
        outs.append(o)
        lses.append(lse)
    mix_w = jax.nn.softmax(jnp.stack(lses), axis=0)
    attn = jnp.einsum('pbhs,pbhsd->bhsd', mix_w, jnp.stack(outs))
    attn = merge_heads(attn).astype(h.dtype)
    qr = split_heads(rq, RET_HEADS, RET_QK_DIM).astype(f32)
    kr = split_heads(rk, RET_HEADS, RET_QK_DIM).astype(f32) * (RET_QK_DIM ** -0.5)
    vr = split_heads(rv, RET_HEADS, RET_V_DIM).astype(f32)
    lg_f = jax.nn.log_sigmoid(decay_f.astype(f32))
    lg_b = jax.nn.log_sigmoid(decay_b.astype(f32))
    y_f = retention_chunkwise(qr, kr, vr, lg_f, inclusive=True)
    flip = lambda t: jnp.flip(t, axis=2)
    y_b = flip(retention_chunkwise(flip(qr), flip(kr), flip(vr), lg_b, inclusive=False))
    ret = head_group_norm(y_f + y_b, gn_w)
    ret = (jax.nn.silu(rg.astype(f32)) * ret).astype(h.dtype)
    return jnp.concatenate([attn, ret], axis=-1) @ w_out


def memory_cross_attention(h, mem_h, w_mq, w_mkv, q_norm_w, k_norm_w, w_mo):
    q = rms_norm(split_heads(h @ w_mq, MEM_HEADS, MEM_HEAD_DIM), q_norm_w) * (MEM_HEAD_DIM ** -0.5)
    mk, mv = jnp.split(mem_h @ w_mkv, 2, axis=-1)
    k = rms_norm(split_heads(mk, MEM_HEADS, MEM_HEAD_DIM), k_norm_w)
    v = split_heads(mv, MEM_HEADS, MEM_HEAD_DIM)
    s = jnp.einsum('bhsd,bhmd->bhsm', q, k, preferred_element_type=jnp.float32)
    p = jax.nn.softmax(s, axis=-1)
    o = jnp.einsum('bhsm,bhmd->bhsd', p.astype(v.dtype), v)
    return merge_heads(o) @ w_mo


def hierarchical_moe(h, w_group, w_router, w1, w3, w2):
    B, S, D = h.shape
    N = B * S
    t = h.reshape(N, D)
    g_logits = jnp.einsum('nd,dg->ng', t, w_group, preferred_element_type=jnp.float32)
    g_prob = jax.nn.softmax(g_logits, axis=-1)
    grp = jnp.argmax(g_logits, axis=-1)
    g_gate = jnp.take_along_axis(g_prob, grp[:, None], axis=-1)
    e_logits = jnp.einsum('nd,gde->nge', t, w_router, preferred_element_type=jnp.float32)
    e_logits = jnp.take_along_axis(e_logits, grp[:, None, None], axis=1)[:, 0]
    top_vals, top_idx = lax.top_k(e_logits, EXPERT_TOP_K)
    gates = g_gate * jax.nn.softmax(top_vals, axis=-1)
    expert_id = grp[:, None] * EXPERTS_PER_GROUP + top_idx
    A = N * EXPERT_TOP_K
    e_flat = expert_id.reshape(A)
    tok_flat = jnp.repeat(jnp.arange(N, dtype=jnp.int32), EXPERT_TOP_K)
    gate_flat = gates.reshape(A)
    order = jnp.argsort(e_flat)
    e_sorted = e_flat[order]
    counts = jnp.bincount(e_flat, length=N_EXPERTS)
    starts = jnp.cumsum(counts) - counts
    pcounts = (counts + MOE_BLOCK - 1) // MOE_BLOCK * MOE_BLOCK
    pends = jnp.cumsum(pcounts)
    pstarts = pends - pcounts
    dest = pstarts[e_sorted] + jnp.arange(A) - starts[e_sorted]
    n_blocks = -(-(A + N_EXPERTS * MOE_BLOCK) // MOE_BLOCK)
    n_slots = n_blocks * MOE_BLOCK
    slot_tok = jnp.full((n_slots,), N, dtype=jnp.int32).at[dest].set(tok_flat[order])
    slot_gate = jnp.zeros((n_slots,), jnp.float32).at[dest].set(gate_flat[order])
    block_expert = jnp.clip(jnp.searchsorted(pends, jnp.arange(n_blocks) * MOE_BLOCK, side='right'),
                            0, N_EXPERTS - 1)
    t_pad = jnp.concatenate([t, jnp.zeros((1, D), t.dtype)], axis=0)

    def expert_block(args):
        toks, e = args
        xb = t_pad[toks]
        return (jax.nn.silu(xb @ w1[e]) * (xb @ w3[e])) @ w2[e]

    y_slots = lax.map(expert_block, (slot_tok.reshape(n_blocks, MOE_BLOCK), block_expert))
    y_slots = y_slots.reshape(n_slots, D) * slot_gate[:, None]
    y = jax.ops.segment_sum(y_slots, slot_tok, num_segments=N + 1)[:N]
    return y.astype(h.dtype).reshape(B, S, D)


def setup_inputs(seed: int = 0) -> dict:
    key = jax.random.key(seed)
    ks = jax.random.split(key, 24)
    f32 = jnp.float32

    def normal(k, shape, scale):
        return jax.random.normal(k, shape, f32) * scale

    def gain(k, shape):
        return 1.0 + 0.05 * jax.random.normal(k, shape, f32)

    base_logit = jnp.log(jnp.exp2(RET_BASE_EXP + jnp.arange(RET_HEADS, dtype=f32)) - 1.0)
    return {
        'x': normal(ks[0], (BATCH, SEQ, D_MODEL), 1.0),
        'mem': normal(ks[1], (BATCH, N_MEM, D_MODEL), 1.0),
        'norm_mix_w': gain(ks[2], (DEPTH, D_MODEL)),
        'w_in': normal(ks[3], (DEPTH, D_MODEL, IN_PROJ_COLS), D_MODEL ** -0.5),
        'attn_q_norm_w': gain(ks[4], (DEPTH, ATTN_HEAD_DIM)),
        'attn_k_norm_w': gain(ks[5], (DEPTH, ATTN_HEAD_DIM)),
        'ret_decay_f': base_logit + 0.1 * jax.random.normal(ks[6], (DEPTH, RET_HEADS), f32),
        'ret_decay_b': base_logit + 0.1 * jax.random.normal(ks[7], (DEPTH, RET_HEADS), f32),
        'ret_gn_w': gain(ks[8], (DEPTH, RET_V_WIDTH)),
        'w_out': normal(ks[9], (DEPTH, MIX_WIDTH, D_MODEL), MIX_WIDTH ** -0.5),
        'norm_mem_w': gain(ks[10], (DEPTH, D_MODEL)),
        'norm_memkv_w': gain(ks[11], (DEPTH, D_MODEL)),
        'w_mq': normal(ks[12], (DEPTH, D_MODEL, D_MODEL), D_MODEL ** -0.5),
        'w_mkv': normal(ks[13], (DEPTH, D_MODEL, 2 * D_MODEL), D_MODEL ** -0.5),
        'mem_q_norm_w': gain(ks[14], (DEPTH, MEM_HEAD_DIM)),
        'mem_k_norm_w': gain(ks[15], (DEPTH, MEM_HEAD_DIM)),
        'w_mo': normal(ks[16], (DEPTH, D_MODEL, D_MODEL), D_MODEL ** -0.5),
        'norm_moe_w': gain(ks[17], (DEPTH, D_MODEL)),
        'w_group': normal(ks[18], (DEPTH, D_MODEL, N_GROUPS), D_MODEL ** -0.5),
        'w_router': normal(ks[19], (DEPTH, N_GROUPS, D_MODEL, EXPERTS_PER_GROUP), D_MODEL ** -0.5),
        'w_exp_gate': normal(ks[20], (DEPTH, N_EXPERTS, D_MODEL, EXPERT_FF), D_MODEL ** -0.5),
        'w_exp_up': normal(ks[21], (DEPTH, N_EXPERTS, D_MODEL, EXPERT_FF), D_MODEL ** -0.5),
        'w_exp_down': normal(ks[22], (DEPTH, N_EXPERTS, EXPERT_FF, D_MODEL), EXPERT_FF ** -0.5),
    }


def reference(x, mem, norm_mix_w, w_in, attn_q_norm_w, attn_k_norm_w, ret_decay_f, ret_decay_b,
              ret_gn_w, w_out, norm_mem_w, norm_memkv_w, w_mq, w_mkv, mem_q_norm_w, mem_k_norm_w,
              w_mo, norm_moe_w, w_group, w_router, w_exp_gate, w_exp_up, w_exp_down):
    for l in range(DEPTH):
        x = x + parallel_mixer(rms_norm(x, norm_mix_w[l]), w_in[l], attn_q_norm_w[l], attn_k_norm_w[l],
                               ret_decay_f[l], ret_decay_b[l], ret_gn_w[l], w_out[l])
        x = x + memory_cross_attention(rms_norm(x, norm_mem_w[l]), rms_norm(mem, norm_memkv_w[l]),
                                       w_mq[l], w_mkv[l], mem_q_norm_w[l], mem_k_norm_w[l], w_mo[l])
        x = x + hierarchical_moe(rms_norm(x, norm_moe_w[l]), w_group[l], w_router[l],
                                 w_exp_gate[l], w_exp_up[l], w_exp_down[l])
    return x
```

```python
import contextlib
import numpy as np
import concourse.bass as bass
import concourse.mybir as mybir
from concourse.bass_utils import run_bass_kernel_spmd

F32 = mybir.dt.float32
BF16 = mybir.dt.bfloat16
I32 = mybir.dt.int32
AF = mybir.ActivationFunctionType
ALU = mybir.AluOpType
AX = mybir.AxisListType

D = 1024
KC = 8
NEG = -30000.0
EPS = 1e-6
GN_EPS = 1e-5
PATTERNS = (1, 4, 16)
SLOPES = [2.0 ** (-8.0 * (h + 1) / 8.0) for h in range(8)]


class Buf:
    def __init__(self, name, t=None):
        self.name = name
        self.t = t
        self.w = {}
        self.r = {}
        self.dsem = {}
        self.dcnt = {}
        self.excl = False

    def __getitem__(self, idx):
        return self.t[idx]


class Ring:
    def __init__(self, bufs):
        self.bufs = bufs
        self.i = 0

    def next(self):
        b = self.bufs[self.i % len(self.bufs)]
        self.i += 1
        return b


class Ctx:
    def __init__(self, nc):
        self.nc = nc
        self.e = {"pe": nc.tensor, "act": nc.scalar, "dve": nc.vector,
                  "pool": nc.gpsimd, "sp": nc.sync}
        self.psem = {}
        self.pcnt = {}
        for k in ("pe", "act", "dve", "pool"):
            self.psem[k] = nc.alloc_semaphore("prog_" + k)
            self.pcnt[k] = 0
        self.seen = {k: {} for k in self.e}
        self.nwait = 0
        self.ninst = 0
        self.nsem = 4
        self.owners = []
        self.stacks = []
        self.bar_sem = nc.alloc_semaphore("bar")
        self.bar_cnt = 0

    def sb(self, name, shape, dt=F32):
        if self.stacks:
            t = self.stacks[-1].enter_context(self.nc.sbuf_tensor(name, list(shape), dt, side="right"))
            return Buf(name, t)
        return Buf(name, self.nc.alloc_sbuf_tensor(name, list(shape), dt, side="left"))

    def barrier(self):
        need = {}
        for k in self.psem:
            if self.pcnt[k] > 0:
                need["prog_" + k] = (self.psem[k], self.pcnt[k])
        for o in self.owners:
            for qt, sm in o.dsem.items():
                need["d%s_%s" % (qt, o.name)] = (sm, o.dcnt[qt])
        self._wait("sp", need)
        self.bar_cnt += 1
        self.e["sp"].sem_inc(self.bar_sem, 1)
        for k in ("pe", "act", "dve", "pool"):
            self.e[k].wait_ge(self.bar_sem, self.bar_cnt)
            for kk, (sm, v) in need.items():
                if self.seen[k].get(kk, 0) < v:
                    self.seen[k][kk] = v

    def push(self):
        self.stacks.append(contextlib.ExitStack())

    def pop(self):
        self.barrier()
        self.stacks.pop().close()

    def pop_all(self):
        while self.stacks:
            self.pop()

    def ring(self, name, n, shape, dt=F32):
        return Ring([self.sb("%s%d" % (name, i), shape, dt) for i in range(n)])

    def ps(self, name):
        b = Buf(name, self.nc.alloc_psum_tensor(name, [128, 512], F32))
        b.excl = True
        return b

    def dram(self, name, shape, dt, kind="Internal"):
        return Buf(name, self.nc.dram_tensor(name, list(shape), dt, kind=kind))

    def _collect(self, eng, reads, writes):
        need = {}

        def add(d):
            for k, (s, v) in d.items():
                if k not in need or need[k][1] < v:
                    need[k] = (s, v)
        own = "prog_" + eng
        for b in reads:
            add(b.w)
            if b.excl:
                add({k: v for k, v in b.r.items() if k != own})
        for b in writes:
            add(b.w)
            add(b.r)
        if eng == "pe":
            need.pop("prog_pe", None)
        return need

    def _wait(self, eng, need):
        seen = self.seen[eng]
        for k, (s, v) in need.items():
            if seen.get(k, 0) < v:
                self.e[eng].wait_ge(s, v)
                seen[k] = v
                self.nwait += 1

    def _record(self, key, sem, val, reads, writes, merge_w):
        for b in reads:
            old = b.r.get(key)
            if old is None or old[1] < val:
                b.r[key] = (sem, val)
        for b in writes:
            if merge_w:
                b.w[key] = (sem, val)
            else:
                b.w = {key: (sem, val)}
                b.r = {}

    def op(self, eng, fn, reads=(), writes=(), merge_w=False):
        need = self._collect(eng, reads, writes)
        self._wait(eng, need)
        inst = fn(self.e[eng])
        self.pcnt[eng] += 1
        inst.then_inc(self.psem[eng], 1)
        self.ninst += 1
        self._record("prog_" + eng, self.psem[eng], self.pcnt[eng], reads, writes, merge_w)
        return inst

    def dma(self, q, fn, owner, reads=(), writes=(), merge_w=False):
        need = self._collect(q, reads, writes)
        self._wait(q, need)
        qt = "sw" if q == "pool" else "hw"
        if qt not in owner.dsem:
            owner.dsem[qt] = self.nc.alloc_semaphore("d%s_%s" % (qt, owner.name))
            owner.dcnt[qt] = 0
            self.nsem += 1
            if owner not in self.owners:
                self.owners.append(owner)
        inst = fn(self.e[q])
        owner.dcnt[qt] += 16
        inst.then_inc(owner.dsem[qt], 16)
        self.ninst += 1
        self._record("d%s_%s" % (qt, owner.name), owner.dsem[qt], owner.dcnt[qt], reads, writes, merge_w)
        return inst

    def finish(self, bufs=(), eng="sp"):
        need = {}
        for o in self.owners:
            for qt, s in o.dsem.items():
                need["d%s_%s" % (qt, o.name)] = (s, o.dcnt[qt])
        for b in bufs:
            for k, (s, v) in b.w.items():
                if k not in need or need[k][1] < v:
                    need[k] = (s, v)
        self._wait(eng, need)


class Cfg:
    def __init__(self, NT=32, NF=96, debug=False, stop=None):
        self.NT = NT
        self.NH = 8
        self.NF = NF
        self.NA = NT + 16
        self.LS = NT * 128
        self.NBLK = 2 * NT + 32
        self.debug = debug
        self.stop = stop
        self.nq = [self.LS // d // 128 for d in PATTERNS]
        self.nch = [q + 1 for q in self.nq]
        self.chbase = []
        o = 0
        for d, n in zip(PATTERNS, self.nch):
            self.chbase.append(o)
            o += d * n
        self.NCHT = o


class _Stop(Exception):
    pass


def build(cfg):
    nc = bass.Bass("TRN2", target_bir_lowering=False)
    c = Ctx(nc)
    phases = {}
    try:
        return _build_body(cfg, nc, c, phases)
    except _Stop:
        c.finish()
        c.pop_all()
        return nc, c, phases


def _build_body(cfg, nc, c, phases):
    NT, NF, NA, LS, NBLK = cfg.NT, cfg.NF, cfg.NA, cfg.LS, cfg.NBLK
    NTOK = LS
    dbg = "ExternalOutput" if cfg.debug else "Internal"

    def din(name, shape, dt=F32):
        return c.dram(name, shape, dt, kind="ExternalInput")

    xa = din("xa", [NA * 128, D])
    xf = din("xf", [NF * 128, D])
    posf = din("posf", [128, NF])
    t01 = din("t01", [128, 2])
    valt_d = din("valt", [128, cfg.NCHT])
    memx = din("memx", [256, D])
    nw_mix = din("nw_mix", [128, 8])
    nw_mem = din("nw_mem", [128, 8])
    nw_memkv = din("nw_memkv", [128, 8])
    nw_moe = din("nw_moe", [1, D])
    aq_w = din("aq_w", [128, 1])
    ak_w = din("ak_w", [128, 1])
    mq_w = din("mq_w", [128, 2])
    mk_w = din("mk_w", [128, 2])
    dec_d = din("dec", [1, 8])
    gn_w = din("gn_w", [1, 512])
    w_in = din("w_in", [D, 3072])
    w_out = din("w_out", [D, D])
    w_mq = din("w_mq", [D, D])
    w_mkv = din("w_mkv", [D, 2 * D])
    w_mo = din("w_mo", [D, D])
    w_rt = din("w_rt", [D, 36])
    w1 = din("w1", [32, D, 512])
    w3 = din("w3", [32, D, 512])
    w2 = din("w2", [32, 512, D])
    y_d = c.dram("y", [NTOK, D], F32, kind="ExternalOutput")

    QT_d = c.dram("QT_d", [4, 128, LS], BF16, kind=dbg)
    KT_d = c.dram("KT_d", [4, 128, NA * 128], BF16, kind=dbg)
    VT_d = c.dram("VT_d", [4, 128, NA * 128], BF16, kind=dbg)
    MIX_d = c.dram("MIX_d", [8, 128, LS], BF16, kind=dbg)
    X2_d = c.dram("X2_d", [NTOK, D], F32, kind=dbg)
    H3_d = c.dram("H3_d", [NTOK + 128, D], BF16, kind=dbg)
    YS_d = c.dram("YS_d", [NBLK * 128, D], F32, kind=dbg)
    SLOT_d = c.dram("SLOT_d", [NBLK * 128, 16], I32, kind=dbg)
    if cfg.debug:
        DBG_d = c.dram("DBG_d", [128, 4096], F32, kind="ExternalOutput")

    P = [c.ps("pb%d" % i) for i in range(8)]

    def pbf(b):
        return b.t.bitcast(BF16)

    ident = c.sb("ident", [128, 128], BF16)
    identf = c.sb("identf", [128, 128], F32)
    ones_bf = c.sb("ones_bf", [128, 128], BF16)
    zeros_bf = c.sb("zeros_bf", [128, 128], BF16)
    blk1 = c.sb("blk1", [128, 128], BF16)
    kq = c.sb("kq", [128, 128], F32)
    kqi = c.sb("kqi", [128, 128], I32)
    tmpA = c.sb("tmpA", [128, 128], F32)
    tmpB = c.sb("tmpB", [128, 128], F32)
    tmpC = c.sb("tmpC", [128, 128], F32)
    tmpD = c.sb("tmpD", [128, 128], F32)

    c.op("pool", lambda e: e.iota(kqi[:, :], pattern=[[-1, 128]], base=0, channel_multiplier=1), writes=[kqi])
    c.op("dve", lambda e: e.tensor_copy(out=kq[:, :], in_=kqi[:, :]), reads=[kqi], writes=[kq])
    c.op("dve", lambda e: e.tensor_scalar(out=identf[:, :], in0=kq[:, :], scalar1=0.0, scalar2=None, op0=ALU.is_equal), reads=[kq], writes=[identf])
    c.op("dve", lambda e: e.tensor_copy(out=ident[:, :], in_=identf[:, :]), reads=[identf], writes=[ident])
    c.op("pool", lambda e: e.memset(ones_bf[:, :], 1.0), writes=[ones_bf])
    c.op("pool", lambda e: e.memset(zeros_bf[:, :], 0.0), writes=[zeros_bf])
    zrow = c.sb("zrow", [128, 512], BF16)
    c.op("pool", lambda e: e.memset(zrow[:, :], 0.0), writes=[zrow])
    c.op("pool", lambda e: e.memset(blk1[:, :], 0.0), writes=[blk1])
    c.op("pool", lambda e: e.memset(blk1[0:64, 0:64], 1.0), writes=[blk1], merge_w=True)
    c.op("pool", lambda e: e.memset(blk1[64:128, 64:128], 1.0), writes=[blk1], merge_w=True)

    def load_small(name, src_ap, shape, dt=F32):
        b = c.sb(name, shape, dt)
        c.dma("sp", lambda e: e.dma_start(out=b.t[tuple(slice(None) for _ in shape)], in_=src_ap), owner=b, writes=[b])
        return b

    nwmix = load_small("nwmix", nw_mix[:, :], [128, 8])
    nwmem = load_small("nwmem", nw_mem[:, :], [128, 8])
    nwmemkv = load_small("nwmemkv", nw_memkv[:, :], [128, 8])
    aqw = load_small("aqw", aq_w[:, :], [128, 1])
    akw = load_small("akw", ak_w[:, :], [128, 1])
    mqw = load_small("mqw", mq_w[:, :], [128, 2])
    mkw = load_small("mkw", mk_w[:, :], [128, 2])
    decb = load_small("decb", dec_d[0:1, :].to_broadcast([128, 8]), [128, 8])
    posf_s = load_small("posf_s", posf[:, :], [128, NF])
    t01_s = load_small("t01_s", t01[:, :], [128, 2])
    c.op("dve", lambda e: e.tensor_scalar(out=aqw[:, :], in0=aqw[:, :], scalar1=0.125, scalar2=None, op0=ALU.mult), reads=[aqw], writes=[aqw])
    c.op("dve", lambda e: e.tensor_scalar(out=mqw[:, :], in0=mqw[:, :], scalar1=1.0 / 16.0, scalar2=None, op0=ALU.mult), reads=[mqw], writes=[mqw])

    xs_ring = c.ring("xs", 3, [128, D], F32)
    junk_ring = c.ring("junk", 2, [128, D], BF16)
    xn_ring = c.ring("xn", 2, [128, D], BF16)
    ss_ring = c.ring("ss", 4, [128, 1], F32)
    rstd_ring = c.ring("rstd", 4, [128, 1], F32)
    wst_ring = c.ring("wst", 2, [128, D], F32)
    cast_rr = [0]

    def wcast(dst_ap, src_ap, ncols, col_ap=None, mul=None, dstbuf=None):
        st = wst_ring.next()
        c.dma("sp", lambda e: e.dma_start(out=st[:, 0:ncols], in_=src_ap), owner=st, writes=[st])
        eng = ("dve", "pool")[cast_rr[0] % 2]
        cast_rr[0] += 1
        rd = [st]
        if col_ap is not None:
            rd.append(col_ap[0])
        if col_ap is None and mul is None:
            c.op(eng, lambda e: e.tensor_copy(out=dst_ap, in_=st[:, 0:ncols]), reads=rd, writes=[dstbuf], merge_w=True)
        elif col_ap is None:
            c.op(eng, lambda e: e.tensor_scalar(out=dst_ap, in0=st[:, 0:ncols], scalar1=float(mul), scalar2=None, op0=ALU.mult), reads=rd, writes=[dstbuf], merge_w=True)
        elif mul is None:
            c.op(eng, lambda e: e.tensor_scalar(out=dst_ap, in0=st[:, 0:ncols], scalar1=col_ap[1], scalar2=None, op0=ALU.mult), reads=rd, writes=[dstbuf], merge_w=True)
        else:
            c.op(eng, lambda e: e.tensor_scalar(out=dst_ap, in0=st[:, 0:ncols], scalar1=col_ap[1], scalar2=float(mul), op0=ALU.mult, op1=ALU.mult), reads=rd, writes=[dstbuf], merge_w=True)

    def norm_rstd(xbuf, xap):
        jk = junk_ring.next()
        ss = ss_ring.next()
        rs = rstd_ring.next()
        c.op("act", lambda e: e.activation(out=jk[:, :], in_=xap, func=AF.Square, accum_out=ss[:, 0:1]), reads=[xbuf], writes=[jk, ss])
        c.op("act", lambda e: e.activation(out=rs[:, :], in_=ss[:, :], func=AF.Ln, scale=1.0 / D, bias=EPS), reads=[ss], writes=[rs])
        c.op("act", lambda e: e.activation(out=rs[:, :], in_=rs[:, :], func=AF.Exp, scale=-0.5), reads=[rs], writes=[rs])
        return rs

    def norm_T(xbuf, xap, ptb, dst_buf, dst_ap, eng_scale="pool"):
        rs = norm_rstd(xbuf, xap)
        xn = xn_ring.next()
        c.op(eng_scale, lambda e: e.tensor_scalar(out=xn[:, :], in0=xap, scalar1=rs[:, 0:1], scalar2=None, op0=ALU.mult), reads=[xbuf, rs], writes=[xn])
        pv = pbf(ptb)
        for kc in range(KC):
            c.op("pe", lambda e: e.transpose(out=pv[:, kc * 128:(kc + 1) * 128], in_=xn[:, kc * 128:(kc + 1) * 128], identity=ident[:, :]), reads=[xn, ident], writes=[ptb])
        c.op("act", lambda e: e.activation(out=dst_ap, in_=pv[:, 0:1024].rearrange("p (k t) -> p k t", k=8), func=AF.Identity), reads=[ptb], writes=[dst_buf], merge_w=True)
        return rs

    def load_x(src, row0):
        xs = xs_ring.next()
        c.dma("sp", lambda e: e.dma_start(out=xs[:, :], in_=src[row0:row0 + 128, :]), owner=xs, writes=[xs])
        return xs

    def stop_here(label):
        if cfg.stop == label:
            raise _Stop()

    c.push()
    STb = c.sb("STb", [128, NT, 512], BF16)
    lg = c.sb("lg", [128, 8])
    c.op("act", lambda e: e.activation(out=lg[:, :], in_=decb[:, :], func=AF.Exp, scale=-1.0), reads=[decb], writes=[lg])
    c.op("act", lambda e: e.activation(out=lg[:, :], in_=lg[:, :], func=AF.Ln, bias=1.0), reads=[lg], writes=[lg])
    c.op("dve", lambda e: e.tensor_scalar(out=lg[:, :], in0=lg[:, :], scalar1=-1.0, scalar2=None, op0=ALU.mult), reads=[lg], writes=[lg])
    lgsel = c.sb("lgsel", [128, 4])
    c.op("dve", lambda e: e.tensor_copy(out=lgsel[0:64, :], in_=lg[0:64, 0:4]), reads=[lg], writes=[lgsel], merge_w=True)
    c.op("dve", lambda e: e.tensor_copy(out=lgsel[64:128, :], in_=lg[64:128, 4:8]), reads=[lg], writes=[lgsel], merge_w=True)
    cdec = c.sb("cdec", [128, 4])
    c.op("act", lambda e: e.activation(out=cdec[:, :], in_=lgsel[:, :], func=AF.Exp, scale=128.0), reads=[lgsel], writes=[cdec])

    DT = c.sb("DT", [128, 4, 128])
    kqp = c.sb("kqp", [128, 128])
    kqn = c.sb("kqn", [128, 128])
    mle = c.sb("mle", [128, 128])
    mgt = c.sb("mgt", [128, 128])
    c.op("dve", lambda e: e.tensor_scalar(out=kqp[:, :], in0=kq[:, :], scalar1=0.0, scalar2=None, op0=ALU.max), reads=[kq], writes=[kqp])
    c.op("dve", lambda e: e.tensor_scalar(out=kqn[:, :], in0=kq[:, :], scalar1=-1.0, scalar2=0.0, op0=ALU.mult, op1=ALU.max), reads=[kq], writes=[kqn])
    c.op("dve", lambda e: e.tensor_scalar(out=mle[:, :], in0=kq[:, :], scalar1=0.0, scalar2=None, op0=ALU.is_le), reads=[kq], writes=[mle])
    c.op("dve", lambda e: e.tensor_scalar(out=mgt[:, :], in0=kq[:, :], scalar1=0.0, scalar2=None, op0=ALU.is_gt), reads=[kq], writes=[mgt])
    for h in range(4):
        c.op("act", lambda e: e.activation(out=tmpA[:, :], in_=kqn[:, :], func=AF.Exp, scale=lg[:, h:h + 1]), reads=[kqn, lg], writes=[tmpA])
        c.op("act", lambda e: e.activation(out=tmpB[:, :], in_=kqp[:, :], func=AF.Exp, scale=lg[:, 4 + h:5 + h]), reads=[kqp, lg], writes=[tmpB])
        c.op("dve", lambda e: e.tensor_tensor(out=tmpA[:, :], in0=tmpA[:, :], in1=mle[:, :], op=ALU.mult), reads=[tmpA, mle], writes=[tmpA])
        c.op("dve", lambda e: e.tensor_tensor(out=tmpB[:, :], in0=tmpB[:, :], in1=mgt[:, :], op=ALU.mult), reads=[tmpB, mgt], writes=[tmpB])
        c.op("dve", lambda e: e.tensor_tensor(out=DT[:, h, :], in0=tmpA[:, :], in1=tmpB[:, :], op=ALU.add), reads=[tmpA, tmpB], writes=[DT], merge_w=True)

    expo_i = c.sb("expo_i", [128, 128], I32)
    expo = c.sb("expo", [128, 128])
    DEC = c.sb("DEC", [128, 4, 128])
    c.op("pool", lambda e: e.iota(expo_i[0:64, :], pattern=[[1, 128]], base=1, channel_multiplier=0), writes=[expo_i], merge_w=True)
    c.op("pool", lambda e: e.iota(expo_i[64:128, :], pattern=[[-1, 128]], base=128, channel_multiplier=0), writes=[expo_i], merge_w=True)
    c.op("dve", lambda e: e.tensor_copy(out=expo[:, :], in_=expo_i[:, :]), reads=[expo_i], writes=[expo])
    for h in range(4):
        c.op("act", lambda e: e.activation(out=DEC[:, h, :], in_=expo[:, :], func=AF.Exp, scale=lgsel[:, h:h + 1]), reads=[expo, lgsel], writes=[DEC], merge_w=True)

    pcol_i = c.sb("pcol_i", [128, 2], I32)
    pcol = c.sb("pcol", [128, 2])
    c.op("pool", lambda e: e.iota(pcol_i[:, 0:1], pattern=[[0, 1]], base=127, channel_multiplier=-1), writes=[pcol_i], merge_w=True)
    c.op("pool", lambda e: e.iota(pcol_i[:, 1:2], pattern=[[0, 1]], base=0, channel_multiplier=1), writes=[pcol_i], merge_w=True)
    c.op("dve", lambda e: e.tensor_copy(out=pcol[:, :], in_=pcol_i[:, :]), reads=[pcol_i], writes=[pcol])
    kdec = c.sb("kdec", [128, 4, 2])
    for h in range(4):
        c.op("act", lambda e: e.activation(out=kdec[:, h, 0:1], in_=pcol[:, 0:1], func=AF.Exp, scale=lg[:, h:h + 1]), reads=[pcol, lg], writes=[kdec], merge_w=True)
        c.op("act", lambda e: e.activation(out=kdec[:, h, 1:2], in_=pcol[:, 1:2], func=AF.Exp, scale=lg[:, 4 + h:5 + h]), reads=[pcol, lg], writes=[kdec], merge_w=True)

    CFB = c.sb("CFB", [128, NF, 4, 2])
    dfd = c.sb("dfd", [128, NF])
    dbd = c.sb("dbd", [128, NF])
    mfd = c.sb("mfd", [128, NF])
    mbd = c.sb("mbd", [128, NF])
    tmpF = c.sb("tmpF", [128, NF])
    c.op("dve", lambda e: e.tensor_scalar(out=dfd[:, :], in0=posf_s[:, :], scalar1=-1.0, scalar2=t01_s[:, 0:1], op0=ALU.mult, op1=ALU.add), reads=[posf_s, t01_s], writes=[dfd])
    c.op("dve", lambda e: e.tensor_scalar(out=dfd[:, :], in0=dfd[:, :], scalar1=-1.0, scalar2=None, op0=ALU.add), reads=[dfd], writes=[dfd])
    c.op("dve", lambda e: e.tensor_scalar(out=dbd[:, :], in0=posf_s[:, :], scalar1=t01_s[:, 1:2], scalar2=None, op0=ALU.subtract), reads=[posf_s, t01_s], writes=[dbd])
    c.op("dve", lambda e: e.tensor_scalar(out=mfd[:, :], in0=dfd[:, :], scalar1=0.0, scalar2=None, op0=ALU.is_ge), reads=[dfd], writes=[mfd])
    c.op("dve", lambda e: e.tensor_scalar(out=mbd[:, :], in0=dbd[:, :], scalar1=0.0, scalar2=None, op0=ALU.is_ge), reads=[dbd], writes=[mbd])
    c.op("dve", lambda e: e.tensor_scalar(out=dfd[:, :], in0=dfd[:, :], scalar1=0.0, scalar2=None, op0=ALU.max), reads=[dfd], writes=[dfd])
    c.op("dve", lambda e: e.tensor_scalar(out=dbd[:, :], in0=dbd[:, :], scalar1=0.0, scalar2=None, op0=ALU.max), reads=[dbd], writes=[dbd])
    for h in range(4):
        c.op("act", lambda e: e.activation(out=tmpF[:, :], in_=dfd[:, :], func=AF.Exp, scale=lg[:, h:h + 1]), reads=[dfd, lg], writes=[tmpF])
        c.op("dve", lambda e: e.tensor_tensor(out=CFB[:, :, h, 0], in0=tmpF[:, :], in1=mfd[:, :], op=ALU.mult), reads=[tmpF, mfd], writes=[CFB], merge_w=True)
        c.op("act", lambda e: e.activation(out=tmpF[:, :], in_=dbd[:, :], func=AF.Exp, scale=lg[:, 4 + h:5 + h]), reads=[dbd, lg], writes=[tmpF])
        c.op("dve", lambda e: e.tensor_tensor(out=CFB[:, :, h, 1], in0=tmpF[:, :], in1=mbd[:, :], op=ALU.mult), reads=[tmpF, mbd], writes=[CFB], merge_w=True)

    c.push()
    Wf = c.sb("Wf", [128, 8, 768], BF16)
    for kc in range(KC):
        wcast(Wf[:, kc, 0:256], w_in[kc * 128:(kc + 1) * 128, 1792:2048], 256, col_ap=(nwmix, nwmix[:, kc:kc + 1]), mul=0.125, dstbuf=Wf)
        wcast(Wf[:, kc, 256:768], w_in[kc * 128:(kc + 1) * 128, 2048:2560], 512, col_ap=(nwmix, nwmix[:, kc:kc + 1]), dstbuf=Wf)
    hT_ring = c.ring("hTt", 2, [128, 8, 128], BF16)
    kfb_ring = c.ring("kfb", 2, [128, 4, 128], BF16)
    vb_ring = c.ring("vb", 2, [128, 512], BF16)
    CKV = c.sb("CKV", [128, NT, 512], F32)
    Sin = c.sb("Sin", [128, 512], F32)
    PT = [P[0], P[1]]
    PA = [P[2], P[3]]
    PB = [P[4], P[5]]
    PS = P[6]
    PC = [P[6], P[7]]
    c.op("pe", lambda e: e.matmul(PS[:, 0:512], lhsT=zeros_bf[:, :], rhs=zrow[:, :], start=True, stop=False), reads=[zeros_bf, zrow], writes=[PS])
    for it in range(NF + NT):
        far = it < NF
        if far:
            xs = load_x(xf, it * 128)
            coef = CFB[:, it, :, :]
            coefb = CFB
        else:
            n = it - NF
            xs = load_x(xa, (8 + n) * 128)
            coef = kdec[:, :, :]
            coefb = kdec
        if it == NF:
            c.op("pe", lambda e: e.matmul(PS[:, 0:512], lhsT=zeros_bf[:, :], rhs=zrow[:, :], start=False, stop=True), reads=[zeros_bf, zrow], writes=[PS])
            c.op("dve", lambda e: e.tensor_copy(out=Sin[:, :], in_=PS[:, 0:512]), reads=[PS], writes=[Sin])
        hT = hT_ring.next()
        norm_T(xs, xs[:, :], PT[it % 2], hT, hT[:, :, :])
        pa, pb = PA[it % 2], PB[it % 2]
        for kc in range(KC):
            c.op("pe", lambda e: e.matmul(pa[:, 0:256], lhsT=hT[:, kc, :], rhs=Wf[:, kc, 0:256], start=(kc == 0), stop=(kc == KC - 1)), reads=[hT, Wf], writes=[pa])
        for kc in range(KC):
            c.op("pe", lambda e: e.matmul(pb[:, 0:512], lhsT=hT[:, kc, :], rhs=Wf[:, kc, 256:768], start=(kc == 0), stop=(kc == KC - 1)), reads=[hT, Wf], writes=[pb])
        kfb = kfb_ring.next()
        vb = vb_ring.next()
        kview = pa[:, 0:256].rearrange("p (h d) -> p h d", h=4)
        c.op("dve", lambda e: e.tensor_tensor(out=kfb[:, :, 0:64], in0=kview, in1=coef[:, :, 0:1].to_broadcast([128, 4, 64]), op=ALU.mult), reads=[pa, coefb], writes=[kfb], merge_w=True)
        c.op("dve", lambda e: e.tensor_tensor(out=kfb[:, :, 64:128], in0=kview, in1=coef[:, :, 1:2].to_broadcast([128, 4, 64]), op=ALU.mult), reads=[pa, coefb], writes=[kfb], merge_w=True)
        c.op("act", lambda e: e.activation(out=vb[:, :], in_=pb[:, 0:512], func=AF.Identity), reads=[pb], writes=[vb])
        if far:
            for h in range(4):
                c.op("pe", lambda e: e.matmul(PS[:, h * 128:(h + 1) * 128], lhsT=kfb[:, h, :], rhs=vb[:, h * 128:(h + 1) * 128], start=False, stop=False), reads=[kfb, vb], writes=[PS])
        else:
            pc = PC[n % 2]
            for h in range(4):
                c.op("pe", lambda e: e.matmul(pc[:, h * 128:(h + 1) * 128], lhsT=kfb[:, h, :], rhs=vb[:, h * 128:(h + 1) * 128], start=True, stop=True), reads=[kfb, vb], writes=[pc])
            c.op("dve", lambda e: e.tensor_copy(out=CKV[:, n, :], in_=pc[:, 0:512]), reads=[pc], writes=[CKV], merge_w=True)

    Rf = c.sb("Rf", [128, 512], F32)
    c.op("dve", lambda e: e.tensor_copy(out=Rf[:, :], in_=Sin[:, :]), reads=[Sin], writes=[Rf])
    Rlo = Buf("Rlo")
    Rhi = Buf("Rhi")
    Rlo.w = dict(Rf.w)
    Rhi.w = dict(Rf.w)
    for s in range(NT):
        nf_, nb_ = s, NT - 1 - s
        c.op("act", lambda e: e.activation(out=STb[0:64, nf_, :], in_=Rf[0:64, :], func=AF.Identity), reads=[Rlo], writes=[STb], merge_w=True)
        c.op("pool", lambda e: e.tensor_copy(out=STb[64:128, nb_, :], in_=Rf[64:128, :]), reads=[Rhi], writes=[STb], merge_w=True)
        for h in range(4):
            sl = slice(h * 128, (h + 1) * 128)
            c.op("dve", lambda e: e.scalar_tensor_tensor(out=Rf[0:64, sl], in0=Rf[0:64, sl], scalar=cdec[0:64, h:h + 1], in1=CKV[0:64, nf_, sl], op0=ALU.mult, op1=ALU.add), reads=[Rlo, cdec, CKV], writes=[Rlo])
            c.op("dve", lambda e: e.scalar_tensor_tensor(out=Rf[64:128, sl], in0=Rf[64:128, sl], scalar=cdec[64:128, h:h + 1], in1=CKV[64:128, nb_, sl], op0=ALU.mult, op1=ALU.add), reads=[Rhi, cdec, CKV], writes=[Rhi])
    phases["F"] = (c.ninst, c.nwait)

    if cfg.debug:
        c.dma("sp", lambda e: e.dma_start(out=DBG_d[:, 0:512], in_=Sin[:, :]), owner=Sin, reads=[Sin], writes=[DBG_d], merge_w=True)
        c.dma("sp", lambda e: e.dma_start(out=DBG_d[:, 512:1024], in_=CKV[:, 0, :]), owner=CKV, reads=[CKV], writes=[DBG_d], merge_w=True)
    if cfg.stop == "F":
        c.finish([DBG_d])
        c.pop_all()
        return nc, c, phases
    c.pop()

    NBO = NT // 4
    c.push()
    Wr = c.sb("Wr", [128, 8, 1792], BF16)
    for kc in range(KC):
        rows = slice(kc * 128, (kc + 1) * 128)
        st = wst_ring.next()
        c.dma("sp", lambda e: e.dma_start(out=st[:, 0:256], in_=w_in[rows, 1536:1792]), owner=st, writes=[st])
        dup = Wr[:, kc, 0:512].rearrange("p (h t d) -> p h t d", h=4, t=2)
        for two in range(2):
            c.op(("dve", "pool")[two], lambda e: e.tensor_scalar(out=dup[:, :, two, :], in0=st[:, 0:256].rearrange("p (h d) -> p h d", h=4), scalar1=nwmix[:, kc:kc + 1], scalar2=None, op0=ALU.mult), reads=[st, nwmix], writes=[Wr], merge_w=True)
        wcast(Wr[:, kc, 512:768], w_in[rows, 1792:2048], 256, col_ap=(nwmix, nwmix[:, kc:kc + 1]), mul=0.125, dstbuf=Wr)
        wcast(Wr[:, kc, 768:1280], w_in[rows, 2048:2560], 512, col_ap=(nwmix, nwmix[:, kc:kc + 1]), dstbuf=Wr)
        wcast(Wr[:, kc, 1280:1792], w_in[rows, 2560:3072], 512, col_ap=(nwmix, nwmix[:, kc:kc + 1]), dstbuf=Wr)
    GW = c.sb("GW", [128, 512], F32)
    c.dma("sp", lambda e: e.dma_start(out=GW[:, :], in_=gn_w[0:1, :].to_broadcast([128, 512])), owner=GW, writes=[GW])
    stop_here("R0")
    hTb_ring = c.ring("hTb", 2, [128, 8, 512], BF16)
    qfb_ring = c.ring("qfb", 2, [128, 4, 512], BF16)
    qpl_ring = c.ring("qpl", 2, [128, 4, 512], BF16)
    ktr_ring = c.ring("ktr", 2, [128, 2, 512], BF16)
    vb2_ring = c.ring("vb2", 2, [128, 512], BF16)
    gs_ring = c.ring("gs", 2, [128, 512], F32)
    gg_ring = c.ring("gg", 2, [128, 512], F32)
    pts_ring = c.ring("pts", 2, [128, 4, 128], BF16)
    ysq_ring = c.ring("ysq", 2, [128, 512], F32)
    yn_ring = c.ring("yn", 2, [128, 512], F32)
    ro_ring = c.ring("ro", 2, [128, 512], BF16)
    rts_ring = c.ring("rts", 2, [128, 4, 512], BF16)
    st_ring = c.ring("gst", 16, [128, 4], F32)
    pring = Ring([P[2], P[3], P[4]])
    PAt2, PY, PRT = [P[6], P[7]], P[5], P[0]
    for b in range(NBO):
        hTb = hTb_ring.next()
        for ti in range(4):
            xs = load_x(xa, (8 + 4 * b + ti) * 128)
            norm_T(xs, xs[:, :], PT[ti % 2], hTb, hTb[:, :, ti * 128:(ti + 1) * 128])
        stop_here("R1")
        qfb, qpl, ktr = qfb_ring.next(), qpl_ring.next(), ktr_ring.next()
        for h in range(4):
            hb = 64 * (h % 2)
            pq = pring.next()
            for kc in range(KC):
                c.op("pe", lambda e: e.matmul(pq[:, 0:512], lhsT=Wr[:, kc, h * 128:(h + 1) * 128], rhs=hTb[:, kc, :], start=(kc == 0), stop=(kc == KC - 1)), reads=[Wr, hTb], writes=[pq])
            stop_here("R1a")
            c.op("dve", lambda e: e.tensor_tensor(out=qfb[:, h, :].rearrange("p (a i) -> p a i", a=4), in0=pq[:, 0:512].rearrange("p (a i) -> p a i", a=4), in1=DEC[:, h:h + 1, :].to_broadcast([128, 4, 128]), op=ALU.mult), reads=[pq, DEC], writes=[qfb], merge_w=True)
            stop_here("R1b")
            c.op("dve", lambda e: e.tensor_copy(out=qpl[hb:hb + 64, h, :], in_=pq[hb:hb + 64, 0:512]), reads=[pq], writes=[qpl], merge_w=True)
            stop_here("R1c")
        for pr in range(2):
            pk = pring.next()
            for kc in range(KC):
                c.op("pe", lambda e: e.matmul(pk[:, 0:512], lhsT=Wr[:, kc, 512 + pr * 128:512 + (pr + 1) * 128], rhs=hTb[:, kc, :], start=(kc == 0), stop=(kc == KC - 1)), reads=[Wr, hTb], writes=[pk])
            c.op("act", lambda e: e.activation(out=ktr[:, pr, :], in_=pk[:, 0:512], func=AF.Identity), reads=[pk], writes=[ktr], merge_w=True)
        stop_here("R2")
        rts = rts_ring.next()
        for ti in range(4):
            n = 4 * b + ti
            tsl = slice(ti * 128, (ti + 1) * 128)
            pv = pring.next()
            for kc in range(KC):
                c.op("pe", lambda e: e.matmul(pv[:, 0:512], lhsT=hTb[:, kc, tsl], rhs=Wr[:, kc, 768:1280], start=(kc == 0), stop=(kc == KC - 1)), reads=[Wr, hTb], writes=[pv])
            vb2 = vb2_ring.next()
            c.op("act", lambda e: e.activation(out=vb2[:, :], in_=pv[:, 0:512], func=AF.Identity), reads=[pv], writes=[vb2])
            pg = pring.next()
            for kc in range(KC):
                c.op("pe", lambda e: e.matmul(pg[:, 0:512], lhsT=hTb[:, kc, tsl], rhs=Wr[:, kc, 1280:1792], start=(kc == 0), stop=(kc == KC - 1)), reads=[Wr, hTb], writes=[pg])
            gs = gs_ring.next()
            c.op("act", lambda e: e.activation(out=gs[:, :], in_=pg[:, 0:512], func=AF.Exp, scale=-1.0), reads=[pg], writes=[gs])
            c.op("act", lambda e: e.activation(out=gs[:, :], in_=gs[:, :], func=AF.Ln, bias=1.0), reads=[gs], writes=[gs])
            c.op("act", lambda e: e.activation(out=gs[:, :], in_=gs[:, :], func=AF.Exp, scale=-1.0), reads=[gs], writes=[gs])
            c.op("pool", lambda e: e.tensor_tensor(out=gs[:, :], in0=gs[:, :], in1=GW[:, :], op=ALU.mult), reads=[gs, GW], writes=[gs])
            gg = gg_ring.next()
            c.op("dve", lambda e: e.tensor_tensor(out=gg[:, :], in0=pg[:, 0:512], in1=gs[:, :], op=ALU.mult), reads=[pg, gs], writes=[gg])
            stop_here("R3")
            for h in range(4):
                hb = 64 * (h % 2)
                pat = PAt2[h % 2]
                c.op("pe", lambda e: e.matmul(pat[:, (h // 2) * 128:(h // 2 + 1) * 128], lhsT=ktr[hb:hb + 64, h // 2, tsl], rhs=qpl[hb:hb + 64, h, tsl], start=True, stop=True), reads=[ktr, qpl], writes=[pat])
            stop_here("R3a")
            pts = pts_ring.next()
            ptv = pts[:, :, :].rearrange("p (a two) i -> p a two i", two=2)
            dtv = DT[:, :, :].rearrange("p (a two) i -> p a two i", two=2)
            for par in range(2):
                c.op("dve", lambda e: e.tensor_tensor(out=ptv[:, :, par, :], in0=PAt2[par][:, 0:256].rearrange("p (a i) -> p a i", a=2), in1=dtv[:, :, par, :], op=ALU.mult), reads=[PAt2[par], DT], writes=[pts], merge_w=(par == 1))
            stop_here("R3b")
            for h in range(4):
                hs = slice(h * 128, (h + 1) * 128)
                c.op("pe", lambda e: e.matmul(PY[:, hs], lhsT=pts[:, h, :], rhs=vb2[:, hs], start=True, stop=False), reads=[pts, vb2], writes=[PY])
                c.op("pe", lambda e: e.matmul(PY[:, hs], lhsT=qfb[:, h, tsl], rhs=STb[:, n, hs], start=False, stop=True), reads=[qfb, STb], writes=[PY])
            stop_here("R4")
            s1, s2, mu, msq, var, sd, nmr = [st_ring.next() for _ in range(7)]
            pyv = PY[:, 0:512].rearrange("p (h e) -> p h e", h=4)
            c.op("dve", lambda e: e.tensor_reduce(out=s1[:, :], in_=pyv, axis=AX.X, op=ALU.add), reads=[PY], writes=[s1])
            stop_here("R4a")
            ysq = ysq_ring.next()
            c.op("act", lambda e: e.activation(out=ysq[:, :], in_=PY[:, 0:512], func=AF.Square), reads=[PY], writes=[ysq])
            stop_here("R4a2")
            c.op("dve", lambda e: e.tensor_reduce(out=s2[:, :], in_=ysq[:, :].rearrange("p (h e) -> p h e", h=4), axis=AX.X, op=ALU.add), reads=[ysq], writes=[s2])
            stop_here("R4b")
            c.op("dve", lambda e: e.tensor_scalar(out=mu[:, :], in0=s1[:, :], scalar1=1.0 / 128.0, scalar2=None, op0=ALU.mult), reads=[s1], writes=[mu])
            c.op("dve", lambda e: e.tensor_tensor(out=msq[:, :], in0=mu[:, :], in1=mu[:, :], op=ALU.mult), reads=[mu], writes=[msq])
            c.op("dve", lambda e: e.scalar_tensor_tensor(out=var[:, :], in0=s2[:, :], scalar=1.0 / 128.0, in1=msq[:, :], op0=ALU.mult, op1=ALU.subtract), reads=[s2, msq], writes=[var])
            c.op("act", lambda e: e.activation(out=sd[:, :], in_=var[:, :], func=AF.Ln, bias=GN_EPS), reads=[var], writes=[sd])
            c.op("act", lambda e: e.activation(out=sd[:, :], in_=sd[:, :], func=AF.Exp, scale=-0.5), reads=[sd], writes=[sd])
            c.op("dve", lambda e: e.scalar_tensor_tensor(out=nmr[:, :], in0=mu[:, :], scalar=-1.0, in1=sd[:, :], op0=ALU.mult, op1=ALU.mult), reads=[mu, sd], writes=[nmr])
            stop_here("R4c")
            yn = yn_ring.next()
            stop_here("R4c2")
            for h in range(4):
                hs = slice(h * 128, (h + 1) * 128)
                c.op("act", lambda e: e.activation(out=yn[:, hs], in_=PY[:, hs], func=AF.Identity, scale=sd[:, h:h + 1], bias=nmr[:, h:h + 1]), reads=[PY, sd, nmr], writes=[yn], merge_w=True)
            ro = ro_ring.next()
            c.op("dve", lambda e: e.tensor_tensor(out=ro[:, :], in0=yn[:, :], in1=gg[:, :], op=ALU.mult), reads=[yn, gg], writes=[ro])
            stop_here("R5")
            prt = pbf(PRT)
            for h in range(4):
                hs = slice(h * 128, (h + 1) * 128)
                c.op("pe", lambda e: e.transpose(out=prt[:, hs], in_=ro[:, hs], identity=ident[:, :]), reads=[ro, ident], writes=[PRT])
            c.op("act", lambda e: e.activation(out=rts[:, :, tsl], in_=prt[:, 0:512].rearrange("p (h t) -> p h t", h=4), func=AF.Identity), reads=[PRT], writes=[rts], merge_w=True)
        stop_here("R6")
        c.dma("sp", lambda e: e.dma_start(out=MIX_d.t[4:8, :, b * 512:(b + 1) * 512].rearrange("h p t -> p h t"), in_=rts[:, :, :]), owner=rts, reads=[rts], writes=[MIX_d], merge_w=True)
        stop_here("R7")
    phases["R"] = (c.ninst, c.nwait)
    if cfg.stop == "R":
        c.finish([MIX_d])
        c.pop_all()
        return nc, c, phases
    c.pop()
    c.pop()

    c.push()
    Wa = c.sb("Wa", [128, 8, 1536], BF16)
    for kc in range(KC):
        rows = slice(kc * 128, (kc + 1) * 128)
        for part in range(3):
            wcast(Wa[:, kc, part * 512:(part + 1) * 512], w_in[rows, part * 512:(part + 1) * 512], 512, col_ap=(nwmix, nwmix[:, kc:kc + 1]), dstbuf=Wa)
    hTa_ring = c.ring("hTa", 2, [128, 8, 512], BF16)
    sq_ring = c.ring("sqa", 2, [128, 512], BF16)
    ln_ring = c.ring("lna", 2, [128, 512], F32)
    stg_ring = c.ring("stga", 3, [128, 512], BF16)
    pring = Ring([P[2], P[3], P[4]])
    pssr = Ring([P[5], P[6]])
    NBA = NA // 4
    cnt = 0
    for b in range(NBA):
        own = 2 <= b < NBA - 2
        hTb = hTa_ring.next()
        for ti in range(4):
            xs = load_x(xa, (4 * b + ti) * 128)
            norm_T(xs, xs[:, :], PT[ti % 2], hTb, hTb[:, :, ti * 128:(ti + 1) * 128])
        kinds = [("k", pr) for pr in range(4)] + [("v", pr) for pr in range(4)]
        if own:
            kinds += [("q", pr) for pr in range(4)]
        for kind, pr in kinds:
            col0 = {"q": 0, "k": 512, "v": 1024}[kind] + pr * 128
            pq = pring.next()
            for kc in range(KC):
                c.op("pe", lambda e: e.matmul(pq[:, 0:512], lhsT=Wa[:, kc, col0:col0 + 128], rhs=hTb[:, kc, :], start=(kc == 0), stop=(kc == KC - 1)), reads=[Wa, hTb], writes=[pq])
            stg = stg_ring.next()
            if kind == "v":
                eng = ("act", "dve")[cnt % 2]
                cnt += 1
                if eng == "act":
                    c.op("act", lambda e: e.activation(out=stg[:, :], in_=pq[:, 0:512], func=AF.Identity), reads=[pq], writes=[stg])
                else:
                    c.op("dve", lambda e: e.tensor_copy(out=stg[:, :], in_=pq[:, 0:512]), reads=[pq], writes=[stg])
                dst = VT_d.t[pr, :, b * 512:(b + 1) * 512]
                dbuf = VT_d
            else:
                sq = sq_ring.next()
                c.op("act", lambda e: e.activation(out=sq[:, :], in_=pq[:, 0:512], func=AF.Square), reads=[pq], writes=[sq])
                pss = pssr.next()
                c.op("pe", lambda e: e.matmul(pss[:, 0:512], lhsT=blk1[:, :], rhs=sq[:, :], start=True, stop=True), reads=[blk1, sq], writes=[pss])
                ln = ln_ring.next()
                c.op("act", lambda e: e.activation(out=ln[:, :], in_=pss[:, 0:512], func=AF.Ln, scale=1.0 / 64.0, bias=EPS), reads=[pss], writes=[ln])
                c.op("act", lambda e: e.activation(out=ln[:, :], in_=ln[:, :], func=AF.Exp, scale=-0.5), reads=[ln], writes=[ln])
                wcol = aqw if kind == "q" else akw
                c.op("dve", lambda e: e.scalar_tensor_tensor(out=stg[:, :], in0=pq[:, 0:512], scalar=wcol[:, 0:1], in1=ln[:, :], op0=ALU.mult, op1=ALU.mult), reads=[pq, wcol, ln], writes=[stg])
                if kind == "q":
                    dst = QT_d.t[pr, :, (b - 2) * 512:(b - 1) * 512]
                    dbuf = QT_d
                else:
                    dst = KT_d.t[pr, :, b * 512:(b + 1) * 512]
                    dbuf = KT_d
            c.dma("sp", lambda e: e.dma_start(out=dst, in_=stg[:, :]), owner=stg, reads=[stg], writes=[dbuf], merge_w=True)
    phases["A1"] = (c.ninst, c.nwait)
    if cfg.stop == "A1":
        c.finish([QT_d, KT_d, VT_d])
        c.pop_all()
        return nc, c, phases
    c.pop()

    c.push()
    BIAS = c.sb("BIAS", [128, 24, 256], BF16)
    c.op("act", lambda e: e.activation(out=tmpA[:, :], in_=kq[:, :], func=AF.Abs, bias=-64.0), reads=[kq], writes=[tmpA])
    c.op("act", lambda e: e.activation(out=tmpB[:, :], in_=kq[:, :], func=AF.Abs, bias=64.0), reads=[kq], writes=[tmpB])
    c.op("dve", lambda e: e.tensor_scalar(out=tmpC[:, :], in0=kq[:, :], scalar1=0.0, scalar2=None, op0=ALU.is_ge), reads=[kq], writes=[tmpC])
    c.op("dve", lambda e: e.tensor_scalar(out=tmpC[:, :], in0=tmpC[:, :], scalar1=-1.0, scalar2=-NEG, op0=ALU.add, op1=ALU.mult), reads=[tmpC], writes=[tmpC])
    c.op("dve", lambda e: e.tensor_scalar(out=tmpD[:, :], in0=kq[:, :], scalar1=0.0, scalar2=None, op0=ALU.is_le), reads=[kq], writes=[tmpD])
    c.op("dve", lambda e: e.tensor_scalar(out=tmpD[:, :], in0=tmpD[:, :], scalar1=-1.0, scalar2=-NEG, op0=ALU.add, op1=ALU.mult), reads=[tmpD], writes=[tmpD])
    for h in range(8):
        for pi, d in enumerate(PATTERNS):
            sc = -SLOPES[h] * d
            ix = h * 3 + pi
            c.op("dve", lambda e: e.scalar_tensor_tensor(out=BIAS[:, ix, 0:128], in0=tmpA[:, :], scalar=sc, in1=tmpC[:, :], op0=ALU.mult, op1=ALU.add), reads=[tmpA, tmpC], writes=[BIAS], merge_w=True)
            c.op("dve", lambda e: e.scalar_tensor_tensor(out=BIAS[:, ix, 128:256], in0=tmpB[:, :], scalar=sc, in1=tmpD[:, :], op0=ALU.mult, op1=ALU.add), reads=[tmpB, tmpD], writes=[BIAS], merge_w=True)

    valt_f = c.sb("valt_f", [128, cfg.NCHT], F32)
    VALT = c.sb("VALT", [128, cfg.NCHT, 64], BF16)
    c.dma("sp", lambda e: e.dma_start(out=valt_f[:, :], in_=valt_d[:, :]), owner=valt_f, writes=[valt_f])
    c.op("dve", lambda e: e.tensor_copy(out=VALT[:, :, :], in_=valt_f[:, :].unsqueeze(2).to_broadcast([128, cfg.NCHT, 64])), reads=[valt_f], writes=[VALT])
    qts_ring = c.ring("qts", 2, [128, LS], BF16)
    kts_ring = c.ring("kts", 2, [128, NA * 128], BF16)
    vts_ring = c.ring("vts", 2, [128, NA * 128], BF16)
    NUM = c.sb("NUM", [128, LS], F32)
    DEN = c.sb("DEN", [128, LS], F32)
    ATS = c.sb("ATS", [128, LS], BF16)
    es_ring = c.ring("es", 4, [128, 512], BF16)
    vpc_ring = c.ring("vpc", 4, [128, 128], BF16)
    Sring = Ring([P[0], P[1], P[2]])
    ONr = Ring([P[3], P[4]])
    ODr = Ring([P[5], P[6]])
    PVT = P[7]
    for pr in range(4):
        qts, kts, vts = qts_ring.next(), kts_ring.next(), vts_ring.next()
        c.dma("sp", lambda e: e.dma_start(out=qts[:, :], in_=QT_d.t[pr, :, :]), owner=qts, reads=[QT_d], writes=[qts])
        c.dma("sp", lambda e: e.dma_start(out=kts[:, :], in_=KT_d.t[pr, :, :]), owner=kts, reads=[KT_d], writes=[kts])
        c.dma("sp", lambda e: e.dma_start(out=vts[:, :], in_=VT_d.t[pr, :, :]), owner=vts, reads=[VT_d], writes=[vts])
        c.op("pool", lambda e: e.memset(NUM[:, :], 0.0), writes=[NUM])
        c.op("pool", lambda e: e.memset(DEN[:, :], 0.0), writes=[DEN])
        for pi, d in enumerate(PATTERNS):
            nq = cfg.nq[pi]
            nch = cfg.nch[pi]
            for r in range(d):
                vcache = {}

                def kslice(ch):
                    k0 = d * (128 * ch - 64) + r + 1024
                    return slice(k0, k0 + 127 * d + 1, d)

                def getv(ch):
                    if ch not in vcache:
                        vc = vpc_ring.next()
                        pv = pbf(PVT)
                        c.op("pe", lambda e: e.transpose(out=pv[:, 0:128], in_=vts[:, kslice(ch)], identity=ident[:, :]), reads=[vts, ident], writes=[PVT])
                        c.op("dve", lambda e: e.tensor_copy(out=vc[:, :], in_=pv[:, 0:128]), reads=[PVT], writes=[vc])
                        vcache[ch] = vc
                    return vcache[ch]
                for mm in range(nq // 2):
                    m0 = 2 * mm
                    E = []
                    for h2 in range(2):
                        hb = 64 * h2
                        hg = 2 * pr + h2
                        ps = Sring.next()
                        ix = hg * 3 + pi
                        c.op("pe", lambda e: e.matmul(ps[:, 0:512].rearrange("p (a n) -> p a n", a=2), lhsT=ident[:, :], rhs=BIAS[:, ix:ix + 1, :].to_broadcast([128, 2, 256]), start=True, stop=False), reads=[ident, BIAS], writes=[ps])
                        for blk, (tq, ch) in enumerate([(m0, m0), (m0, m0 + 1), (m0 + 1, m0 + 1), (m0 + 1, m0 + 2)]):
                            q0 = d * 128 * tq + r
                            c.op("pe", lambda e: e.matmul(ps[:, blk * 128:(blk + 1) * 128], lhsT=kts[hb:hb + 64, kslice(ch)], rhs=qts[hb:hb + 64, q0:q0 + 127 * d + 1:d], start=False, stop=(blk == 3)), reads=[kts, qts], writes=[ps])
                        es = es_ring.next()
                        c.op("act", lambda e: e.activation(out=es[:, :], in_=ps[:, 0:512], func=AF.Exp), reads=[ps], writes=[es])
                        E.append(es)
                    on, od = ONr.next(), ODr.next()
                    for h2 in range(2):
                        hb = 64 * h2
                        for tqi in range(2):
                            osl = slice(tqi * 128, (tqi + 1) * 128)
                            for half, ch in enumerate((m0 + tqi, m0 + tqi + 1)):
                                blk = 2 * tqi + half
                                vc = getv(ch)
                                c.op("pe", lambda e: e.matmul(on[hb:hb + 64, osl], lhsT=vc[:, hb:hb + 64], rhs=E[h2][:, blk * 128:(blk + 1) * 128], start=(half == 0), stop=(half == 1)), reads=[vc, E[h2]], writes=[on])
                            for half, ch in enumerate((m0 + tqi, m0 + tqi + 1)):
                                blk = 2 * tqi + half
                                vix = cfg.chbase[pi] + r * nch + ch
                                c.op("pe", lambda e: e.matmul(od[hb:hb + 64, osl], lhsT=VALT[:, vix, :], rhs=E[h2][:, blk * 128:(blk + 1) * 128], start=(half == 0), stop=(half == 1)), reads=[VALT, E[h2]], writes=[od])
                    p0 = d * 128 * m0 + r
                    psl = slice(p0, p0 + 255 * d + 1, d)
                    c.op("dve", lambda e: e.tensor_tensor(out=NUM[:, psl], in0=NUM[:, psl], in1=on[:, 0:256], op=ALU.add), reads=[NUM, on], writes=[NUM])
                    c.op("dve", lambda e: e.tensor_tensor(out=DEN[:, psl], in0=DEN[:, psl], in1=od[:, 0:256], op=ALU.add), reads=[DEN, od], writes=[DEN])
        c.op("act", lambda e: e.activation(out=DEN[:, :], in_=DEN[:, :], func=AF.Ln), reads=[DEN], writes=[DEN])
        c.op("act", lambda e: e.activation(out=DEN[:, :], in_=DEN[:, :], func=AF.Exp, scale=-1.0), reads=[DEN], writes=[DEN])
        for q4 in range(LS // 1024):
            qs = slice(q4 * 1024, (q4 + 1) * 1024)
            c.op(("dve", "pool")[q4 % 2], lambda e: e.tensor_tensor(out=ATS[:, qs], in0=NUM[:, qs], in1=DEN[:, qs], op=ALU.mult), reads=[NUM, DEN], writes=[ATS], merge_w=True)
        c.dma("sp", lambda e: e.dma_start(out=MIX_d.t[pr, :, :], in_=ATS[:, :]), owner=ATS, reads=[ATS], writes=[MIX_d], merge_w=True)
    phases["A2"] = (c.ninst, c.nwait)
    if cfg.stop == "A2":
        c.finish([MIX_d])
        c.pop_all()
        return nc, c, phases
    c.pop()

    OHs = c.sb("OHs", [128, NT, 32], F32)
    GTs = c.sb("GTs", [128, NT, 32], F32)
    c.push()
    Wout = c.sb("Wout", [128, 8, 1024], BF16)
    Wmq = c.sb("Wmq", [128, 8, 1024], BF16)
    Wmo = c.sb("Wmo", [128, 8, 1024], BF16)
    for kc in range(KC):
        rows = slice(kc * 128, (kc + 1) * 128)
        wcast(Wout[:, kc, :], w_out[rows, :], 1024, dstbuf=Wout)
        wcast(Wmq[:, kc, :], w_mq[rows, :], 1024, col_ap=(nwmem, nwmem[:, kc:kc + 1]), dstbuf=Wmq)
        wcast(Wmo[:, kc, :], w_mo[rows, :], 1024, dstbuf=Wmo)
    Wrt = c.sb("Wrt", [128, 8, 36], F32)
    c.dma("sp", lambda e: e.dma_start(out=Wrt[:, :, :], in_=w_rt.t.ap().rearrange("(kc p) n -> p kc n", p=128)), owner=Wrt, writes=[Wrt])
    WMOE = c.sb("WMOE", [128, D], F32)
    c.dma("sp", lambda e: e.dma_start(out=WMOE[:, :], in_=nw_moe[0:1, :].to_broadcast([128, D])), owner=WMOE, writes=[WMOE])
    KmT = c.sb("KmT", [128, 8, 256], BF16)
    Vm = c.sb("Vm", [128, 2, 1024], BF16)
    sqm_ring = c.ring("sqm", 2, [128, 512], BF16)
    rsm_ring = c.ring("rsm", 2, [128, 512], F32)
    c.push()
    Wmk = c.sb("Wmk", [128, 8, 2048], BF16)
    for kc in range(KC):
        rows = slice(kc * 128, (kc + 1) * 128)
        for half in range(2):
            wcast(Wmk[:, kc, half * 1024:(half + 1) * 1024], w_mkv[rows, half * 1024:(half + 1) * 1024], 1024, col_ap=(nwmemkv, nwmemkv[:, kc:kc + 1]), dstbuf=Wmk)
    mhT = c.sb("mhT", [128, 8, 256], BF16)
    for mt in range(2):
        xs = load_x(memx, mt * 128)
        norm_T(xs, xs[:, :], PT[mt], mhT, mhT[:, :, mt * 128:(mt + 1) * 128])
    for h in range(4):
        for dc in range(2):
            cidx = 2 * h + dc
            pk = P[2 + dc]
            for kc in range(KC):
                c.op("pe", lambda e: e.matmul(pk[:, 0:256], lhsT=Wmk[:, kc, cidx * 128:(cidx + 1) * 128], rhs=mhT[:, kc, :], start=(kc == 0), stop=(kc == KC - 1)), reads=[Wmk, mhT], writes=[pk])
            sqm = sqm_ring.next()
            c.op("act", lambda e: e.activation(out=sqm[:, 0:256], in_=pk[:, 0:256], func=AF.Square), reads=[pk], writes=[sqm])
            c.op("pe", lambda e: e.matmul(P[4][:, 0:256], lhsT=ones_bf[:, :], rhs=sqm[:, 0:256], start=(dc == 0), stop=(dc == 1)), reads=[ones_bf, sqm], writes=[P[4]])
        rsm = rsm_ring.next()
        c.op("act", lambda e: e.activation(out=rsm[:, 0:256], in_=P[4][:, 0:256], func=AF.Ln, scale=1.0 / 256.0, bias=EPS), reads=[P[4]], writes=[rsm])
        c.op("act", lambda e: e.activation(out=rsm[:, 0:256], in_=rsm[:, 0:256], func=AF.Exp, scale=-0.5), reads=[rsm], writes=[rsm])
        for dc in range(2):
            c.op("dve", lambda e: e.scalar_tensor_tensor(out=KmT[:, 2 * h + dc, :], in0=P[2 + dc][:, 0:256], scalar=mkw[:, dc:dc + 1], in1=rsm[:, 0:256], op0=ALU.mult, op1=ALU.mult), reads=[P[2 + dc], mkw, rsm], writes=[KmT], merge_w=True)
    for mc in range(2):
        for half in range(2):
            pv = P[5 + half]
            for kc in range(KC):
                c.op("pe", lambda e: e.matmul(pv[:, 0:512], lhsT=mhT[:, kc, mc * 128:(mc + 1) * 128], rhs=Wmk[:, kc, 1024 + half * 512:1024 + (half + 1) * 512], start=(kc == 0), stop=(kc == KC - 1)), reads=[Wmk, mhT], writes=[pv])
            c.op("act", lambda e: e.activation(out=Vm[:, mc, half * 512:(half + 1) * 512], in_=pv[:, 0:512], func=AF.Identity), reads=[pv], writes=[Vm], merge_w=True)
    if cfg.debug:
        dbk = c.sb("dbk", [128, 2048], F32)
        c.op("dve", lambda e: e.tensor_copy(out=dbk[:, :], in_=KmT[:, :, :].rearrange("p a b -> p (a b)")), reads=[KmT], writes=[dbk])
        c.dma("sp", lambda e: e.dma_start(out=DBG_d[:, 1024:3072], in_=dbk[:, :]), owner=dbk, reads=[dbk], writes=[DBG_d], merge_w=True)
    c.pop()
    mixb = c.sb("mixb", [128, 8, 512], BF16)
    x1b = c.sb("x1b", [128, 4, D], F32)
    h2T = c.sb("h2T", [128, 8, 512], BF16)
    QmT = c.sb("QmT", [128, 8, 512], BF16)
    OmT = c.sb("OmT", [128, 8, 512], BF16)
    et_ring = c.ring("et", 2, [128, 2, 512], BF16)
    rd_ring = c.ring("rd", 2, [128, 512], F32)
    x2_ring = c.ring("x2t", 2, [128, D], F32)
    h3_ring = c.ring("h3", 2, [128, D], F32)
    h3b_ring = c.ring("h3b", 2, [128, D], BF16)
    h3T_ring = c.ring("h3T", 2, [128, 8, 128], F32)
    sm_ring = c.ring("sm", 32, [128, 36], F32)
    for b in range(NBO):
        c.dma("sp", lambda e: e.dma_start(out=mixb[:, :, :], in_=MIX_d.t[:, :, b * 512:(b + 1) * 512].rearrange("c p t -> p c t")), owner=mixb, reads=[MIX_d], writes=[mixb])
        for ti in range(4):
            tsl = slice(ti * 128, (ti + 1) * 128)
            xs = load_x(xa, (8 + 4 * b + ti) * 128)
            for half in range(2):
                px = P[5 + half]
                hs = slice(half * 512, (half + 1) * 512)
                for kc in range(KC):
                    c.op("pe", lambda e: e.matmul(px[:, 0:512], lhsT=mixb[:, kc, tsl], rhs=Wout[:, kc, hs], start=(kc == 0), stop=(kc == KC - 1)), reads=[mixb, Wout], writes=[px])
                c.op("dve", lambda e: e.tensor_tensor(out=x1b[:, ti, hs], in0=xs[:, hs], in1=px[:, 0:512], op=ALU.add), reads=[xs, px], writes=[x1b], merge_w=True)
            norm_T(x1b, x1b[:, ti, :], PT[ti % 2], h2T, h2T[:, :, tsl])
        for h in range(4):
            for dc in range(2):
                cidx = 2 * h + dc
                pq = P[2 + dc]
                for kc in range(KC):
                    c.op("pe", lambda e: e.matmul(pq[:, 0:512], lhsT=Wmq[:, kc, cidx * 128:(cidx + 1) * 128], rhs=h2T[:, kc, :], start=(kc == 0), stop=(kc == KC - 1)), reads=[Wmq, h2T], writes=[pq])
                sqm = sqm_ring.next()
                c.op("act", lambda e: e.activation(out=sqm[:, :], in_=pq[:, 0:512], func=AF.Square), reads=[pq], writes=[sqm])
                c.op("pe", lambda e: e.matmul(P[4][:, 0:512], lhsT=ones_bf[:, :], rhs=sqm[:, :], start=(dc == 0), stop=(dc == 1)), reads=[ones_bf, sqm], writes=[P[4]])
            rsm = rsm_ring.next()
            c.op("act", lambda e: e.activation(out=rsm[:, :], in_=P[4][:, 0:512], func=AF.Ln, scale=1.0 / 256.0, bias=EPS), reads=[P[4]], writes=[rsm])
            c.op("act", lambda e: e.activation(out=rsm[:, :], in_=rsm[:, :], func=AF.Exp, scale=-0.5), reads=[rsm], writes=[rsm])
            for dc in range(2):
                c.op("dve", lambda e: e.scalar_tensor_tensor(out=QmT[:, 2 * h + dc, :], in0=P[2 + dc][:, 0:512], scalar=mqw[:, dc:dc + 1], in1=rsm[:, :], op0=ALU.mult, op1=ALU.mult), reads=[P[2 + dc], mqw, rsm], writes=[QmT], merge_w=True)
        for h in range(4):
            et = et_ring.next()
            for mc in range(2):
                ps = P[2 + mc]
                for dc in range(2):
                    c.op("pe", lambda e: e.matmul(ps[:, 0:512], lhsT=KmT[:, 2 * h + dc, mc * 128:(mc + 1) * 128], rhs=QmT[:, 2 * h + dc, :], start=(dc == 0), stop=(dc == 1)), reads=[KmT, QmT], writes=[ps])
                c.op("act", lambda e: e.activation(out=et[:, mc, :], in_=ps[:, 0:512], func=AF.Exp), reads=[ps], writes=[et], merge_w=True)
            for mc in range(2):
                c.op("pe", lambda e: e.matmul(P[4][:, 0:512], lhsT=ones_bf[:, :], rhs=et[:, mc, :], start=(mc == 0), stop=(mc == 1)), reads=[ones_bf, et], writes=[P[4]])
            rd = rd_ring.next()
            c.op("act", lambda e: e.activation(out=rd[:, :], in_=P[4][:, 0:512], func=AF.Ln), reads=[P[4]], writes=[rd])
            c.op("act", lambda e: e.activation(out=rd[:, :], in_=rd[:, :], func=AF.Exp, scale=-1.0), reads=[rd], writes=[rd])
            for dvc in range(2):
                po = P[5 + dvc]
                for mc in range(2):
                    c.op("pe", lambda e: e.matmul(po[:, 0:512], lhsT=Vm[:, mc, h * 256 + dvc * 128:h * 256 + (dvc + 1) * 128], rhs=et[:, mc, :], start=(mc == 0), stop=(mc == 1)), reads=[Vm, et], writes=[po])
                c.op("dve", lambda e: e.tensor_tensor(out=OmT[:, 2 * h + dvc, :], in0=po[:, 0:512], in1=rd[:, :], op=ALU.mult), reads=[po, rd], writes=[OmT], merge_w=True)
        for ti in range(4):
            n = 4 * b + ti
            tsl = slice(ti * 128, (ti + 1) * 128)
            x2 = x2_ring.next()
            for half in range(2):
                px = P[5 + half]
                hs = slice(half * 512, (half + 1) * 512)
                for cc in range(8):
                    c.op("pe", lambda e: e.matmul(px[:, 0:512], lhsT=OmT[:, cc, tsl], rhs=Wmo[:, cc, hs], start=(cc == 0), stop=(cc == 7)), reads=[OmT, Wmo], writes=[px])
                c.op("dve", lambda e: e.tensor_tensor(out=x2[:, hs], in0=x1b[:, ti, hs], in1=px[:, 0:512], op=ALU.add), reads=[x1b, px], writes=[x2], merge_w=True)
            c.dma("sp", lambda e: e.dma_start(out=X2_d[n * 128:(n + 1) * 128, :], in_=x2[:, :]), owner=x2, reads=[x2], writes=[X2_d], merge_w=True)
            rs3 = norm_rstd(x2, x2[:, :])
            h3 = h3_ring.next()
            c.op("dve", lambda e: e.scalar_tensor_tensor(out=h3[:, :], in0=x2[:, :], scalar=rs3[:, 0:1], in1=WMOE[:, :], op0=ALU.mult, op1=ALU.mult), reads=[x2, rs3, WMOE], writes=[h3])
            h3b = h3b_ring.next()
            c.op("pool", lambda e: e.tensor_copy(out=h3b[:, :], in_=h3[:, :]), reads=[h3], writes=[h3b])
            c.dma("sp", lambda e: e.dma_start(out=H3_d[n * 128:(n + 1) * 128, :], in_=h3b[:, :]), owner=h3b, reads=[h3b], writes=[H3_d], merge_w=True)
            for kc in range(KC):
                pt = P[2 + kc // 4]
                c.op("pe", lambda e: e.transpose(out=pt[:, (kc % 4) * 128:(kc % 4 + 1) * 128], in_=h3[:, kc * 128:(kc + 1) * 128], identity=identf[:, :]), reads=[h3, identf], writes=[pt])
            h3T = h3T_ring.next()
            c.op("act", lambda e: e.activation(out=h3T[:, 0:4, :], in_=P[2][:, 0:512].rearrange("p (k t) -> p k t", k=4), func=AF.Identity), reads=[P[2]], writes=[h3T], merge_w=True)
            c.op("dve", lambda e: e.tensor_copy(out=h3T[:, 4:8, :], in_=P[3][:, 0:512].rearrange("p (k t) -> p k t", k=4)), reads=[P[3]], writes=[h3T], merge_w=True)
            for kc in range(KC):
                c.op("pe", lambda e: e.matmul(P[4][:, 0:36], lhsT=h3T[:, kc, :], rhs=Wrt[:, kc, :], start=(kc == 0), stop=(kc == KC - 1)), reads=[h3T, Wrt], writes=[P[4]])
            Ls, gmax, gm, ngm, ge, gsum, pen, ML, t8, nv1, ex, gtu, s12, dn = [sm_ring.next() for _ in range(14)]
            c.op("dve", lambda e: e.tensor_copy(out=Ls[:, 0:36], in_=P[4][:, 0:36]), reads=[P[4]], writes=[Ls])
            c.op("dve", lambda e: e.tensor_reduce(out=gmax[:, 0:1], in_=Ls[:, 0:4], axis=AX.X, op=ALU.max), reads=[Ls], writes=[gmax])
            c.op("dve", lambda e: e.tensor_scalar(out=gm[:, 0:4], in0=Ls[:, 0:4], scalar1=gmax[:, 0:1], scalar2=None, op0=ALU.is_equal), reads=[Ls, gmax], writes=[gm])
            c.op("dve", lambda e: e.tensor_scalar(out=ngm[:, 0:1], in0=gmax[:, 0:1], scalar1=-1.0, scalar2=None, op0=ALU.mult), reads=[gmax], writes=[ngm])
            c.op("act", lambda e: e.activation(out=ge[:, 0:4], in_=Ls[:, 0:4], func=AF.Exp, bias=ngm[:, 0:1], accum_out=gsum[:, 0:1]), reads=[Ls, ngm], writes=[ge, gsum])
            c.op("dve", lambda e: e.tensor_scalar(out=pen[:, 0:4], in0=gm[:, 0:4], scalar1=-1.0, scalar2=1e30, op0=ALU.add, op1=ALU.mult), reads=[gm], writes=[pen])
            c.op("dve", lambda e: e.tensor_tensor(out=ML[:, 0:32].rearrange("p (g x) -> p g x", g=4), in0=Ls[:, 4:36].rearrange("p (g x) -> p g x", g=4), in1=pen[:, 0:4].unsqueeze(2).to_broadcast([128, 4, 8]), op=ALU.add), reads=[Ls, pen], writes=[ML])
            c.op("dve", lambda e: e.max(out=t8[:, 0:8], in_=ML[:, 0:32]), reads=[ML], writes=[t8])
            c.op("dve", lambda e: e.tensor_scalar(out=OHs[:, n, :], in0=ML[:, 0:32], scalar1=t8[:, 1:2], scalar2=None, op0=ALU.is_ge), reads=[ML, t8], writes=[OHs], merge_w=True)
            c.op("dve", lambda e: e.tensor_scalar(out=nv1[:, 0:1], in0=t8[:, 0:1], scalar1=-1.0, scalar2=None, op0=ALU.mult), reads=[t8], writes=[nv1])
            c.op("act", lambda e: e.activation(out=ex[:, 0:32], in_=ML[:, 0:32], func=AF.Exp, bias=nv1[:, 0:1]), reads=[ML, nv1], writes=[ex])
            c.op("dve", lambda e: e.scalar_tensor_tensor(out=gtu[:, 0:32], in0=OHs[:, n, :], scalar=1.0, in1=ex[:, 0:32], op0=ALU.mult, op1=ALU.mult, accum_out=s12[:, 0:1]), reads=[OHs, ex], writes=[gtu, s12])
            c.op("dve", lambda e: e.tensor_tensor(out=dn[:, 0:1], in0=gsum[:, 0:1], in1=s12[:, 0:1], op=ALU.mult), reads=[gsum, s12], writes=[dn])
            c.op("dve", lambda e: e.reciprocal(out=dn[:, 0:1], in_=dn[:, 0:1]), reads=[dn], writes=[dn])
            c.op("dve", lambda e: e.tensor_scalar(out=GTs[:, n, :], in0=gtu[:, 0:32], scalar1=dn[:, 0:1], scalar2=None, op0=ALU.mult), reads=[gtu, dn], writes=[GTs], merge_w=True)
    phases["T"] = (c.ninst, c.nwait)
    if cfg.debug:
        c.dma("sp", lambda e: e.dma_start(out=DBG_d[0:128, 3072:3072 + 32], in_=GTs[:, 0, :]), owner=GTs, reads=[GTs], writes=[DBG_d], merge_w=True)
    if cfg.stop == "T":
        c.finish([X2_d, H3_d])
        c.pop_all()
        return nc, c, phases
    c.pop()

    c.push()
    TRI = c.sb("TRI", [128, 128], BF16)
    c.op("dve", lambda e: e.tensor_scalar(out=TRI[:, :], in0=kq[:, :], scalar1=0.0, scalar2=None, op0=ALU.is_lt), reads=[kq], writes=[TRI])
    RK = c.sb("RK", [128, NT, 32], F32)
    acc32 = c.sb("acc32", [128, 32], F32)
    accb_ring = c.ring("accb", 2, [128, 32], BF16)
    ohb_ring = c.ring("ohb", 2, [128, 32], BF16)
    accb = None
    for n in range(NT):
        ohb = ohb_ring.next()
        c.op("pool", lambda e: e.tensor_copy(out=ohb[:, :], in_=OHs[:, n, :]), reads=[OHs], writes=[ohb])
        pr = P[n % 2]
        c.op("pe", lambda e: e.matmul(pr[:, 0:32], lhsT=TRI[:, :], rhs=ohb[:, :], start=True, stop=(n == 0)), reads=[TRI, ohb], writes=[pr])
        if n > 0:
            c.op("pe", lambda e: e.matmul(pr[:, 0:32], lhsT=ones_bf[:, :], rhs=accb[:, :], start=False, stop=True), reads=[ones_bf, accb], writes=[pr])
        c.op("act", lambda e: e.activation(out=RK[:, n, :], in_=pr[:, 0:32], func=AF.Identity), reads=[pr], writes=[RK], merge_w=True)
        if n == 0:
            c.op("dve", lambda e: e.tensor_copy(out=acc32[:, :], in_=OHs[:, 0, :]), reads=[OHs], writes=[acc32])
        else:
            c.op("dve", lambda e: e.tensor_tensor(out=acc32[:, :], in0=acc32[:, :], in1=OHs[:, n, :], op=ALU.add), reads=[acc32, OHs], writes=[acc32])
        accb = accb_ring.next()
        c.op("dve", lambda e: e.tensor_copy(out=accb[:, :], in_=acc32[:, :]), reads=[acc32], writes=[accb])
    c.op("pe", lambda e: e.matmul(P[2][:, 0:32], lhsT=ones_bf[:, :], rhs=accb[:, :], start=True, stop=True), reads=[ones_bf, accb], writes=[P[2]])
    cnt_i = c.sb("cnt_i", [128, 32], I32)
    pcf = c.sb("pcf", [128, 32], F32)
    pend = c.sb("pend", [128, 32], F32)
    pst1 = c.sb("pst1", [128, 32], F32)
    ones32 = c.sb("ones32", [128, 32], F32)
    c.op("pool", lambda e: e.memset(ones32[:, :], 1.0), writes=[ones32])
    c.op("dve", lambda e: e.tensor_scalar(out=cnt_i[:, :], in0=P[2][:, 0:32], scalar1=127.0, scalar2=None, op0=ALU.add), reads=[P[2]], writes=[cnt_i])
    c.op("dve", lambda e: e.tensor_scalar(out=cnt_i[:, :], in0=cnt_i[:, :], scalar1=7, scalar2=7, op0=ALU.logical_shift_right, op1=ALU.logical_shift_left), reads=[cnt_i], writes=[cnt_i])
    c.op("dve", lambda e: e.tensor_copy(out=pcf[:, :], in_=cnt_i[:, :]), reads=[cnt_i], writes=[pcf])
    c.op("dve", lambda e: e.tensor_tensor_scan(out=pend[:, :], data0=ones32[:, :], data1=pcf[:, :], initial=0.0, op0=ALU.mult, op1=ALU.add), reads=[ones32, pcf], writes=[pend])
    c.op("dve", lambda e: e.tensor_tensor(out=pst1[:, :], in0=pend[:, :], in1=pcf[:, :], op=ALU.subtract), reads=[pend, pcf], writes=[pst1])
    c.op("dve", lambda e: e.tensor_scalar(out=pst1[:, :], in0=pst1[:, :], scalar1=1.0, scalar2=None, op0=ALU.add), reads=[pst1], writes=[pst1])
    thr_i = c.sb("thr_i", [128, NBLK], I32)
    thr = c.sb("thr", [128, NBLK], F32)
    cmpb = c.sb("cmpb", [128, NBLK, 32], F32)
    bef = c.sb("bef", [128, NBLK], F32)
    pidx_i = c.sb("pidx_i", [128, 1], I32)
    pidx = c.sb("pidx", [128, 1], F32)
    widx_f = c.sb("widx_f", [128, NBLK, 2], F32)
    WIDX = c.sb("WIDX", [128, NBLK, 2], I32)
    c.op("pool", lambda e: e.iota(thr_i[:, :], pattern=[[128, NBLK]], base=0, channel_multiplier=0), writes=[thr_i])
    c.op("dve", lambda e: e.tensor_copy(out=thr[:, :], in_=thr_i[:, :]), reads=[thr_i], writes=[thr])
    c.op("dve", lambda e: e.tensor_tensor(out=cmpb[:, :, :], in0=pend[:, :].unsqueeze(1).to_broadcast([128, NBLK, 32]), in1=thr[:, :].unsqueeze(2).to_broadcast([128, NBLK, 32]), op=ALU.is_le), reads=[pend, thr], writes=[cmpb])
    c.op("dve", lambda e: e.tensor_reduce(out=bef[:, :], in_=cmpb[:, :, :], axis=AX.X, op=ALU.add), reads=[cmpb], writes=[bef])
    c.op("dve", lambda e: e.tensor_scalar(out=bef[:, :], in0=bef[:, :], scalar1=31.0, scalar2=None, op0=ALU.min), reads=[bef], writes=[bef])
    c.op("pool", lambda e: e.iota(pidx_i[:, :], pattern=[[0, 1]], base=0, channel_multiplier=1), writes=[pidx_i])
    c.op("dve", lambda e: e.tensor_copy(out=pidx[:, :], in_=pidx_i[:, :]), reads=[pidx_i], writes=[pidx])
    c.op("dve", lambda e: e.tensor_scalar(out=widx_f[:, :, 0], in0=bef[:, :], scalar1=128.0, scalar2=pidx[:, 0:1], op0=ALU.mult, op1=ALU.add), reads=[bef, pidx], writes=[widx_f], merge_w=True)
    c.op("dve", lambda e: e.tensor_scalar(out=widx_f[:, :, 0], in0=widx_f[:, :, 0], scalar1=2.0, scalar2=None, op0=ALU.mult), reads=[widx_f], writes=[widx_f], merge_w=True)
    c.op("dve", lambda e: e.tensor_scalar(out=widx_f[:, :, 1], in0=widx_f[:, :, 0], scalar1=1.0, scalar2=None, op0=ALU.add), reads=[widx_f], writes=[widx_f], merge_w=True)
    c.op("dve", lambda e: e.tensor_copy(out=WIDX[:, :, :], in_=widx_f[:, :, :]), reads=[widx_f], writes=[WIDX])
    DSTf = c.sb("DSTf", [128, NT, 2], F32)
    DSTi = c.sb("DSTi", [128, NT, 2], I32)
    GAB = c.sb("GAB", [128, NT, 2], F32)
    m1_ring = c.ring("m1r", 4, [128, 32], F32)
    sc_ring = c.ring("scr", 8, [128, 2], F32)
    for n in range(NT):
        d1, m1, mb, jk2 = [m1_ring.next() for _ in range(4)]
        db1, sm, gs = [sc_ring.next() for _ in range(3)]
        c.op("dve", lambda e: e.tensor_tensor(out=d1[:, :], in0=RK[:, n, :], in1=pst1[:, :], op=ALU.add), reads=[RK, pst1], writes=[d1])
        c.op("dve", lambda e: e.tensor_tensor(out=m1[:, :], in0=d1[:, :], in1=OHs[:, n, :], op=ALU.mult), reads=[d1, OHs], writes=[m1])
        c.op("dve", lambda e: e.tensor_reduce(out=db1[:, 0:1], in_=m1[:, :], axis=AX.X, op=ALU.max), reads=[m1], writes=[db1])
        c.op("dve", lambda e: e.tensor_reduce(out=sm[:, 0:1], in_=m1[:, :], axis=AX.X, op=ALU.add), reads=[m1], writes=[sm])
        c.op("dve", lambda e: e.scalar_tensor_tensor(out=DSTf[:, n, 0:1], in0=sm[:, 0:1], scalar=-1.0, in1=db1[:, 0:1], op0=ALU.add, op1=ALU.subtract), reads=[sm, db1], writes=[DSTf], merge_w=True)
        c.op("dve", lambda e: e.tensor_scalar(out=DSTf[:, n, 1:2], in0=db1[:, 0:1], scalar1=-1.0, scalar2=None, op0=ALU.add), reads=[db1], writes=[DSTf], merge_w=True)
        c.op("dve", lambda e: e.tensor_scalar(out=mb[:, :], in0=m1[:, :], scalar1=db1[:, 0:1], scalar2=None, op0=ALU.is_ge), reads=[m1, db1], writes=[mb])
        c.op("dve", lambda e: e.scalar_tensor_tensor(out=jk2[:, :], in0=GTs[:, n, :], scalar=1.0, in1=mb[:, :], op0=ALU.mult, op1=ALU.mult, accum_out=GAB[:, n, 1:2]), reads=[GTs, mb], writes=[jk2, GAB], merge_w=True)
        c.op("dve", lambda e: e.tensor_reduce(out=gs[:, 0:1], in_=GTs[:, n, :], axis=AX.X, op=ALU.add), reads=[GTs], writes=[gs])
        c.op("dve", lambda e: e.tensor_tensor(out=GAB[:, n, 0:1], in0=gs[:, 0:1], in1=GAB[:, n, 1:2], op=ALU.subtract), reads=[gs, GAB], writes=[GAB], merge_w=True)
    c.op("dve", lambda e: e.tensor_copy(out=DSTi[:, :, :], in_=DSTf[:, :, :]), reads=[DSTf], writes=[DSTi])
    fillb = c.sb("fillb", [128, NBLK, 16], I32)
    TOK16 = c.sb("TOK16", [128, NT, 16], I32)
    zpad = c.sb("zpad", [128, D], BF16)
    c.op("pool", lambda e: e.iota(fillb[:, :, :], pattern=[[0, NBLK], [0, 16]], base=NTOK, channel_multiplier=0), writes=[fillb])
    c.op("pool", lambda e: e.iota(TOK16[:, :, :], pattern=[[128, NT], [0, 16]], base=0, channel_multiplier=1), writes=[TOK16])
    c.op("pool", lambda e: e.memset(zpad[:, :], 0.0), writes=[zpad])
    c.dma("sp", lambda e: e.dma_start(out=SLOT_d.t.ap().rearrange("(b p) x -> p b x", p=128), in_=fillb[:, :, :]), owner=fillb, reads=[fillb], writes=[SLOT_d])
    c.dma("sp", lambda e: e.dma_start(out=H3_d[NTOK:NTOK + 128, :], in_=zpad[:, :]), owner=zpad, reads=[zpad], writes=[H3_d], merge_w=True)
    for n in range(NT):
        for k2 in range(2):
            c.dma("pool", lambda e: e.indirect_dma_start(out=SLOT_d.t.ap(), out_offset=bass.IndirectOffsetOnAxis(ap=DSTi[:, n, k2:k2 + 1], axis=0), in_=TOK16[:, n, :], in_offset=None), owner=TOK16, reads=[TOK16, DSTi], writes=[SLOT_d], merge_w=True)
    phases["M1"] = (c.ninst, c.nwait)
    if cfg.debug:
        c.dma("sp", lambda e: e.dma_start(out=DBG_d[:, 3200:3200 + 2 * NT], in_=DSTf[:, :, :].rearrange("p n k -> p (n k)")), owner=DSTf, reads=[DSTf], writes=[DBG_d], merge_w=True)
        c.dma("sp", lambda e: e.dma_start(out=DBG_d[:, 3400:3400 + 2 * NT], in_=GAB[:, :, :].rearrange("p n k -> p (n k)")), owner=GAB, reads=[GAB], writes=[DBG_d], merge_w=True)
        c.dma("sp", lambda e: e.dma_start(out=DBG_d[:, 3600:3600 + NBLK], in_=bef[:, :]), owner=bef, reads=[bef], writes=[DBG_d], merge_w=True)
    if cfg.stop == "M1":
        c.finish([SLOT_d])
        c.pop_all()
        return nc, c, phases

    w1v = w1.t.ap().rearrange("e (p k) f -> (e p) (k f)", p=128).rearrange("r (two x) -> (r two) x", two=2)
    w3v = w3.t.ap().rearrange("e (p k) f -> (e p) (k f)", p=128).rearrange("r (two x) -> (r two) x", two=2)
    w2v = w2.t.ap().rearrange("e (p k) f -> (e p) (k f)", p=128).rearrange("r (two x) -> (r two) x", two=2)
    sidx_ring = c.ring("sidx", 3, [128, 16], I32)
    xg_ring = c.ring("xg", 2, [128, D], BF16)
    xT_ring = c.ring("xTm", 2, [128, 8, 128], BF16)
    w1b_ring = c.ring("w1b", 2, [128, 4096], BF16)
    w3b_ring = c.ring("w3b", 2, [128, 4096], BF16)
    w2b_ring = c.ring("w2b", 2, [128, 4096], BF16)
    g1_ring = c.ring("g1", 2, [128, 512], F32)
    ggb_ring = c.ring("ggb", 2, [128, 512], BF16)
    gT_ring = c.ring("gTm", 2, [128, 4, 128], BF16)
    ys_ring = c.ring("ys", 2, [128, D], F32)
    for b in range(NBLK):
        sidx = sidx_ring.next()
        c.dma("sp", lambda e: e.dma_start(out=sidx[:, :], in_=SLOT_d[b * 128:(b + 1) * 128, :]), owner=sidx, reads=[SLOT_d], writes=[sidx])
        xg = xg_ring.next()
        c.dma("pool", lambda e: e.indirect_dma_start(out=xg[:, :], out_offset=None, in_=H3_d.t.ap(), in_offset=bass.IndirectOffsetOnAxis(ap=sidx[:, 0:1], axis=0)), owner=xg, reads=[H3_d, sidx], writes=[xg])
        w1b, w3b, w2b = w1b_ring.next(), w3b_ring.next(), w2b_ring.next()
        for wv, wb in ((w1v, w1b), (w3v, w3b), (w2v, w2b)):
            for half in range(2):
                c.dma("pool", lambda e: e.indirect_dma_start(out=wb[:, half * 2048:(half + 1) * 2048], out_offset=None, in_=wv, in_offset=bass.IndirectOffsetOnAxis(ap=WIDX[:, b, half:half + 1], axis=0)), owner=wb, reads=[WIDX], writes=[wb], merge_w=(half == 1))
        pxt = P[b % 2]
        pv = pbf(pxt)
        for kc in range(KC):
            c.op("pe", lambda e: e.transpose(out=pv[:, kc * 128:(kc + 1) * 128], in_=xg[:, kc:kc + 8 * 127 + 1:8], identity=ident[:, :]), reads=[xg, ident], writes=[pxt])
        xT = xT_ring.next()
        c.op("act", lambda e: e.activation(out=xT[:, :, :], in_=pv[:, 0:1024].rearrange("p (k t) -> p k t", k=8), func=AF.Identity), reads=[pxt], writes=[xT])
        ph1, ph3 = P[2 + 2 * (b % 2)], P[3 + 2 * (b % 2)]
        w1k = w1b[:, :].rearrange("p (k f) -> p k f", k=8)
        w3k = w3b[:, :].rearrange("p (k f) -> p k f", k=8)
        w2k = w2b[:, :].rearrange("p (k f) -> p k f", k=4)
        for kc in range(KC):
            c.op("pe", lambda e: e.matmul(ph1[:, 0:512], lhsT=xT[:, kc, :], rhs=w1k[:, kc, :], start=(kc == 0), stop=(kc == KC - 1)), reads=[xT, w1b], writes=[ph1])
        for kc in range(KC):
            c.op("pe", lambda e: e.matmul(ph3[:, 0:512], lhsT=xT[:, kc, :], rhs=w3k[:, kc, :], start=(kc == 0), stop=(kc == KC - 1)), reads=[xT, w3b], writes=[ph3])
        g1 = g1_ring.next()
        c.op("act", lambda e: e.activation(out=g1[:, :], in_=ph1[:, 0:512], func=AF.Exp, scale=-1.0), reads=[ph1], writes=[g1])
        c.op("act", lambda e: e.activation(out=g1[:, :], in_=g1[:, :], func=AF.Ln, bias=1.0), reads=[g1], writes=[g1])
        c.op("act", lambda e: e.activation(out=g1[:, :], in_=g1[:, :], func=AF.Exp, scale=-1.0), reads=[g1], writes=[g1])
        c.op("dve", lambda e: e.tensor_tensor(out=g1[:, :], in0=g1[:, :], in1=ph1[:, 0:512], op=ALU.mult), reads=[g1, ph1], writes=[g1])
        ggb = ggb_ring.next()
        c.op("dve", lambda e: e.tensor_tensor(out=ggb[:, :], in0=g1[:, :], in1=ph3[:, 0:512], op=ALU.mult), reads=[g1, ph3], writes=[ggb])
        for fc in range(4):
            c.op("pe", lambda e: e.transpose(out=pv[:, fc * 128:(fc + 1) * 128], in_=ggb[:, fc:fc + 4 * 127 + 1:4], identity=ident[:, :]), reads=[ggb, ident], writes=[pxt])
        gT = gT_ring.next()
        c.op("act", lambda e: e.activation(out=gT[:, :, :], in_=pv[:, 0:512].rearrange("p (k t) -> p k t", k=4), func=AF.Identity), reads=[pxt], writes=[gT])
        ys = ys_ring.next()
        for half in range(2):
            py = P[6 + half]
            for fc in range(4):
                c.op("pe", lambda e: e.matmul(py[:, 0:512], lhsT=gT[:, fc, :], rhs=w2k[:, fc, half * 512:(half + 1) * 512], start=(fc == 0), stop=(fc == 3)), reads=[gT, w2b], writes=[py])
            if half == 0:
                c.op("act", lambda e: e.activation(out=ys[:, 0:512], in_=py[:, 0:512], func=AF.Identity), reads=[py], writes=[ys], merge_w=True)
            else:
                c.op("dve", lambda e: e.tensor_copy(out=ys[:, 512:1024], in_=py[:, 0:512]), reads=[py], writes=[ys], merge_w=True)
        c.dma("sp", lambda e: e.dma_start(out=YS_d[b * 128:(b + 1) * 128, :], in_=ys[:, :]), owner=ys, reads=[ys], writes=[YS_d], merge_w=True)
    phases["M2"] = (c.ninst, c.nwait)

    ya_ring = c.ring("ya", 2, [128, D], F32)
    yb_ring = c.ring("yb", 2, [128, D], F32)
    x2c_ring = c.ring("x2c", 2, [128, D], F32)
    oo_ring = c.ring("oo", 2, [128, D], F32)
    for n in range(NT):
        ya, yb, x2c, oo = ya_ring.next(), yb_ring.next(), x2c_ring.next(), oo_ring.next()
        c.dma("pool", lambda e: e.indirect_dma_start(out=ya[:, :], out_offset=None, in_=YS_d.t.ap(), in_offset=bass.IndirectOffsetOnAxis(ap=DSTi[:, n, 0:1], axis=0)), owner=ya, reads=[YS_d, DSTi], writes=[ya])
        c.dma("pool", lambda e: e.indirect_dma_start(out=yb[:, :], out_offset=None, in_=YS_d.t.ap(), in_offset=bass.IndirectOffsetOnAxis(ap=DSTi[:, n, 1:2], axis=0)), owner=yb, reads=[YS_d, DSTi], writes=[yb])
        c.dma("sp", lambda e: e.dma_start(out=x2c[:, :], in_=X2_d[n * 128:(n + 1) * 128, :]), owner=x2c, reads=[X2_d], writes=[x2c])
        c.op("dve", lambda e: e.scalar_tensor_tensor(out=oo[:, :], in0=ya[:, :], scalar=GAB[:, n, 0:1], in1=x2c[:, :], op0=ALU.mult, op1=ALU.add), reads=[ya, GAB, x2c], writes=[oo])
        c.op("dve", lambda e: e.scalar_tensor_tensor(out=oo[:, :], in0=yb[:, :], scalar=GAB[:, n, 1:2], in1=oo[:, :], op0=ALU.mult, op1=ALU.add), reads=[yb, GAB, oo], writes=[oo])
        c.dma("sp", lambda e: e.dma_start(out=y_d[n * 128:(n + 1) * 128, :], in_=oo[:, :]), owner=oo, reads=[oo], writes=[y_d], merge_w=True)
    phases["M3"] = (c.ninst, c.nwait)
    c.finish([y_d])
    c.pop_all()
    return nc, c, phases


def prep_core(inp, cfg, b, j, S):
    LS, NA, NF = cfg.LS, cfg.NA, cfg.NF
    t0 = j * LS
    x = inp["x"][b]
    xa = np.zeros((NA * 128, D), np.float32)
    lo, hi = t0 - 1024, t0 + LS + 1024
    slo, shi = max(lo, 0), min(hi, S)
    xa[slo - lo:shi - lo] = x[slo:shi]
    far_idx = np.concatenate([np.arange(0, t0), np.arange(t0 + LS, S)]).astype(np.int64)
    assert far_idx.size == NF * 128
    xf = np.ascontiguousarray(x[far_idx])
    posf = np.ascontiguousarray(far_idx.astype(np.float32).reshape(NF, 128).T)
    t01 = np.zeros((128, 2), np.float32)
    t01[:, 0] = t0
    t01[:, 1] = t0 + LS
    valt = np.zeros((128, cfg.NCHT), np.float32)
    kk = np.arange(128)
    for pi, d in enumerate(PATTERNS):
        for r in range(d):
            for ch in range(cfg.nch[pi]):
                T = d * (128 * ch - 64 + kk) + r + t0
                valt[:, cfg.chbase[pi] + r * cfg.nch[pi] + ch] = ((T >= 0) & (T < S)).astype(np.float32)

    def kc_layout(v):
        return np.ascontiguousarray(v.reshape(8, 128).T)

    def f32(a):
        return np.ascontiguousarray(a, dtype=np.float32)
    m = {
        "xa": xa, "xf": xf, "posf": posf, "t01": t01, "valt": valt,
        "memx": f32(inp["mem"][b]),
        "nw_mix": kc_layout(inp["norm_mix_w"][0]),
        "nw_mem": kc_layout(inp["norm_mem_w"][0]),
        "nw_memkv": kc_layout(inp["norm_memkv_w"][0]),
        "nw_moe": f32(inp["norm_moe_w"][0].reshape(1, D)),
        "aq_w": f32(np.tile(inp["attn_q_norm_w"][0], 2).reshape(128, 1)),
        "ak_w": f32(np.tile(inp["attn_k_norm_w"][0], 2).reshape(128, 1)),
        "mq_w": f32(inp["mem_q_norm_w"][0].reshape(2, 128).T),
        "mk_w": f32(inp["mem_k_norm_w"][0].reshape(2, 128).T),
        "dec": f32(np.concatenate([inp["ret_decay_f"][0], inp["ret_decay_b"][0]]).reshape(1, 8)),
        "gn_w": f32(inp["ret_gn_w"][0].reshape(1, 512)),
        "w_in": f32(inp["w_in"][0]), "w_out": f32(inp["w_out"][0]),
        "w_mq": f32(inp["w_mq"][0]), "w_mkv": f32(inp["w_mkv"][0]), "w_mo": f32(inp["w_mo"][0]),
        "w_rt": f32(np.concatenate([inp["w_group"][0], inp["w_router"][0].transpose(1, 0, 2).reshape(D, 32)], axis=1)),
        "w1": f32(inp["w_exp_gate"][0]), "w3": f32(inp["w_exp_up"][0]), "w2": f32(inp["w_exp_down"][0]),
    }
    return m


_CACHE = {}


def kernel(**inputs):
    inp = {k: np.asarray(v) for k, v in inputs.items()}
    B, S, _ = inp["x"].shape
    cfg = Cfg(NT=32, NF=96)
    assert S == 4 * cfg.LS and B == 2
    if "nc" not in _CACHE:
        _CACHE["nc"] = build(cfg)[0]
    nc = _CACHE["nc"]
    in_maps = [prep_core(inp, cfg, cidx // 4, cidx % 4, S) for cidx in range(8)]
    res = run_bass_kernel_spmd(nc, in_maps, core_ids=list(range(8)))
    out = np.empty((B, S, D), np.float32)
    for cidx in range(8):
        b, j = cidx // 4, cidx % 4
        out[b, j * cfg.LS:(j + 1) * cfg.LS] = np.asarray(res.results[cidx]["y"], dtype=np.float32)
    return out
```

```python
import contextlib
import numpy as np
import concourse.bass as bass
import concourse.mybir as mybir
from concourse.bass_utils import run_bass_kernel_spmd

F32 = mybir.dt.float32
BF16 = mybir.dt.bfloat16
I32 = mybir.dt.int32
AF = mybir.ActivationFunctionType
ALU = mybir.AluOpType
AX = mybir.AxisListType

D = 1024
KC = 8
NEG = -30000.0
EPS = 1e-6
GN_EPS = 1e-5
PATTERNS = (1, 4, 16)
SLOPES = [2.0 ** (-8.0 * (h + 1) / 8.0) for h in range(8)]


class Buf:
    def __init__(self, name, t=None):
        self.name = name
        self.t = t
        self.w = {}
        self.r = {}
        self.dsem = {}
        self.dcnt = {}
        self.excl = False

    def __getitem__(self, idx):
        return self.t[idx]


class Ring:
    def __init__(self, bufs):
        self.bufs = bufs
        self.i = 0

    def next(self):
        b = self.bufs[self.i % len(self.bufs)]
        self.i += 1
        return b


class Ctx:
    def __init__(self, nc):
        self.nc = nc
        self.e = {"pe": nc.tensor, "act": nc.scalar, "dve": nc.vector,
                  "pool": nc.gpsimd, "sp": nc.sync}
        self.psem = {}
        self.pcnt = {}
        for k in ("pe", "act", "dve", "pool"):
            self.psem[k] = nc.alloc_semaphore("prog_" + k)
            self.pcnt[k] = 0
        self.seen = {k: {} for k in self.e}
        self.nwait = 0
        self.ninst = 0
        self.nsem = 4
        self.owners = []
        self.stacks = []
        self.bar_sem = nc.alloc_semaphore("bar")
        self.bar_cnt = 0

    def sb(self, name, shape, dt=F32):
        if self.stacks:
            t = self.stacks[-1].enter_context(self.nc.sbuf_tensor(name, list(shape), dt, side="right"))
            return Buf(name, t)
        return Buf(name, self.nc.alloc_sbuf_tensor(name, list(shape), dt, side="left"))

    def barrier(self):
        need = {}
        for k in self.psem:
            if self.pcnt[k] > 0:
                need["prog_" + k] = (self.psem[k], self.pcnt[k])
        for o in self.owners:
            for qt, sm in o.dsem.items():
                need["d%s_%s" % (qt, o.name)] = (sm, o.dcnt[qt])
        self._wait("sp", need)
        self.bar_cnt += 1
        self.e["sp"].sem_inc(self.bar_sem, 1)
        for k in ("pe", "act", "dve", "pool"):
            self.e[k].wait_ge(self.bar_sem, self.bar_cnt)
            for kk, (sm, v) in need.items():
                if self.seen[k].get(kk, 0) < v:
                    self.seen[k][kk] = v

    def push(self):
        self.stacks.append(contextlib.ExitStack())

    def pop(self):
        self.barrier()
        self.stacks.pop().close()

    def pop_all(self):
        while self.stacks:
            self.pop()

    def ring(self, name, n, shape, dt=F32):
        return Ring([self.sb("%s%d" % (name, i), shape, dt) for i in range(n)])

    def ps(self, name):
        b = Buf(name, self.nc.alloc_psum_tensor(name, [128, 512], F32))
        b.excl = True
        return b

    def dram(self, name, shape, dt, kind="Internal"):
        return Buf(name, self.nc.dram_tensor(name, list(shape), dt, kind=kind))

    def _collect(self, eng, reads, writes):
        need = {}

        def add(d):
            for k, (s, v) in d.items():
                if k not in need or need[k][1] < v:
                    need[k] = (s, v)
        own = "prog_" + eng
        for b in reads:
            add(b.w)
            if b.excl:
                add({k: v for k, v in b.r.items() if k != own})
        for b in writes:
            add(b.w)
            add(b.r)
        if eng == "pe":
            need.pop("prog_pe", None)
        return need

    def _wait(self, eng, need):
        seen = self.seen[eng]
        for k, (s, v) in need.items():
            if seen.get(k, 0) < v:
                self.e[eng].wait_ge(s, v)
                seen[k] = v
                self.nwait += 1

    def _record(self, key, sem, val, reads, writes, merge_w):
        for b in reads:
            old = b.r.get(key)
            if old is None or old[1] < val:
                b.r[key] = (sem, val)
        for b in writes:
            if merge_w:
                b.w[key] = (sem, val)
            else:
                b.w = {key: (sem, val)}
                b.r = {}

    def op(self, eng, fn, reads=(), writes=(), merge_w=False):
        need = self._collect(eng, reads, writes)
        self._wait(eng, need)
        inst = fn(self.e[eng])
        self.pcnt[eng] += 1
        inst.then_inc(self.psem[eng], 1)
        self.ninst += 1
        self._record("prog_" + eng, self.psem[eng], self.pcnt[eng], reads, writes, merge_w)
        return inst

    def dma(self, q, fn, owner, reads=(), writes=(), merge_w=False):
        need = self._collect(q, reads, writes)
        self._wait(q, need)
        qt = "sw" if q == "pool" else "hw"
        if qt not in owner.dsem:
            owner.dsem[qt] = self.nc.alloc_semaphore("d%s_%s" % (qt, owner.name))
            owner.dcnt[qt] = 0
            self.nsem += 1
            if owner not in self.owners:
                self.owners.append(owner)
        inst = fn(self.e[q])
        owner.dcnt[qt] += 16
        inst.then_inc(owner.dsem[qt], 16)
        self.ninst += 1
        self._record("d%s_%s" % (qt, owner.name), owner.dsem[qt], owner.dcnt[qt], reads, writes, merge_w)
        return inst

    def finish(self, bufs=(), eng="sp"):
        need = {}
        for o in self.owners:
            for qt, s in o.dsem.items():
                need["d%s_%s" % (qt, o.name)] = (s, o.dcnt[qt])
        for b in bufs:
            for k, (s, v) in b.w.items():
                if k not in need or need[k][1] < v:
                    need[k] = (s, v)
        self._wait(eng, need)


class Cfg:
    def __init__(self, NT=32, NF=96, debug=False, stop=None):
        self.NT = NT
        self.NH = 8
        self.NF = NF
        self.NA = NT + 16
        self.LS = NT * 128
        self.NBLK = 2 * NT + 32
        self.debug = debug
        self.stop = stop
        self.nq = [self.LS // d // 128 for d in PATTERNS]
        self.nch = [q + 1 for q in self.nq]
        self.chbase = []
        o = 0
        for d, n in zip(PATTERNS, self.nch):
            self.chbase.append(o)
            o += d * n
        self.NCHT = o


class _Stop(Exception):
    pass


def build(cfg):
    nc = bass.Bass("TRN2", target_bir_lowering=False)
    c = Ctx(nc)
    phases = {}
    try:
        return _build_body(cfg, nc, c, phases)
    except _Stop:
        c.finish()
        c.pop_all()
        return nc, c, phases


def _build_body(cfg, nc, c, phases):
    NT, NF, NA, LS, NBLK = cfg.NT, cfg.NF, cfg.NA, cfg.LS, cfg.NBLK
    NTOK = LS
    dbg = "ExternalOutput" if cfg.debug else "Internal"

    def din(name, shape, dt=F32):
        return c.dram(name, shape, dt, kind="ExternalInput")

    xa = din("xa", [NA * 128, D])
    xf = din("xf", [NF * 128, D])
    posf = din("posf", [128, NF])
    t01 = din("t01", [128, 2])
    valt_d = din("valt", [128, cfg.NCHT])
    memx = din("memx", [256, D])
    nw_mix = din("nw_mix", [128, 8])
    nw_mem = din("nw_mem", [128, 8])
    nw_memkv = din("nw_memkv", [128, 8])
    nw_moe = din("nw_moe", [1, D])
    aq_w = din("aq_w", [128, 1])
    ak_w = din("ak_w", [128, 1])
    mq_w = din("mq_w", [128, 2])
    mk_w = din("mk_w", [128, 2])
    dec_d = din("dec", [1, 8])
    gn_w = din("gn_w", [1, 512])
    w_in = din("w_in", [D, 3072])
    w_out = din("w_out", [D, D])
    w_mq = din("w_mq", [D, D])
    w_mkv = din("w_mkv", [D, 2 * D])
    w_mo = din("w_mo", [D, D])
    w_rt = din("w_rt", [D, 36])
    w1 = din("w1", [32, D, 512])
    w3 = din("w3", [32, D, 512])
    w2 = din("w2", [32, 512, D])
    y_d = c.dram("y", [NTOK, D], F32, kind="ExternalOutput")

    QT_d = c.dram("QT_d", [4, 128, LS], BF16, kind=dbg)
    KT_d = c.dram("KT_d", [4, 128, NA * 128], BF16, kind=dbg)
    VT_d = c.dram("VT_d", [4, 128, NA * 128], BF16, kind=dbg)
    MIX_d = c.dram("MIX_d", [8, 128, LS], BF16, kind=dbg)
    X2_d = c.dram("X2_d", [NTOK, D], F32, kind=dbg)
    H3_d = c.dram("H3_d", [NTOK + 128, D], BF16, kind=dbg)
    YS_d = c.dram("YS_d", [NBLK * 128, D], F32, kind=dbg)
    SLOT_d = c.dram("SLOT_d", [NBLK * 128, 16], I32, kind=dbg)
    if cfg.debug:
        DBG_d = c.dram("DBG_d", [128, 4096], F32, kind="ExternalOutput")

    P = [c.ps("pb%d" % i) for i in range(8)]

    def pbf(b):
        return b.t.bitcast(BF16)

    ident = c.sb("ident", [128, 128], BF16)
    identf = c.sb("identf", [128, 128], F32)
    ones_bf = c.sb("ones_bf", [128, 128], BF16)
    zeros_bf = c.sb("zeros_bf", [128, 128], BF16)
    blk1 = c.sb("blk1", [128, 128], BF16)
    kq = c.sb("kq", [128, 128], F32)
    kqi = c.sb("kqi", [128, 128], I32)
    tmpA = c.sb("tmpA", [128, 128], F32)
    tmpB = c.sb("tmpB", [128, 128], F32)
    tmpC = c.sb("tmpC", [128, 128], F32)
    tmpD = c.sb("tmpD", [128, 128], F32)

    c.op("pool", lambda e: e.iota(kqi[:, :], pattern=[[-1, 128]], base=0, channel_multiplier=1), writes=[kqi])
    c.op("dve", lambda e: e.tensor_copy(out=kq[:, :], in_=kqi[:, :]), reads=[kqi], writes=[kq])
    c.op("dve", lambda e: e.tensor_scalar(out=identf[:, :], in0=kq[:, :], scalar1=0.0, scalar2=None, op0=ALU.is_equal), reads=[kq], writes=[identf])
    c.op("dve", lambda e: e.tensor_copy(out=ident[:, :], in_=identf[:, :]), reads=[identf], writes=[ident])
    c.op("pool", lambda e: e.memset(ones_bf[:, :], 1.0), writes=[ones_bf])
    c.op("pool", lambda e: e.memset(zeros_bf[:, :], 0.0), writes=[zeros_bf])
    zrow = c.sb("zrow", [128, 512], BF16)
    c.op("pool", lambda e: e.memset(zrow[:, :], 0.0), writes=[zrow])
    c.op("pool", lambda e: e.memset(blk1[:, :], 0.0), writes=[blk1])
    c.op("pool", lambda e: e.memset(blk1[0:64, 0:64], 1.0), writes=[blk1], merge_w=True)
    c.op("pool", lambda e: e.memset(blk1[64:128, 64:128], 1.0), writes=[blk1], merge_w=True)

    def load_small(name, src_ap, shape, dt=F32):
        b = c.sb(name, shape, dt)
        c.dma("sp", lambda e: e.dma_start(out=b.t[tuple(slice(None) for _ in shape)], in_=src_ap), owner=b, writes=[b])
        return b

    nwmix = load_small("nwmix", nw_mix[:, :], [128, 8])
    nwmem = load_small("nwmem", nw_mem[:, :], [128, 8])
    nwmemkv = load_small("nwmemkv", nw_memkv[:, :], [128, 8])
    aqw = load_small("aqw", aq_w[:, :], [128, 1])
    akw = load_small("akw", ak_w[:, :], [128, 1])
    mqw = load_small("mqw", mq_w[:, :], [128, 2])
    mkw = load_small("mkw", mk_w[:, :], [128, 2])
    decb = load_small("decb", dec_d[0:1, :].to_broadcast([128, 8]), [128, 8])
    posf_s = load_small("posf_s", posf[:, :], [128, NF])
    t01_s = load_small("t01_s", t01[:, :], [128, 2])
    c.op("dve", lambda e: e.tensor_scalar(out=aqw[:, :], in0=aqw[:, :], scalar1=0.125, scalar2=None, op0=ALU.mult), reads=[aqw], writes=[aqw])
    c.op("dve", lambda e: e.tensor_scalar(out=mqw[:, :], in0=mqw[:, :], scalar1=1.0 / 16.0, scalar2=None, op0=ALU.mult), reads=[mqw], writes=[mqw])

    xs_ring = c.ring("xs", 3, [128, D], F32)
    junk_ring = c.ring("junk", 2, [128, D], BF16)
    xn_ring = c.ring("xn", 2, [128, D], BF16)
    ss_ring = c.ring("ss", 4, [128, 1], F32)
    rstd_ring = c.ring("rstd", 4, [128, 1], F32)
    wst_ring = c.ring("wst", 2, [128, D], F32)
    cast_rr = [0]

    def wcast(dst_ap, src_ap, ncols, col_ap=None, mul=None, dstbuf=None):
        st = wst_ring.next()
        c.dma("sp", lambda e: e.dma_start(out=st[:, 0:ncols], in_=src_ap), owner=st, writes=[st])
        eng = ("dve", "pool")[cast_rr[0] % 2]
        cast_rr[0] += 1
        rd = [st]
        if col_ap is not None:
            rd.append(col_ap[0])
        if col_ap is None and mul is None:
            c.op(eng, lambda e: e.tensor_copy(out=dst_ap, in_=st[:, 0:ncols]), reads=rd, writes=[dstbuf], merge_w=True)
        elif col_ap is None:
            c.op(eng, lambda e: e.tensor_scalar(out=dst_ap, in0=st[:, 0:ncols], scalar1=float(mul), scalar2=1.0, op0=ALU.mult, op1=ALU.mult), reads=rd, writes=[dstbuf], merge_w=True)
        elif mul is None:
            c.op(eng, lambda e: e.tensor_scalar(out=dst_ap, in0=st[:, 0:ncols], scalar1=col_ap[1], scalar2=1.0, op0=ALU.mult, op1=ALU.mult), reads=rd, writes=[dstbuf], merge_w=True)
        else:
            c.op(eng, lambda e: e.tensor_scalar(out=dst_ap, in0=st[:, 0:ncols], scalar1=col_ap[1], scalar2=float(mul), op0=ALU.mult, op1=ALU.mult), reads=rd, writes=[dstbuf], merge_w=True)

    def norm_rstd(xbuf, xap):
        jk = junk_ring.next()
        ss = ss_ring.next()
        rs = rstd_ring.next()
        c.op("act", lambda e: e.activation(out=jk[:, :], in_=xap, func=AF.Square, accum_out=ss[:, 0:1]), reads=[xbuf], writes=[jk, ss])
        c.op("act", lambda e: e.activation(out=rs[:, :], in_=ss[:, :], func=AF.Ln, scale=1.0 / D, bias=EPS), reads=[ss], writes=[rs])
        c.op("act", lambda e: e.activation(out=rs[:, :], in_=rs[:, :], func=AF.Exp, scale=-0.5), reads=[rs], writes=[rs])
        return rs

    def norm_T(xbuf, xap, ptb, dst_buf, dst_ap, eng_scale="dve"):
        rs = norm_rstd(xbuf, xap)
        xn = xn_ring.next()
        c.op(eng_scale, lambda e: e.tensor_scalar(out=xn[:, :], in0=xap, scalar1=rs[:, 0:1], scalar2=None, op0=ALU.mult), reads=[xbuf, rs], writes=[xn])
        pv = pbf(ptb)
        for kc in range(KC):
            c.op("pe", lambda e: e.transpose(out=pv[:, kc * 128:(kc + 1) * 128], in_=xn[:, kc * 128:(kc + 1) * 128], identity=ident[:, :]), reads=[xn, ident], writes=[ptb])
        c.op("act", lambda e: e.activation(out=dst_ap, in_=pv[:, 0:1024].rearrange("p (k t) -> p k t", k=8), func=AF.Identity), reads=[ptb], writes=[dst_buf], merge_w=True)
        return rs

    def load_x(src, row0):
        xs = xs_ring.next()
        c.dma("sp", lambda e: e.dma_start(out=xs[:, :], in_=src[row0:row0 + 128, :]), owner=xs, writes=[xs])
        return xs

    def stop_here(label):
        if cfg.stop == label:
            raise _Stop()

    c.push()
    STb = c.sb("STb", [128, NT, 512], BF16)
    lg = c.sb("lg", [128, 8])
    c.op("act", lambda e: e.activation(out=lg[:, :], in_=decb[:, :], func=AF.Exp, scale=-1.0), reads=[decb], writes=[lg])
    c.op("act", lambda e: e.activation(out=lg[:, :], in_=lg[:, :], func=AF.Ln, bias=1.0), reads=[lg], writes=[lg])
    c.op("dve", lambda e: e.tensor_scalar(out=lg[:, :], in0=lg[:, :], scalar1=-1.0, scalar2=None, op0=ALU.mult), reads=[lg], writes=[lg])
    lgsel = c.sb("lgsel", [128, 4])
    c.op("dve", lambda e: e.tensor_copy(out=lgsel[0:64, :], in_=lg[0:64, 0:4]), reads=[lg], writes=[lgsel], merge_w=True)
    c.op("dve", lambda e: e.tensor_copy(out=lgsel[64:128, :], in_=lg[64:128, 4:8]), reads=[lg], writes=[lgsel], merge_w=True)
    cdec = c.sb("cdec", [128, 4])
    c.op("act", lambda e: e.activation(out=cdec[:, :], in_=lgsel[:, :], func=AF.Exp, scale=128.0), reads=[lgsel], writes=[cdec])

    DT = c.sb("DT", [128, 4, 128])
    kqp = c.sb("kqp", [128, 128])
    kqn = c.sb("kqn", [128, 128])
    mle = c.sb("mle", [128, 128])
    mgt = c.sb("mgt", [128, 128])
    c.op("dve", lambda e: e.tensor_scalar(out=kqp[:, :], in0=kq[:, :], scalar1=0.0, scalar2=None, op0=ALU.max), reads=[kq], writes=[kqp])
    c.op("dve", lambda e: e.tensor_scalar(out=kqn[:, :], in0=kq[:, :], scalar1=-1.0, scalar2=0.0, op0=ALU.mult, op1=ALU.max), reads=[kq], writes=[kqn])
    c.op("dve", lambda e: e.tensor_scalar(out=mle[:, :], in0=kq[:, :], scalar1=0.0, scalar2=None, op0=ALU.is_le), reads=[kq], writes=[mle])
    c.op("dve", lambda e: e.tensor_scalar(out=mgt[:, :], in0=kq[:, :], scalar1=0.0, scalar2=None, op0=ALU.is_gt), reads=[kq], writes=[mgt])
    for h in range(4):
        c.op("act", lambda e: e.activation(out=tmpA[:, :], in_=kqn[:, :], func=AF.Exp, scale=lg[:, h:h + 1]), reads=[kqn, lg], writes=[tmpA])
        c.op("act", lambda e: e.activation(out=tmpB[:, :], in_=kqp[:, :], func=AF.Exp, scale=lg[:, 4 + h:5 + h]), reads=[kqp, lg], writes=[tmpB])
        c.op("dve", lambda e: e.tensor_tensor(out=tmpA[:, :], in0=tmpA[:, :], in1=mle[:, :], op=ALU.mult), reads=[tmpA, mle], writes=[tmpA])
        c.op("dve", lambda e: e.tensor_tensor(out=tmpB[:, :], in0=tmpB[:, :], in1=mgt[:, :], op=ALU.mult), reads=[tmpB, mgt], writes=[tmpB])
        c.op("dve", lambda e: e.tensor_tensor(out=DT[:, h, :], in0=tmpA[:, :], in1=tmpB[:, :], op=ALU.add), reads=[tmpA, tmpB], writes=[DT], merge_w=True)

    expo_i = c.sb("expo_i", [128, 128], I32)
    expo = c.sb("expo", [128, 128])
    DEC = c.sb("DEC", [128, 4, 128])
    c.op("pool", lambda e: e.iota(expo_i[0:64, :], pattern=[[1, 128]], base=1, channel_multiplier=0), writes=[expo_i], merge_w=True)
    c.op("pool", lambda e: e.iota(expo_i[64:128, :], pattern=[[-1, 128]], base=128, channel_multiplier=0), writes=[expo_i], merge_w=True)
    c.op("dve", lambda e: e.tensor_copy(out=expo[:, :], in_=expo_i[:, :]), reads=[expo_i], writes=[expo])
    for h in range(4):
        c.op("act", lambda e: e.activation(out=DEC[:, h, :], in_=expo[:, :], func=AF.Exp, scale=lgsel[:, h:h + 1]), reads=[expo, lgsel], writes=[DEC], merge_w=True)

    pcol_i = c.sb("pcol_i", [128, 2], I32)
    pcol = c.sb("pcol", [128, 2])
    c.op("pool", lambda e: e.iota(pcol_i[:, 0:1], pattern=[[0, 1]], base=127, channel_multiplier=-1), writes=[pcol_i], merge_w=True)
    c.op("pool", lambda e: e.iota(pcol_i[:, 1:2], pattern=[[0, 1]], base=0, channel_multiplier=1), writes=[pcol_i], merge_w=True)
    c.op("dve", lambda e: e.tensor_copy(out=pcol[:, :], in_=pcol_i[:, :]), reads=[pcol_i], writes=[pcol])
    kdec = c.sb("kdec", [128, 4, 2])
    for h in range(4):
        c.op("act", lambda e: e.activation(out=kdec[:, h, 0:1], in_=pcol[:, 0:1], func=AF.Exp, scale=lg[:, h:h + 1]), reads=[pcol, lg], writes=[kdec], merge_w=True)
        c.op("act", lambda e: e.activation(out=kdec[:, h, 1:2], in_=pcol[:, 1:2], func=AF.Exp, scale=lg[:, 4 + h:5 + h]), reads=[pcol, lg], writes=[kdec], merge_w=True)

    CFB = c.sb("CFB", [128, NF, 4, 2])
    dfd = c.sb("dfd", [128, NF])
    dbd = c.sb("dbd", [128, NF])
    mfd = c.sb("mfd", [128, NF])
    mbd = c.sb("mbd", [128, NF])
    tmpF = c.sb("tmpF", [128, NF])
    c.op("dve", lambda e: e.tensor_scalar(out=dfd[:, :], in0=posf_s[:, :], scalar1=-1.0, scalar2=t01_s[:, 0:1], op0=ALU.mult, op1=ALU.add), reads=[posf_s, t01_s], writes=[dfd])
    c.op("dve", lambda e: e.tensor_scalar(out=dfd[:, :], in0=dfd[:, :], scalar1=-1.0, scalar2=None, op0=ALU.add), reads=[dfd], writes=[dfd])
    c.op("dve", lambda e: e.tensor_scalar(out=dbd[:, :], in0=posf_s[:, :], scalar1=t01_s[:, 1:2], scalar2=None, op0=ALU.subtract), reads=[posf_s, t01_s], writes=[dbd])
    c.op("dve", lambda e: e.tensor_scalar(out=mfd[:, :], in0=dfd[:, :], scalar1=0.0, scalar2=None, op0=ALU.is_ge), reads=[dfd], writes=[mfd])
    c.op("dve", lambda e: e.tensor_scalar(out=mbd[:, :], in0=dbd[:, :], scalar1=0.0, scalar2=None, op0=ALU.is_ge), reads=[dbd], writes=[mbd])
    c.op("dve", lambda e: e.tensor_scalar(out=dfd[:, :], in0=dfd[:, :], scalar1=0.0, scalar2=None, op0=ALU.max), reads=[dfd], writes=[dfd])
    c.op("dve", lambda e: e.tensor_scalar(out=dbd[:, :], in0=dbd[:, :], scalar1=0.0, scalar2=None, op0=ALU.max), reads=[dbd], writes=[dbd])
    for h in range(4):
        c.op("act", lambda e: e.activation(out=tmpF[:, :], in_=dfd[:, :], func=AF.Exp, scale=lg[:, h:h + 1]), reads=[dfd, lg], writes=[tmpF])
        c.op("dve", lambda e: e.tensor_tensor(out=CFB[:, :, h, 0], in0=tmpF[:, :], in1=mfd[:, :], op=ALU.mult), reads=[tmpF, mfd], writes=[CFB], merge_w=True)
        c.op("act", lambda e: e.activation(out=tmpF[:, :], in_=dbd[:, :], func=AF.Exp, scale=lg[:, 4 + h:5 + h]), reads=[dbd, lg], writes=[tmpF])
        c.op("dve", lambda e: e.tensor_tensor(out=CFB[:, :, h, 1], in0=tmpF[:, :], in1=mbd[:, :], op=ALU.mult), reads=[tmpF, mbd], writes=[CFB], merge_w=True)

    c.push()
    Wf = c.sb("Wf", [128, 8, 768], BF16)
    for kc in range(KC):
        wcast(Wf[:, kc, 0:256], w_in[kc * 128:(kc + 1) * 128, 1792:2048], 256, col_ap=(nwmix, nwmix[:, kc:kc + 1]), mul=0.125, dstbuf=Wf)
        wcast(Wf[:, kc, 256:768], w_in[kc * 128:(kc + 1) * 128, 2048:2560], 512, col_ap=(nwmix, nwmix[:, kc:kc + 1]), dstbuf=Wf)
    hT_ring = c.ring("hTt", 2, [128, 8, 128], BF16)
    kfb_ring = c.ring("kfb", 2, [128, 4, 128], BF16)
    vb_ring = c.ring("vb", 2, [128, 512], BF16)
    CKV = c.sb("CKV", [128, NT, 512], F32)
    Sin = c.sb("Sin", [128, 512], F32)
    PT = [P[0], P[1]]
    PA = [P[2], P[3]]
    PB = [P[4], P[5]]
    PS = P[6]
    PC = [P[6], P[7]]
    c.op("pe", lambda e: e.matmul(PS[:, 0:512], lhsT=zeros_bf[:, :], rhs=zrow[:, :], start=True, stop=False), reads=[zeros_bf, zrow], writes=[PS])
    for it in range(NF + NT):
        far = it < NF
        if far:
            xs = load_x(xf, it * 128)
            coef = CFB[:, it, :, :]
            coefb = CFB
        else:
            n = it - NF
            xs = load_x(xa, (8 + n) * 128)
            coef = kdec[:, :, :]
            coefb = kdec
        if it == NF:
            c.op("pe", lambda e: e.matmul(PS[:, 0:512], lhsT=zeros_bf[:, :], rhs=zrow[:, :], start=False, stop=True), reads=[zeros_bf, zrow], writes=[PS])
            c.op("dve", lambda e: e.tensor_copy(out=Sin[:, :], in_=PS[:, 0:512]), reads=[PS], writes=[Sin])
        hT = hT_ring.next()
        norm_T(xs, xs[:, :], PT[it % 2], hT, hT[:, :, :])
        pa, pb = PA[it % 2], PB[it % 2]
        for kc in range(KC):
            c.op("pe", lambda e: e.matmul(pa[:, 0:256], lhsT=hT[:, kc, :], rhs=Wf[:, kc, 0:256], start=(kc == 0), stop=(kc == KC - 1)), reads=[hT, Wf], writes=[pa])
        for kc in range(KC):
            c.op("pe", lambda e: e.matmul(pb[:, 0:512], lhsT=hT[:, kc, :], rhs=Wf[:, kc, 256:768], start=(kc == 0), stop=(kc == KC - 1)), reads=[hT, Wf], writes=[pb])
        kfb = kfb_ring.next()
        vb = vb_ring.next()
        kview = pa[:, 0:256].rearrange("p (h d) -> p h d", h=4)
        c.op("dve", lambda e: e.tensor_tensor(out=kfb[:, :, 0:64], in0=kview, in1=coef[:, :, 0:1].to_broadcast([128, 4, 64]), op=ALU.mult), reads=[pa, coefb], writes=[kfb], merge_w=True)
        c.op("dve", lambda e: e.tensor_tensor(out=kfb[:, :, 64:128], in0=kview, in1=coef[:, :, 1:2].to_broadcast([128, 4, 64]), op=ALU.mult), reads=[pa, coefb], writes=[kfb], merge_w=True)
        c.op("act", lambda e: e.activation(out=vb[:, :], in_=pb[:, 0:512], func=AF.Identity), reads=[pb], writes=[vb])
        if far:
            for h in range(4):
                c.op("pe", lambda e: e.matmul(PS[:, h * 128:(h + 1) * 128], lhsT=kfb[:, h, :], rhs=vb[:, h * 128:(h + 1) * 128], start=False, stop=False), reads=[kfb, vb], writes=[PS])
        else:
            pc = PC[n % 2]
            for h in range(4):
                c.op("pe", lambda e: e.matmul(pc[:, h * 128:(h + 1) * 128], lhsT=kfb[:, h, :], rhs=vb[:, h * 128:(h + 1) * 128], start=True, stop=True), reads=[kfb, vb], writes=[pc])
            c.op("dve", lambda e: e.tensor_copy(out=CKV[:, n, :], in_=pc[:, 0:512]), reads=[pc], writes=[CKV], merge_w=True)

    Rf = c.sb("Rf", [128, 512], F32)
    c.op("dve", lambda e: e.tensor_copy(out=Rf[:, :], in_=Sin[:, :]), reads=[Sin], writes=[Rf])
    Rlo = Buf("Rlo")
    Rhi = Buf("Rhi")
    Rlo.w = dict(Rf.w)
    Rhi.w = dict(Rf.w)
    for s in range(NT):
        nf_, nb_ = s, NT - 1 - s
        c.op("act", lambda e: e.activation(out=STb[0:64, nf_, :], in_=Rf[0:64, :], func=AF.Identity), reads=[Rlo], writes=[STb], merge_w=True)
        c.op("pool", lambda e: e.tensor_copy(out=STb[64:128, nb_, :], in_=Rf[64:128, :]), reads=[Rhi], writes=[STb], merge_w=True)
        for h in range(4):
            sl = slice(h * 128, (h + 1) * 128)
            c.op("dve", lambda e: e.scalar_tensor_tensor(out=Rf[0:64, sl], in0=Rf[0:64, sl], scalar=cdec[0:64, h:h + 1], in1=CKV[0:64, nf_, sl], op0=ALU.mult, op1=ALU.add), reads=[Rlo, cdec, CKV], writes=[Rlo])
            c.op("dve", lambda e: e.scalar_tensor_tensor(out=Rf[64:128, sl], in0=Rf[64:128, sl], scalar=cdec[64:128, h:h + 1], in1=CKV[64:128, nb_, sl], op0=ALU.mult, op1=ALU.add), reads=[Rhi, cdec, CKV], writes=[Rhi])
    phases["F"] = (c.ninst, c.nwait)

    if cfg.debug:
        c.dma("sp", lambda e: e.dma_start(out=DBG_d[:, 0:512], in_=Sin[:, :]), owner=Sin, reads=[Sin], writes=[DBG_d], merge_w=True)
        c.dma("sp", lambda e: e.dma_start(out=DBG_d[:, 512:1024], in_=CKV[:, 0, :]), owner=CKV, reads=[CKV], writes=[DBG_d], merge_w=True)
    if cfg.stop == "F":
        c.finish()
        c.pop_all()
        return nc, c, phases
    c.pop()

    NBO = NT // 4
    c.push()
    Wr = c.sb("Wr", [128, 8, 1792], BF16)
    for kc in range(KC):
        rows = slice(kc * 128, (kc + 1) * 128)
        st = wst_ring.next()
        c.dma("sp", lambda e: e.dma_start(out=st[:, 0:256], in_=w_in[rows, 1536:1792]), owner=st, writes=[st])
        dup = Wr[:, kc, 0:512].rearrange("p (h t d) -> p h t d", h=4, t=2)
        for two in range(2):
            c.op(("dve", "pool")[two], lambda e: e.tensor_scalar(out=dup[:, :, two, :], in0=st[:, 0:256].rearrange("p (h d) -> p h d", h=4), scalar1=nwmix[:, kc:kc + 1], scalar2=1.0, op0=ALU.mult, op1=ALU.mult), reads=[st, nwmix], writes=[Wr], merge_w=True)
        wcast(Wr[:, kc, 512:768], w_in[rows, 1792:2048], 256, col_ap=(nwmix, nwmix[:, kc:kc + 1]), mul=0.125, dstbuf=Wr)
        wcast(Wr[:, kc, 768:1280], w_in[rows, 2048:2560], 512, col_ap=(nwmix, nwmix[:, kc:kc + 1]), dstbuf=Wr)
        wcast(Wr[:, kc, 1280:1792], w_in[rows, 2560:3072], 512, col_ap=(nwmix, nwmix[:, kc:kc + 1]), dstbuf=Wr)
    GW = c.sb("GW", [128, 512], F32)
    c.dma("sp", lambda e: e.dma_start(out=GW[:, :], in_=gn_w[0:1, :].to_broadcast([128, 512])), owner=GW, writes=[GW])
    stop_here("R0")
    hTb_ring = c.ring("hTb", 2, [128, 8, 512], BF16)
    qfb_ring = c.ring("qfb", 2, [128, 4, 512], BF16)
    qpl_ring = c.ring("qpl", 2, [128, 4, 512], BF16)
    ktr_ring = c.ring("ktr", 2, [128, 2, 512], BF16)
    vb2_ring = c.ring("vb2", 2, [128, 512], BF16)
    gs_ring = c.ring("gs", 2, [128, 512], F32)
    gg_ring = c.ring("gg", 2, [128, 512], F32)
    pts_ring = c.ring("pts", 2, [128, 4, 128], BF16)
    ysq_ring = c.ring("ysq", 2, [128, 512], F32)
    yn_ring = c.ring("yn", 2, [128, 512], F32)
    ro_ring = c.ring("ro", 2, [128, 512], BF16)
    rts_ring = c.ring("rts", 2, [128, 4, 512], BF16)
    st_ring = c.ring("gst", 16, [128, 4], F32)
    pring = Ring([P[2], P[3], P[4]])
    PAt2, PY, PRT = [P[6], P[7]], P[5], P[0]
    for b in range(NBO):
        hTb = hTb_ring.next()
        for ti in range(4):
            xs = load_x(xa, (8 + 4 * b + ti) * 128)
            norm_T(xs, xs[:, :], PT[ti % 2], hTb, hTb[:, :, ti * 128:(ti + 1) * 128])
        stop_here("R1")
        qfb, qpl, ktr = qfb_ring.next(), qpl_ring.next(), ktr_ring.next()
        for h in range(4):
            hb = 64 * (h % 2)
            pq = pring.next()
            for kc in range(KC):
                c.op("pe", lambda e: e.matmul(pq[:, 0:512], lhsT=Wr[:, kc, h * 128:(h + 1) * 128], rhs=hTb[:, kc, :], start=(kc == 0), stop=(kc == KC - 1)), reads=[Wr, hTb], writes=[pq])
            stop_here("R1a")
            c.op("dve", lambda e: e.tensor_tensor(out=qfb[:, h, :].rearrange("p (a i) -> p a i", a=4), in0=pq[:, 0:512].rearrange("p (a i) -> p a i", a=4), in1=DEC[:, h:h + 1, :].to_broadcast([128, 4, 128]), op=ALU.mult), reads=[pq, DEC], writes=[qfb], merge_w=True)
            stop_here("R1b")
            c.op("dve", lambda e: e.tensor_copy(out=qpl[hb:hb + 64, h, :], in_=pq[hb:hb + 64, 0:512]), reads=[pq], writes=[qpl], merge_w=True)
            stop_here("R1c")
        for pr in range(2):
            pk = pring.next()
            for kc in range(KC):
                c.op("pe", lambda e: e.matmul(pk[:, 0:512], lhsT=Wr[:, kc, 512 + pr * 128:512 + (pr + 1) * 128], rhs=hTb[:, kc, :], start=(kc == 0), stop=(kc == KC - 1)), reads=[Wr, hTb], writes=[pk])
            c.op("act", lambda e: e.activation(out=ktr[:, pr, :], in_=pk[:, 0:512], func=AF.Identity), reads=[pk], writes=[ktr], merge_w=True)
        stop_here("R2")
        rts = rts_ring.next()
        for ti in range(4):
            n = 4 * b + ti
            tsl = slice(ti * 128, (ti + 1) * 128)
            pv = pring.next()
            for kc in range(KC):
                c.op("pe", lambda e: e.matmul(pv[:, 0:512], lhsT=hTb[:, kc, tsl], rhs=Wr[:, kc, 768:1280], start=(kc == 0), stop=(kc == KC - 1)), reads=[Wr, hTb], writes=[pv])
            vb2 = vb2_ring.next()
            c.op("act", lambda e: e.activation(out=vb2[:, :], in_=pv[:, 0:512], func=AF.Identity), reads=[pv], writes=[vb2])
            pg = pring.next()
            for kc in range(KC):
                c.op("pe", lambda e: e.matmul(pg[:, 0:512], lhsT=hTb[:, kc, tsl], rhs=Wr[:, kc, 1280:1792], start=(kc == 0), stop=(kc == KC - 1)), reads=[Wr, hTb], writes=[pg])
            gs = gs_ring.next()
            c.op("act", lambda e: e.activation(out=gs[:, :], in_=pg[:, 0:512], func=AF.Exp, scale=-1.0), reads=[pg], writes=[gs])
            c.op("act", lambda e: e.activation(out=gs[:, :], in_=gs[:, :], func=AF.Ln, bias=1.0), reads=[gs], writes=[gs])
            c.op("act", lambda e: e.activation(out=gs[:, :], in_=gs[:, :], func=AF.Exp, scale=-1.0), reads=[gs], writes=[gs])
            c.op("pool", lambda e: e.tensor_tensor(out=gs[:, :], in0=gs[:, :], in1=GW[:, :], op=ALU.mult), reads=[gs, GW], writes=[gs])
            gg = gg_ring.next()
            c.op("dve", lambda e: e.tensor_tensor(out=gg[:, :], in0=pg[:, 0:512], in1=gs[:, :], op=ALU.mult), reads=[pg, gs], writes=[gg])
            stop_here("R3")
            for h in range(4):
                hb = 64 * (h % 2)
                pat = PAt2[h % 2]
                c.op("pe", lambda e: e.matmul(pat[:, (h // 2) * 128:(h // 2 + 1) * 128], lhsT=ktr[hb:hb + 64, h // 2, tsl], rhs=qpl[hb:hb + 64, h, tsl], start=True, stop=True), reads=[ktr, qpl], writes=[pat])
            stop_here("R3a")
            pts = pts_ring.next()
            ptv = pts[:, :, :].rearrange("p (a two) i -> p a two i", two=2)
            dtv = DT[:, :, :].rearrange("p (a two) i -> p a two i", two=2)
            for par in range(2):
                c.op("dve", lambda e: e.tensor_tensor(out=ptv[:, :, par, :], in0=PAt2[par][:, 0:256].rearrange("p (a i) -> p a i", a=2), in1=dtv[:, :, par, :], op=ALU.mult), reads=[PAt2[par], DT], writes=[pts], merge_w=(par == 1))
            stop_here("R3b")
            for h in range(4):
                hs = slice(h * 128, (h + 1) * 128)
                c.op("pe", lambda e: e.matmul(PY[:, hs], lhsT=pts[:, h, :], rhs=vb2[:, hs], start=True, stop=False), reads=[pts, vb2], writes=[PY])
                c.op("pe", lambda e: e.matmul(PY[:, hs], lhsT=qfb[:, h, tsl], rhs=STb[:, n, hs], start=False, stop=True), reads=[qfb, STb], writes=[PY])
            stop_here("R4")
            s1, s2, mu, msq, var, sd, nmr = [st_ring.next() for _ in range(7)]
            pyv = PY[:, 0:512].rearrange("p (h e) -> p h e", h=4)
            c.op("dve", lambda e: e.tensor_reduce(out=s1[:, :], in_=pyv, axis=AX.X, op=ALU.add), reads=[PY], writes=[s1])
            stop_here("R4a")
            ysq = ysq_ring.next()
            c.op("act", lambda e: e.activation(out=ysq[:, :], in_=PY[:, 0:512], func=AF.Square), reads=[PY], writes=[ysq])
            stop_here("R4a2")
            c.op("dve", lambda e: e.tensor_reduce(out=s2[:, :], in_=ysq[:, :].rearrange("p (h e) -> p h e", h=4), axis=AX.X, op=ALU.add), reads=[ysq], writes=[s2])
            stop_here("R4b")
            c.op("dve", lambda e: e.tensor_scalar(out=mu[:, :], in0=s1[:, :], scalar1=1.0 / 128.0, scalar2=None, op0=ALU.mult), reads=[s1], writes=[mu])
            c.op("dve", lambda e: e.tensor_tensor(out=msq[:, :], in0=mu[:, :], in1=mu[:, :], op=ALU.mult), reads=[mu], writes=[msq])
            c.op("dve", lambda e: e.scalar_tensor_tensor(out=var[:, :], in0=s2[:, :], scalar=1.0 / 128.0, in1=msq[:, :], op0=ALU.mult, op1=ALU.subtract), reads=[s2, msq], writes=[var])
            c.op("act", lambda e: e.activation(out=sd[:, :], in_=var[:, :], func=AF.Ln, bias=GN_EPS), reads=[var], writes=[sd])
            c.op("act", lambda e: e.activation(out=sd[:, :], in_=sd[:, :], func=AF.Exp, scale=-0.5), reads=[sd], writes=[sd])
            c.op("dve", lambda e: e.scalar_tensor_tensor(out=nmr[:, :], in0=mu[:, :], scalar=-1.0, in1=sd[:, :], op0=ALU.mult, op1=ALU.mult), reads=[mu, sd], writes=[nmr])
            stop_here("R4c")
            yn = yn_ring.next()
            stop_here("R4c2")
            for h in range(4):
                hs = slice(h * 128, (h + 1) * 128)
                c.op("act", lambda e: e.activation(out=yn[:, hs], in_=PY[:, hs], func=AF.Identity, scale=sd[:, h:h + 1], bias=nmr[:, h:h + 1]), reads=[PY, sd, nmr], writes=[yn], merge_w=True)
            ro = ro_ring.next()
            c.op("dve", lambda e: e.tensor_tensor(out=ro[:, :], in0=yn[:, :], in1=gg[:, :], op=ALU.mult), reads=[yn, gg], writes=[ro])
            stop_here("R5")
            prt = pbf(PRT)
            for h in range(4):
                hs = slice(h * 128, (h + 1) * 128)
                c.op("pe", lambda e: e.transpose(out=prt[:, hs], in_=ro[:, hs], identity=ident[:, :]), reads=[ro, ident], writes=[PRT])
            c.op("act", lambda e: e.activation(out=rts[:, :, tsl], in_=prt[:, 0:512].rearrange("p (h t) -> p h t", h=4), func=AF.Identity), reads=[PRT], writes=[rts], merge_w=True)
        stop_here("R6")
        c.dma("pool", lambda e: e.dma_start(out=MIX_d.t[4:8, :, b * 512:(b + 1) * 512].rearrange("h p t -> p h t"), in_=rts[:, :, :]), owner=rts, reads=[rts], writes=[MIX_d], merge_w=True)
        stop_here("R7")
    phases["R"] = (c.ninst, c.nwait)
    if cfg.stop == "R":
        c.finish([MIX_d])
        c.pop_all()
        return nc, c, phases
    c.pop()
    c.pop()

    c.push()
    Wa = c.sb("Wa", [128, 8, 1536], BF16)
    for kc in range(KC):
        rows = slice(kc * 128, (kc + 1) * 128)
        for part in range(3):
            wcast(Wa[:, kc, part * 512:(part + 1) * 512], w_in[rows, part * 512:(part + 1) * 512], 512, col_ap=(nwmix, nwmix[:, kc:kc + 1]), dstbuf=Wa)
    hTa_ring = c.ring("hTa", 2, [128, 8, 512], BF16)
    sq_ring = c.ring("sqa", 2, [128, 512], BF16)
    ln_ring = c.ring("lna", 2, [128, 512], F32)
    stg_ring = c.ring("stga", 3, [128, 512], BF16)
    pring = Ring([P[2], P[3], P[4]])
    pssr = Ring([P[5], P[6]])
    NBA = NA // 4
    cnt = 0
    for b in range(NBA):
        own = 2 <= b < NBA - 2
        hTb = hTa_ring.next()
        for ti in range(4):
            xs = load_x(xa, (4 * b + ti) * 128)
            norm_T(xs, xs[:, :], PT[ti % 2], hTb, hTb[:, :, ti * 128:(ti + 1) * 128])
        kinds = [("k", pr) for pr in range(4)] + [("v", pr) for pr in range(4)]
        if own:
            kinds += [("q", pr) for pr in range(4)]
        for kind, pr in kinds:
            col0 = {"q": 0, "k": 512, "v": 1024}[kind] + pr * 128
            pq = pring.next()
            for kc in range(KC):
                c.op("pe", lambda e: e.matmul(pq[:, 0:512], lhsT=Wa[:, kc, col0:col0 + 128], rhs=hTb[:, kc, :], start=(kc == 0), stop=(kc == KC - 1)), reads=[Wa, hTb], writes=[pq])
            stg = stg_ring.next()
            if kind == "v":
                eng = ("act", "dve")[cnt % 2]
                cnt += 1
                if eng == "act":
                    c.op("act", lambda e: e.activation(out=stg[:, :], in_=pq[:, 0:512], func=AF.Identity), reads=[pq], writes=[stg])
                else:
                    c.op("dve", lambda e: e.tensor_copy(out=stg[:, :], in_=pq[:, 0:512]), reads=[pq], writes=[stg])
                dst = VT_d.t[pr, :, b * 512:(b + 1) * 512]
                dbuf = VT_d
            else:
                sq = sq_ring.next()
                c.op("act", lambda e: e.activation(out=sq[:, :], in_=pq[:, 0:512], func=AF.Square), reads=[pq], writes=[sq])
                pss = pssr.next()
                c.op("pe", lambda e: e.matmul(pss[:, 0:512], lhsT=blk1[:, :], rhs=sq[:, :], start=True, stop=True), reads=[blk1, sq], writes=[pss])
                ln = ln_ring.next()
                c.op("act", lambda e: e.activation(out=ln[:, :], in_=pss[:, 0:512], func=AF.Ln, scale=1.0 / 64.0, bias=EPS), reads=[pss], writes=[ln])
                c.op("act", lambda e: e.activation(out=ln[:, :], in_=ln[:, :], func=AF.Exp, scale=-0.5), reads=[ln], writes=[ln])
                wcol = aqw if kind == "q" else akw
                c.op("dve", lambda e: e.scalar_tensor_tensor(out=stg[:, :], in0=pq[:, 0:512], scalar=wcol[:, 0:1], in1=ln[:, :], op0=ALU.mult, op1=ALU.mult), reads=[pq, wcol, ln], writes=[stg])
                if kind == "q":
                    dst = QT_d.t[pr, :, (b - 2) * 512:(b - 1) * 512]
                    dbuf = QT_d
                else:
                    dst = KT_d.t[pr, :, b * 512:(b + 1) * 512]
                    dbuf = KT_d
            c.dma("pool", lambda e: e.dma_start(out=dst, in_=stg[:, :]), owner=stg, reads=[stg], writes=[dbuf], merge_w=True)
    phases["A1"] = (c.ninst, c.nwait)
    if cfg.stop == "A1":
        c.finish([QT_d, KT_d, VT_d])
        c.pop_all()
        return nc, c, phases
    c.pop()

    c.push()
    BIAS = c.sb("BIAS", [128, 24, 256], BF16)
    c.op("act", lambda e: e.activation(out=tmpA[:, :], in_=kq[:, :], func=AF.Abs, bias=-64.0), reads=[kq], writes=[tmpA])
    c.op("act", lambda e: e.activation(out=tmpB[:, :], in_=kq[:, :], func=AF.Abs, bias=64.0), reads=[kq], writes=[tmpB])
    c.op("dve", lambda e: e.tensor_scalar(out=tmpC[:, :], in0=kq[:, :], scalar1=0.0, scalar2=None, op0=ALU.is_ge), reads=[kq], writes=[tmpC])
    c.op("dve", lambda e: e.tensor_scalar(out=tmpC[:, :], in0=tmpC[:, :], scalar1=-1.0, scalar2=-NEG, op0=ALU.add, op1=ALU.mult), reads=[tmpC], writes=[tmpC])
    c.op("dve", lambda e: e.tensor_scalar(out=tmpD[:, :], in0=kq[:, :], scalar1=0.0, scalar2=None, op0=ALU.is_le), reads=[kq], writes=[tmpD])
    c.op("dve", lambda e: e.tensor_scalar(out=tmpD[:, :], in0=tmpD[:, :], scalar1=-1.0, scalar2=-NEG, op0=ALU.add, op1=ALU.mult), reads=[tmpD], writes=[tmpD])
    for h in range(8):
        for pi, d in enumerate(PATTERNS):
            sc = -SLOPES[h] * d
            ix = h * 3 + pi
            c.op("dve", lambda e: e.scalar_tensor_tensor(out=BIAS[:, ix, 0:128], in0=tmpA[:, :], scalar=sc, in1=tmpC[:, :], op0=ALU.mult, op1=ALU.add), reads=[tmpA, tmpC], writes=[BIAS], merge_w=True)
            c.op("dve", lambda e: e.scalar_tensor_tensor(out=BIAS[:, ix, 128:256], in0=tmpB[:, :], scalar=sc, in1=tmpD[:, :], op0=ALU.mult, op1=ALU.add), reads=[tmpB, tmpD], writes=[BIAS], merge_w=True)

    valt_f = c.sb("valt_f", [128, cfg.NCHT], F32)
    VALT = c.sb("VALT", [128, cfg.NCHT, 64], BF16)
    c.dma("sp", lambda e: e.dma_start(out=valt_f[:, :], in_=valt_d[:, :]), owner=valt_f, writes=[valt_f])
    c.op("dve", lambda e: e.tensor_copy(out=VALT[:, :, :], in_=valt_f[:, :].unsqueeze(2).to_broadcast([128, cfg.NCHT, 64])), reads=[valt_f], writes=[VALT])
    qts_ring = c.ring("qts", 2, [128, LS], BF16)
    kts_ring = c.ring("kts", 2, [128, NA * 128], BF16)
    vts_ring = c.ring("vts", 2, [128, NA * 128], BF16)
    NUM = c.sb("NUM", [128, LS], F32)
    DEN = c.sb("DEN", [128, LS], F32)
    ATS = c.sb("ATS", [128, LS], BF16)
    es_ring = c.ring("es", 4, [128, 512], BF16)
    vpc_ring = c.ring("vpc", 4, [128, 128], BF16)
    Sring = Ring([P[0], P[1], P[2]])
    ONr = Ring([P[3], P[4]])
    ODr = Ring([P[5], P[6]])
    PVT = P[7]
    for pr in range(4):
        qts, kts, vts = qts_ring.next(), kts_ring.next(), vts_ring.next()
        c.dma("sp", lambda e: e.dma_start(out=qts[:, :], in_=QT_d.t[pr, :, :]), owner=qts, reads=[QT_d], writes=[qts])
        c.dma("sp", lambda e: e.dma_start(out=kts[:, :], in_=KT_d.t[pr, :, :]), owner=kts, reads=[KT_d], writes=[kts])
        c.dma("sp", lambda e: e.dma_start(out=vts[:, :], in_=VT_d.t[pr, :, :]), owner=vts, reads=[VT_d], writes=[vts])
        c.op("pool", lambda e: e.memset(NUM[:, :], 0.0), writes=[NUM])
        c.op("pool", lambda e: e.memset(DEN[:, :], 0.0), writes=[DEN])
        for pi, d in enumerate(PATTERNS):
            nq = cfg.nq[pi]
            nch = cfg.nch[pi]
            for r in range(d):
                vcache = {}

                def kslice(ch):
                    k0 = d * (128 * ch - 64) + r + 1024
                    return slice(k0, k0 + 127 * d + 1, d)

                def getv(ch):
                    if ch not in vcache:
                        vc = vpc_ring.next()
                        pv = pbf(PVT)
                        c.op("pe", lambda e: e.transpose(out=pv[:, 0:128], in_=vts[:, kslice(ch)], identity=ident[:, :]), reads=[vts, ident], writes=[PVT])
                        c.op("dve", lambda e: e.tensor_copy(out=vc[:, :], in_=pv[:, 0:128]), reads=[PVT], writes=[vc])
                        vcache[ch] = vc
                    return vcache[ch]
                for mm in range(nq // 2):
                    m0 = 2 * mm
                    E = []
                    for h2 in range(2):
                        hb = 64 * h2
                        hg = 2 * pr + h2
                        ps = Sring.next()
                        ix = hg * 3 + pi
                        c.op("pe", lambda e: e.matmul(ps[:, 0:512].rearrange("p (a n) -> p a n", a=2), lhsT=ident[:, :], rhs=BIAS[:, ix:ix + 1, :].to_broadcast([128, 2, 256]), start=True, stop=False), reads=[ident, BIAS], writes=[ps])
                        for blk, (tq, ch) in enumerate([(m0, m0), (m0, m0 + 1), (m0 + 1, m0 + 1), (m0 + 1, m0 + 2)]):
                            q0 = d * 128 * tq + r
                            c.op("pe", lambda e: e.matmul(ps[:, blk * 128:(blk + 1) * 128], lhsT=kts[hb:hb + 64, kslice(ch)], rhs=qts[hb:hb + 64, q0:q0 + 127 * d + 1:d], start=False, stop=(blk == 3)), reads=[kts, qts], writes=[ps])
                        es = es_ring.next()
                        c.op("act", lambda e: e.activation(out=es[:, :], in_=ps[:, 0:512], func=AF.Exp), reads=[ps], writes=[es])
                        E.append(es)
                    on, od = ONr.next(), ODr.next()
                    for h2 in range(2):
                        hb = 64 * h2
                        for tqi in range(2):
                            osl = slice(tqi * 128, (tqi + 1) * 128)
                            for half, ch in enumerate((m0 + tqi, m0 + tqi + 1)):
                                blk = 2 * tqi + half
                                vc = getv(ch)
                                c.op("pe", lambda e: e.matmul(on[hb:hb + 64, osl], lhsT=vc[:, hb:hb + 64], rhs=E[h2][:, blk * 128:(blk + 1) * 128], start=(half == 0), stop=(half == 1)), reads=[vc, E[h2]], writes=[on])
                            for half, ch in enumerate((m0 + tqi, m0 + tqi + 1)):
                                blk = 2 * tqi + half
                                vix = cfg.chbase[pi] + r * nch + ch
                                c.op("pe", lambda e: e.matmul(od[hb:hb + 64, osl], lhsT=VALT[:, vix, :], rhs=E[h2][:, blk * 128:(blk + 1) * 128], start=(half == 0), stop=(half == 1)), reads=[VALT, E[h2]], writes=[od])
                    p0 = d * 128 * m0 + r
                    psl = slice(p0, p0 + 255 * d + 1, d)
                    c.op("dve", lambda e: e.tensor_tensor(out=NUM[:, psl], in0=NUM[:, psl], in1=on[:, 0:256], op=ALU.add), reads=[NUM, on], writes=[NUM])
                    c.op("dve", lambda e: e.tensor_tensor(out=DEN[:, psl], in0=DEN[:, psl], in1=od[:, 0:256], op=ALU.add), reads=[DEN, od], writes=[DEN])
        c.op("act", lambda e: e.activation(out=DEN[:, :], in_=DEN[:, :], func=AF.Ln), reads=[DEN], writes=[DEN])
        c.op("act", lambda e: e.activation(out=DEN[:, :], in_=DEN[:, :], func=AF.Exp, scale=-1.0), reads=[DEN], writes=[DEN])
        for q4 in range(LS // 1024):
            qs = slice(q4 * 1024, (q4 + 1) * 1024)
            c.op(("dve", "pool")[q4 % 2], lambda e: e.tensor_tensor(out=ATS[:, qs], in0=NUM[:, qs], in1=DEN[:, qs], op=ALU.mult), reads=[NUM, DEN], writes=[ATS], merge_w=True)
        c.dma("pool", lambda e: e.dma_start(out=MIX_d.t[pr, :, :], in_=ATS[:, :]), owner=ATS, reads=[ATS], writes=[MIX_d], merge_w=True)
    phases["A2"] = (c.ninst, c.nwait)
    if cfg.stop == "A2":
        c.finish([MIX_d])
        c.pop_all()
        return nc, c, phases
    c.pop()

    OHs = c.sb("OHs", [128, NT, 32], F32)
    GTs = c.sb("GTs", [128, NT, 32], F32)
    c.push()
    Wout = c.sb("Wout", [128, 8, 1024], BF16)
    Wmq = c.sb("Wmq", [128, 8, 1024], BF16)
    Wmo = c.sb("Wmo", [128, 8, 1024], BF16)
    for kc in range(KC):
        rows = slice(kc * 128, (kc + 1) * 128)
        wcast(Wout[:, kc, :], w_out[rows, :], 1024, dstbuf=Wout)
        wcast(Wmq[:, kc, :], w_mq[rows, :], 1024, col_ap=(nwmem, nwmem[:, kc:kc + 1]), dstbuf=Wmq)
        wcast(Wmo[:, kc, :], w_mo[rows, :], 1024, dstbuf=Wmo)
    Wrt = c.sb("Wrt", [128, 8, 36], F32)
    c.dma("sp", lambda e: e.dma_start(out=Wrt[:, :, :], in_=w_rt.t.ap().rearrange("(kc p) n -> p kc n", p=128)), owner=Wrt, writes=[Wrt])
    WMOE = c.sb("WMOE", [128, D], F32)
    c.dma("sp", lambda e: e.dma_start(out=WMOE[:, :], in_=nw_moe[0:1, :].to_broadcast([128, D])), owner=WMOE, writes=[WMOE])
    KmT = c.sb("KmT", [128, 8, 256], BF16)
    Vm = c.sb("Vm", [128, 2, 1024], BF16)
    sqm_ring = c.ring("sqm", 2, [128, 512], BF16)
    rsm_ring = c.ring("rsm", 2, [128, 512], F32)
    c.push()
    Wmk = c.sb("Wmk", [128, 8, 2048], BF16)
    for kc in range(KC):
        rows = slice(kc * 128, (kc + 1) * 128)
        for half in range(2):
            wcast(Wmk[:, kc, half * 1024:(half + 1) * 1024], w_mkv[rows, half * 1024:(half + 1) * 1024], 1024, col_ap=(nwmemkv, nwmemkv[:, kc:kc + 1]), dstbuf=Wmk)
    mhT = c.sb("mhT", [128, 8, 256], BF16)
    for mt in range(2):
        xs = load_x(memx, mt * 128)
        norm_T(xs, xs[:, :], PT[mt], mhT, mhT[:, :, mt * 128:(mt + 1) * 128])
    for h in range(4):
        for dc in range(2):
            cidx = 2 * h + dc
            pk = P[2 + dc]
            for kc in range(KC):
                c.op("pe", lambda e: e.matmul(pk[:, 0:256], lhsT=Wmk[:, kc, cidx * 128:(cidx + 1) * 128], rhs=mhT[:, kc, :], start=(kc == 0), stop=(kc == KC - 1)), reads=[Wmk, mhT], writes=[pk])
            sqm = sqm_ring.next()
            c.op("act", lambda e: e.activation(out=sqm[:, 0:256], in_=pk[:, 0:256], func=AF.Square), reads=[pk], writes=[sqm])
            c.op("pe", lambda e: e.matmul(P[4][:, 0:256], lhsT=ones_bf[:, :], rhs=sqm[:, 0:256], start=(dc == 0), stop=(dc == 1)), reads=[ones_bf, sqm], writes=[P[4]])
        rsm = rsm_ring.next()
        c.op("act", lambda e: e.activation(out=rsm[:, 0:256], in_=P[4][:, 0:256], func=AF.Ln, scale=1.0 / 256.0, bias=EPS), reads=[P[4]], writes=[rsm])
        c.op("act", lambda e: e.activation(out=rsm[:, 0:256], in_=rsm[:, 0:256], func=AF.Exp, scale=-0.5), reads=[rsm], writes=[rsm])
        for dc in range(2):
            c.op("dve", lambda e: e.scalar_tensor_tensor(out=KmT[:, 2 * h + dc, :], in0=P[2 + dc][:, 0:256], scalar=mkw[:, dc:dc + 1], in1=rsm[:, 0:256], op0=ALU.mult, op1=ALU.mult), reads=[P[2 + dc], mkw, rsm], writes=[KmT], merge_w=True)
    for mc in range(2):
        for half in range(2):
            pv = P[5 + half]
            for kc in range(KC):
                c.op("pe", lambda e: e.matmul(pv[:, 0:512], lhsT=mhT[:, kc, mc * 128:(mc + 1) * 128], rhs=Wmk[:, kc, 1024 + half * 512:1024 + (half + 1) * 512], start=(kc == 0), stop=(kc == KC - 1)), reads=[Wmk, mhT], writes=[pv])
            c.op("act", lambda e: e.activation(out=Vm[:, mc, half * 512:(half + 1) * 512], in_=pv[:, 0:512], func=AF.Identity), reads=[pv], writes=[Vm], merge_w=True)
    if cfg.debug:
        dbk = c.sb("dbk", [128, 2048], F32)
        c.op("dve", lambda e: e.tensor_copy(out=dbk[:, :], in_=KmT[:, :, :].rearrange("p a b -> p (a b)")), reads=[KmT], writes=[dbk])
        c.dma("sp", lambda e: e.dma_start(out=DBG_d[:, 1024:3072], in_=dbk[:, :]), owner=dbk, reads=[dbk], writes=[DBG_d], merge_w=True)
    c.pop()
    mixb = c.sb("mixb", [128, 8, 512], BF16)
    x1b = c.sb("x1b", [128, 4, D], F32)
    h2T = c.sb("h2T", [128, 8, 512], BF16)
    QmT = c.sb("QmT", [128, 8, 512], BF16)
    OmT = c.sb("OmT", [128, 8, 512], BF16)
    et_ring = c.ring("et", 2, [128, 2, 512], BF16)
    rd_ring = c.ring("rd", 2, [128, 512], F32)
    x2_ring = c.ring("x2t", 2, [128, D], F32)
    h3_ring = c.ring("h3", 2, [128, D], F32)
    h3b_ring = c.ring("h3b", 2, [128, D], BF16)
    h3T_ring = c.ring("h3T", 2, [128, 8, 128], F32)
    sm_ring = c.ring("sm", 32, [128, 36], F32)
    for b in range(NBO):
        c.dma("sp", lambda e: e.dma_start(out=mixb[:, :, :], in_=MIX_d.t[:, :, b * 512:(b + 1) * 512].rearrange("c p t -> p c t")), owner=mixb, reads=[MIX_d], writes=[mixb])
        for ti in range(4):
            tsl = slice(ti * 128, (ti + 1) * 128)
            xs = load_x(xa, (8 + 4 * b + ti) * 128)
            for half in range(2):
                px = P[5 + half]
                hs = slice(half * 512, (half + 1) * 512)
                for kc in range(KC):
                    c.op("pe", lambda e: e.matmul(px[:, 0:512], lhsT=mixb[:, kc, tsl], rhs=Wout[:, kc, hs], start=(kc == 0), stop=(kc == KC - 1)), reads=[mixb, Wout], writes=[px])
                c.op("dve", lambda e: e.tensor_tensor(out=x1b[:, ti, hs], in0=xs[:, hs], in1=px[:, 0:512], op=ALU.add), reads=[xs, px], writes=[x1b], merge_w=True)
            norm_T(x1b, x1b[:, ti, :], PT[ti % 2], h2T, h2T[:, :, tsl])
        for h in range(4):
            for dc in range(2):
                cidx = 2 * h + dc
                pq = P[2 + dc]
                for kc in range(KC):
                    c.op("pe", lambda e: e.matmul(pq[:, 0:512], lhsT=Wmq[:, kc, cidx * 128:(cidx + 1) * 128], rhs=h2T[:, kc, :], start=(kc == 0), stop=(kc == KC - 1)), reads=[Wmq, h2T], writes=[pq])
                sqm = sqm_ring.next()
                c.op("act", lambda e: e.activation(out=sqm[:, :], in_=pq[:, 0:512], func=AF.Square), reads=[pq], writes=[sqm])
                c.op("pe", lambda e: e.matmul(P[4][:, 0:512], lhsT=ones_bf[:, :], rhs=sqm[:, :], start=(dc == 0), stop=(dc == 1)), reads=[ones_bf, sqm], writes=[P[4]])
            rsm = rsm_ring.next()
            c.op("act", lambda e: e.activation(out=rsm[:, :], in_=P[4][:, 0:512], func=AF.Ln, scale=1.0 / 256.0, bias=EPS), reads=[P[4]], writes=[rsm])
            c.op("act", lambda e: e.activation(out=rsm[:, :], in_=rsm[:, :], func=AF.Exp, scale=-0.5), reads=[rsm], writes=[rsm])
            for dc in range(2):
                c.op("dve", lambda e: e.scalar_tensor_tensor(out=QmT[:, 2 * h + dc, :], in0=P[2 + dc][:, 0:512], scalar=mqw[:, dc:dc + 1], in1=rsm[:, :], op0=ALU.mult, op1=ALU.mult), reads=[P[2 + dc], mqw, rsm], writes=[QmT], merge_w=True)
        for h in range(4):
            et = et_ring.next()
            for mc in range(2):
                ps = P[2 + mc]
                for dc in range(2):
                    c.op("pe", lambda e: e.matmul(ps[:, 0:512], lhsT=KmT[:, 2 * h + dc, mc * 128:(mc + 1) * 128], rhs=QmT[:, 2 * h + dc, :], start=(dc == 0), stop=(dc == 1)), reads=[KmT, QmT], writes=[ps])
                c.op("act", lambda e: e.activation(out=et[:, mc, :], in_=ps[:, 0:512], func=AF.Exp), reads=[ps], writes=[et], merge_w=True)
            for mc in range(2):
                c.op("pe", lambda e: e.matmul(P[4][:, 0:512], lhsT=ones_bf[:, :], rhs=et[:, mc, :], start=(mc == 0), stop=(mc == 1)), reads=[ones_bf, et], writes=[P[4]])
            rd = rd_ring.next()
            c.op("act", lambda e: e.activation(out=rd[:, :], in_=P[4][:, 0:512], func=AF.Ln), reads=[P[4]], writes=[rd])
            c.op("act", lambda e: e.activation(out=rd[:, :], in_=rd[:, :], func=AF.Exp, scale=-1.0), reads=[rd], writes=[rd])
            for dvc in range(2):
                po = P[5 + dvc]
                for mc in range(2):
                    c.op("pe", lambda e: e.matmul(po[:, 0:512], lhsT=Vm[:, mc, h * 256 + dvc * 128:h * 256 + (dvc + 1) * 128], rhs=et[:, mc, :], start=(mc == 0), stop=(mc == 1)), reads=[Vm, et], writes=[po])
                c.op("dve", lambda e: e.tensor_tensor(out=OmT[:, 2 * h + dvc, :], in0=po[:, 0:512], in1=rd[:, :], op=ALU.mult), reads=[po, rd], writes=[OmT], merge_w=True)
        for ti in range(4):
            n = 4 * b + ti
            tsl = slice(ti * 128, (ti + 1) * 128)
            x2 = x2_ring.next()
            for half in range(2):
                px = P[5 + half]
                hs = slice(half * 512, (half + 1) * 512)
                for cc in range(8):
                    c.op("pe", lambda e: e.matmul(px[:, 0:512], lhsT=OmT[:, cc, tsl], rhs=Wmo[:, cc, hs], start=(cc == 0), stop=(cc == 7)), reads=[OmT, Wmo], writes=[px])
                c.op("dve", lambda e: e.tensor_tensor(out=x2[:, hs], in0=x1b[:, ti, hs], in1=px[:, 0:512], op=ALU.add), reads=[x1b, px], writes=[x2], merge_w=True)
            c.dma("pool", lambda e: e.dma_start(out=X2_d[n * 128:(n + 1) * 128, :], in_=x2[:, :]), owner=x2, reads=[x2], writes=[X2_d], merge_w=True)
            rs3 = norm_rstd(x2, x2[:, :])
            h3 = h3_ring.next()
            c.op("dve", lambda e: e.scalar_tensor_tensor(out=h3[:, :], in0=x2[:, :], scalar=rs3[:, 0:1], in1=WMOE[:, :], op0=ALU.mult, op1=ALU.mult), reads=[x2, rs3, WMOE], writes=[h3])
            h3b = h3b_ring.next()
            c.op("pool", lambda e: e.tensor_copy(out=h3b[:, :], in_=h3[:, :]), reads=[h3], writes=[h3b])
            c.dma("pool", lambda e: e.dma_start(out=H3_d[n * 128:(n + 1) * 128, :], in_=h3b[:, :]), owner=h3b, reads=[h3b], writes=[H3_d], merge_w=True)
            for kc in range(KC):
                pt = P[2 + kc // 4]
                c.op("pe", lambda e: e.transpose(out=pt[:, (kc % 4) * 128:(kc % 4 + 1) * 128], in_=h3[:, kc * 128:(kc + 1) * 128], identity=identf[:, :]), reads=[h3, identf], writes=[pt])
            h3T = h3T_ring.next()
            c.op("act", lambda e: e.activation(out=h3T[:, 0:4, :], in_=P[2][:, 0:512].rearrange("p (k t) -> p k t", k=4), func=AF.Identity), reads=[P[2]], writes=[h3T], merge_w=True)
            c.op("dve", lambda e: e.tensor_copy(out=h3T[:, 4:8, :], in_=P[3][:, 0:512].rearrange("p (k t) -> p k t", k=4)), reads=[P[3]], writes=[h3T], merge_w=True)
            for kc in range(KC):
                c.op("pe", lambda e: e.matmul(P[4][:, 0:36], lhsT=h3T[:, kc, :], rhs=Wrt[:, kc, :], start=(kc == 0), stop=(kc == KC - 1)), reads=[h3T, Wrt], writes=[P[4]])
            Ls, gmax, gm, ngm, ge, gsum, pen, ML, t8, nv1, ex, gtu, s12, dn = [sm_ring.next() for _ in range(14)]
            c.op("dve", lambda e: e.tensor_copy(out=Ls[:, 0:36], in_=P[4][:, 0:36]), reads=[P[4]], writes=[Ls])
            c.op("dve", lambda e: e.tensor_reduce(out=gmax[:, 0:1], in_=Ls[:, 0:4], axis=AX.X, op=ALU.max), reads=[Ls], writes=[gmax])
            c.op("dve", lambda e: e.tensor_scalar(out=gm[:, 0:4], in0=Ls[:, 0:4], scalar1=gmax[:, 0:1], scalar2=None, op0=ALU.is_equal), reads=[Ls, gmax], writes=[gm])
            c.op("dve", lambda e: e.tensor_scalar(out=ngm[:, 0:1], in0=gmax[:, 0:1], scalar1=-1.0, scalar2=None, op0=ALU.mult), reads=[gmax], writes=[ngm])
            c.op("act", lambda e: e.activation(out=ge[:, 0:4], in_=Ls[:, 0:4], func=AF.Exp, bias=ngm[:, 0:1], accum_out=gsum[:, 0:1]), reads=[Ls, ngm], writes=[ge, gsum])
            c.op("dve", lambda e: e.tensor_scalar(out=pen[:, 0:4], in0=gm[:, 0:4], scalar1=-1.0, scalar2=1e30, op0=ALU.add, op1=ALU.mult), reads=[gm], writes=[pen])
            c.op("dve", lambda e: e.tensor_tensor(out=ML[:, 0:32].rearrange("p (g x) -> p g x", g=4), in0=Ls[:, 4:36].rearrange("p (g x) -> p g x", g=4), in1=pen[:, 0:4].unsqueeze(2).to_broadcast([128, 4, 8]), op=ALU.add), reads=[Ls, pen], writes=[ML])
            c.op("dve", lambda e: e.max(out=t8[:, 0:8], in_=ML[:, 0:32]), reads=[ML], writes=[t8])
            c.op("dve", lambda e: e.tensor_scalar(out=OHs[:, n, :], in0=ML[:, 0:32], scalar1=t8[:, 1:2], scalar2=None, op0=ALU.is_ge), reads=[ML, t8], writes=[OHs], merge_w=True)
            c.op("dve", lambda e: e.tensor_scalar(out=nv1[:, 0:1], in0=t8[:, 0:1], scalar1=-1.0, scalar2=None, op0=ALU.mult), reads=[t8], writes=[nv1])
            c.op("act", lambda e: e.activation(out=ex[:, 0:32], in_=ML[:, 0:32], func=AF.Exp, bias=nv1[:, 0:1]), reads=[ML, nv1], writes=[ex])
            c.op("dve", lambda e: e.scalar_tensor_tensor(out=gtu[:, 0:32], in0=OHs[:, n, :], scalar=1.0, in1=ex[:, 0:32], op0=ALU.mult, op1=ALU.mult, accum_out=s12[:, 0:1]), reads=[OHs, ex], writes=[gtu, s12])
            c.op("dve", lambda e: e.tensor_tensor(out=dn[:, 0:1], in0=gsum[:, 0:1], in1=s12[:, 0:1], op=ALU.mult), reads=[gsum, s12], writes=[dn])
            c.op("dve", lambda e: e.reciprocal(out=dn[:, 0:1], in_=dn[:, 0:1]), reads=[dn], writes=[dn])
            c.op("dve", lambda e: e.tensor_scalar(out=GTs[:, n, :], in0=gtu[:, 0:32], scalar1=dn[:, 0:1], scalar2=None, op0=ALU.mult), reads=[gtu, dn], writes=[GTs], merge_w=True)
    phases["T"] = (c.ninst, c.nwait)
    if cfg.debug:
        c.dma("sp", lambda e: e.dma_start(out=DBG_d[0:128, 3072:3072 + 32], in_=GTs[:, 0, :]), owner=GTs, reads=[GTs], writes=[DBG_d], merge_w=True)
    if cfg.stop == "T":
        c.finish([X2_d, H3_d])
        c.pop_all()
        return nc, c, phases
    c.pop()

    c.push()
    TRI = c.sb("TRI", [128, 128], BF16)
    c.op("dve", lambda e: e.tensor_scalar(out=TRI[:, :], in0=kq[:, :], scalar1=0.0, scalar2=None, op0=ALU.is_lt), reads=[kq], writes=[TRI])
    RK = c.sb("RK", [128, NT, 32], F32)
    acc32 = c.sb("acc32", [128, 32], F32)
    accb_ring = c.ring("accb", 2, [128, 32], BF16)
    ohb_ring = c.ring("ohb", 2, [128, 32], BF16)
    accb = None
    for n in range(NT):
        ohb = ohb_ring.next()
        c.op("pool", lambda e: e.tensor_copy(out=ohb[:, :], in_=OHs[:, n, :]), reads=[OHs], writes=[ohb])
        pr = P[n % 2]
        c.op("pe", lambda e: e.matmul(pr[:, 0:32], lhsT=TRI[:, :], rhs=ohb[:, :], start=True, stop=(n == 0)), reads=[TRI, ohb], writes=[pr])
        if n > 0:
            c.op("pe", lambda e: e.matmul(pr[:, 0:32], lhsT=ones_bf[:, :], rhs=accb[:, :], start=False, stop=True), reads=[ones_bf, accb], writes=[pr])
        c.op("act", lambda e: e.activation(out=RK[:, n, :], in_=pr[:, 0:32], func=AF.Identity), reads=[pr], writes=[RK], merge_w=True)
        if n == 0:
            c.op("dve", lambda e: e.tensor_copy(out=acc32[:, :], in_=OHs[:, 0, :]), reads=[OHs], writes=[acc32])
        else:
            c.op("dve", lambda e: e.tensor_tensor(out=acc32[:, :], in0=acc32[:, :], in1=OHs[:, n, :], op=ALU.add), reads=[acc32, OHs], writes=[acc32])
        accb = accb_ring.next()
        c.op("dve", lambda e: e.tensor_copy(out=accb[:, :], in_=acc32[:, :]), reads=[acc32], writes=[accb])
    c.op("pe", lambda e: e.matmul(P[2][:, 0:32], lhsT=ones_bf[:, :], rhs=accb[:, :], start=True, stop=True), reads=[ones_bf, accb], writes=[P[2]])
    cnt_i = c.sb("cnt_i", [128, 32], I32)
    pcf = c.sb("pcf", [128, 32], F32)
    pend = c.sb("pend", [128, 32], F32)
    pst1 = c.sb("pst1", [128, 32], F32)
    ones32 = c.sb("ones32", [128, 32], F32)
    c.op("pool", lambda e: e.memset(ones32[:, :], 1.0), writes=[ones32])
    c.op("dve", lambda e: e.tensor_scalar(out=cnt_i[:, :], in0=P[2][:, 0:32], scalar1=127.0, scalar2=None, op0=ALU.add), reads=[P[2]], writes=[cnt_i])
    c.op("dve", lambda e: e.tensor_scalar(out=cnt_i[:, :], in0=cnt_i[:, :], scalar1=7, scalar2=7, op0=ALU.logical_shift_right, op1=ALU.logical_shift_left), reads=[cnt_i], writes=[cnt_i])
    c.op("dve", lambda e: e.tensor_copy(out=pcf[:, :], in_=cnt_i[:, :]), reads=[cnt_i], writes=[pcf])
    c.op("dve", lambda e: e.tensor_tensor_scan(out=pend[:, :], data0=ones32[:, :], data1=pcf[:, :], initial=0.0, op0=ALU.mult, op1=ALU.add), reads=[ones32, pcf], writes=[pend])
    c.op("dve", lambda e: e.tensor_tensor(out=pst1[:, :], in0=pend[:, :], in1=pcf[:, :], op=ALU.subtract), reads=[pend, pcf], writes=[pst1])
    c.op("dve", lambda e: e.tensor_scalar(out=pst1[:, :], in0=pst1[:, :], scalar1=1.0, scalar2=None, op0=ALU.add), reads=[pst1], writes=[pst1])
    thr_i = c.sb("thr_i", [128, NBLK], I32)
    thr = c.sb("thr", [128, NBLK], F32)
    cmpb = c.sb("cmpb", [128, NBLK, 32], F32)
    bef = c.sb("bef", [128, NBLK], F32)
    pidx_i = c.sb("pidx_i", [128, 1], I32)
    pidx = c.sb("pidx", [128, 1], F32)
    widx_f = c.sb("widx_f", [128, NBLK, 2], F32)
    WIDX = c.sb("WIDX", [128, NBLK, 2], I32)
    c.op("pool", lambda e: e.iota(thr_i[:, :], pattern=[[128, NBLK]], base=0, channel_multiplier=0), writes=[thr_i])
    c.op("dve", lambda e: e.tensor_copy(out=thr[:, :], in_=thr_i[:, :]), reads=[thr_i], writes=[thr])
    c.op("dve", lambda e: e.tensor_tensor(out=cmpb[:, :, :], in0=pend[:, :].unsqueeze(1).to_broadcast([128, NBLK, 32]), in1=thr[:, :].unsqueeze(2).to_broadcast([128, NBLK, 32]), op=ALU.is_le), reads=[pend, thr], writes=[cmpb])
    c.op("dve", lambda e: e.tensor_reduce(out=bef[:, :], in_=cmpb[:, :, :], axis=AX.X, op=ALU.add), reads=[cmpb], writes=[bef])
    c.op("dve", lambda e: e.tensor_scalar(out=bef[:, :], in0=bef[:, :], scalar1=31.0, scalar2=None, op0=ALU.min), reads=[bef], writes=[bef])
    c.op("pool", lambda e: e.iota(pidx_i[:, :], pattern=[[0, 1]], base=0, channel_multiplier=1), writes=[pidx_i])
    c.op("dve", lambda e: e.tensor_copy(out=pidx[:, :], in_=pidx_i[:, :]), reads=[pidx_i], writes=[pidx])
    c.op("dve", lambda e: e.tensor_scalar(out=widx_f[:, :, 0], in0=bef[:, :], scalar1=128.0, scalar2=pidx[:, 0:1], op0=ALU.mult, op1=ALU.add), reads=[bef, pidx], writes=[widx_f], merge_w=True)
    c.op("dve", lambda e: e.tensor_scalar(out=widx_f[:, :, 0], in0=widx_f[:, :, 0], scalar1=2.0, scalar2=None, op0=ALU.mult), reads=[widx_f], writes=[widx_f], merge_w=True)
    c.op("dve", lambda e: e.tensor_scalar(out=widx_f[:, :, 1], in0=widx_f[:, :, 0], scalar1=1.0, scalar2=None, op0=ALU.add), reads=[widx_f], writes=[widx_f], merge_w=True)
    skp = c.sb("skp", [128, NBLK], F32)
    c.op("dve", lambda e: e.tensor_tensor(out=skp[:, 2:NBLK], in0=bef[:, 2:NBLK], in1=bef[:, 0:NBLK - 2], op=ALU.is_equal), reads=[bef], writes=[skp])
    c.op("dve", lambda e: e.tensor_scalar(out=skp[:, 2:NBLK], in0=skp[:, 2:NBLK], scalar1=1.0e6, scalar2=None, op0=ALU.mult), reads=[skp], writes=[skp])
    c.op("dve", lambda e: e.tensor_tensor(out=widx_f[:, 2:NBLK, :], in0=widx_f[:, 2:NBLK, :], in1=skp[:, 2:NBLK].unsqueeze(2).to_broadcast([128, NBLK - 2, 2]), op=ALU.add), reads=[widx_f, skp], writes=[widx_f])
    c.op("dve", lambda e: e.tensor_copy(out=WIDX[:, :, :], in_=widx_f[:, :, :]), reads=[widx_f], writes=[WIDX])
    DSTf = c.sb("DSTf", [128, NT, 2], F32)
    DSTi = c.sb("DSTi", [128, NT, 2], I32)
    GAB = c.sb("GAB", [128, NT, 2], F32)
    m1_ring = c.ring("m1r", 4, [128, 32], F32)
    sc_ring = c.ring("scr", 8, [128, 2], F32)
    for n in range(NT):
        d1, m1, mb, jk2 = [m1_ring.next() for _ in range(4)]
        db1, sm, gs = [sc_ring.next() for _ in range(3)]
        c.op("dve", lambda e: e.tensor_tensor(out=d1[:, :], in0=RK[:, n, :], in1=pst1[:, :], op=ALU.add), reads=[RK, pst1], writes=[d1])
        c.op("dve", lambda e: e.tensor_tensor(out=m1[:, :], in0=d1[:, :], in1=OHs[:, n, :], op=ALU.mult), reads=[d1, OHs], writes=[m1])
        c.op("dve", lambda e: e.tensor_reduce(out=db1[:, 0:1], in_=m1[:, :], axis=AX.X, op=ALU.max), reads=[m1], writes=[db1])
        c.op("dve", lambda e: e.tensor_reduce(out=sm[:, 0:1], in_=m1[:, :], axis=AX.X, op=ALU.add), reads=[m1], writes=[sm])
        c.op("dve", lambda e: e.scalar_tensor_tensor(out=DSTf[:, n, 0:1], in0=sm[:, 0:1], scalar=-1.0, in1=db1[:, 0:1], op0=ALU.add, op1=ALU.subtract), reads=[sm, db1], writes=[DSTf], merge_w=True)
        c.op("dve", lambda e: e.tensor_scalar(out=DSTf[:, n, 1:2], in0=db1[:, 0:1], scalar1=-1.0, scalar2=None, op0=ALU.add), reads=[db1], writes=[DSTf], merge_w=True)
        c.op("dve", lambda e: e.tensor_scalar(out=mb[:, :], in0=m1[:, :], scalar1=db1[:, 0:1], scalar2=None, op0=ALU.is_ge), reads=[m1, db1], writes=[mb])
        c.op("dve", lambda e: e.scalar_tensor_tensor(out=jk2[:, :], in0=GTs[:, n, :], scalar=1.0, in1=mb[:, :], op0=ALU.mult, op1=ALU.mult, accum_out=GAB[:, n, 1:2]), reads=[GTs, mb], writes=[jk2, GAB], merge_w=True)
        c.op("dve", lambda e: e.tensor_reduce(out=gs[:, 0:1], in_=GTs[:, n, :], axis=AX.X, op=ALU.add), reads=[GTs], writes=[gs])
        c.op("dve", lambda e: e.tensor_tensor(out=GAB[:, n, 0:1], in0=gs[:, 0:1], in1=GAB[:, n, 1:2], op=ALU.subtract), reads=[gs, GAB], writes=[GAB], merge_w=True)
    c.op("dve", lambda e: e.tensor_copy(out=DSTi[:, :, :], in_=DSTf[:, :, :]), reads=[DSTf], writes=[DSTi])
    fillb = c.sb("fillb", [128, NBLK, 16], I32)
    TOK16 = c.sb("TOK16", [128, NT, 16], I32)
    zpad = c.sb("zpad", [128, D], BF16)
    c.op("pool", lambda e: e.iota(fillb[:, :, :], pattern=[[0, NBLK], [0, 16]], base=NTOK, channel_multiplier=0), writes=[fillb])
    c.op("pool", lambda e: e.iota(TOK16[:, :, :], pattern=[[128, NT], [0, 16]], base=0, channel_multiplier=1), writes=[TOK16])
    c.op("pool", lambda e: e.memset(zpad[:, :], 0.0), writes=[zpad])
    c.dma("sp", lambda e: e.dma_start(out=SLOT_d.t.ap().rearrange("(b p) x -> p b x", p=128), in_=fillb[:, :, :]), owner=fillb, reads=[fillb], writes=[SLOT_d])
    c.dma("sp", lambda e: e.dma_start(out=H3_d[NTOK:NTOK + 128, :], in_=zpad[:, :]), owner=zpad, reads=[zpad], writes=[H3_d], merge_w=True)
    for n in range(NT):
        for k2 in range(2):
            c.dma("pool", lambda e: e.indirect_dma_start(out=SLOT_d.t.ap(), out_offset=bass.IndirectOffsetOnAxis(ap=DSTi[:, n, k2:k2 + 1], axis=0), in_=TOK16[:, n, :], in_offset=None), owner=TOK16, reads=[TOK16, DSTi], writes=[SLOT_d], merge_w=True)
    phases["M1"] = (c.ninst, c.nwait)
    if cfg.debug:
        c.dma("sp", lambda e: e.dma_start(out=DBG_d[:, 3200:3200 + 2 * NT], in_=DSTf[:, :, :].rearrange("p n k -> p (n k)")), owner=DSTf, reads=[DSTf], writes=[DBG_d], merge_w=True)
        c.dma("sp", lambda e: e.dma_start(out=DBG_d[:, 3400:3400 + 2 * NT], in_=GAB[:, :, :].rearrange("p n k -> p (n k)")), owner=GAB, reads=[GAB], writes=[DBG_d], merge_w=True)
        c.dma("sp", lambda e: e.dma_start(out=DBG_d[:, 3600:3600 + NBLK], in_=bef[:, :]), owner=bef, reads=[bef], writes=[DBG_d], merge_w=True)
    if cfg.stop == "M1":
        c.finish([SLOT_d])
        c.pop_all()
        return nc, c, phases

    w1v = w1.t.ap().rearrange("e (p k) f -> (e p) (k f)", p=128).rearrange("r (two x) -> (r two) x", two=2)
    w3v = w3.t.ap().rearrange("e (p k) f -> (e p) (k f)", p=128).rearrange("r (two x) -> (r two) x", two=2)
    w2v = w2.t.ap().rearrange("e (p k) f -> (e p) (k f)", p=128).rearrange("r (two x) -> (r two) x", two=2)
    bc_reg = nc.gpsimd.alloc_register("bc_reg")
    nc.gpsimd.reg_mov(bc_reg, 32 * 128 * 2 - 1)
    sidx_ring = c.ring("sidx", 3, [128, 16], I32)
    xg_ring = c.ring("xg", 2, [128, D], BF16)
    xT_ring = c.ring("xTm", 2, [128, 8, 128], BF16)
    w1b_ring = c.ring("w1b", 2, [128, 4096], BF16)
    w3b_ring = c.ring("w3b", 2, [128, 4096], BF16)
    w2b_ring = c.ring("w2b", 2, [128, 4096], BF16)
    g1_ring = c.ring("g1", 2, [128, 512], F32)
    ggb_ring = c.ring("ggb", 2, [128, 512], BF16)
    gT_ring = c.ring("gTm", 2, [128, 4, 128], BF16)
    ys_ring = c.ring("ys", 2, [128, D], F32)
    SLT = c.sb("SLT", [128, NBLK, 16], I32)
    c.dma("sp", lambda e: e.dma_start(out=SLT[:, :, :], in_=SLOT_d.t.ap().rearrange("(b p) x -> p b x", p=128)), owner=SLT, reads=[SLOT_d], writes=[SLT])
    for b in range(NBLK):
        xg = xg_ring.next()
        c.dma("pool", lambda e: e.indirect_dma_start(out=xg[:, :], out_offset=None, in_=H3_d.t.ap(), in_offset=bass.IndirectOffsetOnAxis(ap=SLT[:, b, 0:1], axis=0)), owner=xg, reads=[H3_d, SLT], writes=[xg])
        w1b, w3b, w2b = w1b_ring.next(), w3b_ring.next(), w2b_ring.next()
        for wv, wb in ((w1v, w1b), (w3v, w3b), (w2v, w2b)):
            for half in range(2):
                c.dma("pool", lambda e: e.indirect_dma_start(out=wb[:, half * 2048:(half + 1) * 2048], out_offset=None, in_=wv, in_offset=bass.IndirectOffsetOnAxis(ap=WIDX[:, b, half:half + 1], axis=0), bounds_check=bc_reg, oob_is_err=False), owner=wb, reads=[WIDX], writes=[wb], merge_w=(half == 1))
        pxt = P[b % 2]
        pv = pbf(pxt)
        for kc in range(KC):
            c.op("pe", lambda e: e.transpose(out=pv[:, kc * 128:(kc + 1) * 128], in_=xg[:, kc:kc + 8 * 127 + 1:8], identity=ident[:, :]), reads=[xg, ident], writes=[pxt])
        xT = xT_ring.next()
        c.op("act", lambda e: e.activation(out=xT[:, :, :], in_=pv[:, 0:1024].rearrange("p (k t) -> p k t", k=8), func=AF.Identity), reads=[pxt], writes=[xT])
        ph1, ph3 = P[2 + 2 * (b % 2)], P[3 + 2 * (b % 2)]
        w1k = w1b[:, :].rearrange("p (k f) -> p k f", k=8)
        w3k = w3b[:, :].rearrange("p (k f) -> p k f", k=8)
        w2k = w2b[:, :].rearrange("p (k f) -> p k f", k=4)
        for kc in range(KC):
            c.op("pe", lambda e: e.matmul(ph1[:, 0:512], lhsT=xT[:, kc, :], rhs=w1k[:, kc, :], start=(kc == 0), stop=(kc == KC - 1)), reads=[xT, w1b], writes=[ph1])
        for kc in range(KC):
            c.op("pe", lambda e: e.matmul(ph3[:, 0:512], lhsT=xT[:, kc, :], rhs=w3k[:, kc, :], start=(kc == 0), stop=(kc == KC - 1)), reads=[xT, w3b], writes=[ph3])
        g1 = g1_ring.next()
        c.op("act", lambda e: e.activation(out=g1[:, :], in_=ph1[:, 0:512], func=AF.Exp, scale=-1.0), reads=[ph1], writes=[g1])
        c.op("act", lambda e: e.activation(out=g1[:, :], in_=g1[:, :], func=AF.Ln, bias=1.0), reads=[g1], writes=[g1])
        c.op("act", lambda e: e.activation(out=g1[:, :], in_=g1[:, :], func=AF.Exp, scale=-1.0), reads=[g1], writes=[g1])
        c.op("dve", lambda e: e.tensor_tensor(out=g1[:, :], in0=g1[:, :], in1=ph1[:, 0:512], op=ALU.mult), reads=[g1, ph1], writes=[g1])
        ggb = ggb_ring.next()
        c.op("dve", lambda e: e.tensor_tensor(out=ggb[:, :], in0=g1[:, :], in1=ph3[:, 0:512], op=ALU.mult), reads=[g1, ph3], writes=[ggb])
        for fc in range(4):
            c.op("pe", lambda e: e.transpose(out=pv[:, fc * 128:(fc + 1) * 128], in_=ggb[:, fc:fc + 4 * 127 + 1:4], identity=ident[:, :]), reads=[ggb, ident], writes=[pxt])
        gT = gT_ring.next()
        c.op("act", lambda e: e.activation(out=gT[:, :, :], in_=pv[:, 0:512].rearrange("p (k t) -> p k t", k=4), func=AF.Identity), reads=[pxt], writes=[gT])
        ys = ys_ring.next()
        for half in range(2):
            py = P[6 + half]
            for fc in range(4):
                c.op("pe", lambda e: e.matmul(py[:, 0:512], lhsT=gT[:, fc, :], rhs=w2k[:, fc, half * 512:(half + 1) * 512], start=(fc == 0), stop=(fc == 3)), reads=[gT, w2b], writes=[py])
            if half == 0:
                c.op("act", lambda e: e.activation(out=ys[:, 0:512], in_=py[:, 0:512], func=AF.Identity), reads=[py], writes=[ys], merge_w=True)
            else:
                c.op("dve", lambda e: e.tensor_copy(out=ys[:, 512:1024], in_=py[:, 0:512]), reads=[py], writes=[ys], merge_w=True)
        c.dma("sp", lambda e: e.dma_start(out=YS_d[b * 128:(b + 1) * 128, :], in_=ys[:, :]), owner=ys, reads=[ys], writes=[YS_d], merge_w=True)
    phases["M2"] = (c.ninst, c.nwait)

    ya_ring = c.ring("ya", 2, [128, D], F32)
    yb_ring = c.ring("yb", 2, [128, D], F32)
    x2c_ring = c.ring("x2c", 2, [128, D], F32)
    oo_ring = c.ring("oo", 2, [128, D], F32)
    for n in range(NT):
        ya, yb, x2c, oo = ya_ring.next(), yb_ring.next(), x2c_ring.next(), oo_ring.next()
        c.dma("pool", lambda e: e.indirect_dma_start(out=ya[:, :], out_offset=None, in_=YS_d.t.ap(), in_offset=bass.IndirectOffsetOnAxis(ap=DSTi[:, n, 0:1], axis=0)), owner=ya, reads=[YS_d, DSTi], writes=[ya])
        c.dma("pool", lambda e: e.indirect_dma_start(out=yb[:, :], out_offset=None, in_=YS_d.t.ap(), in_offset=bass.IndirectOffsetOnAxis(ap=DSTi[:, n, 1:2], axis=0)), owner=yb, reads=[YS_d, DSTi], writes=[yb])
        c.dma("sp", lambda e: e.dma_start(out=x2c[:, :], in_=X2_d[n * 128:(n + 1) * 128, :]), owner=x2c, reads=[X2_d], writes=[x2c])
        c.op("dve", lambda e: e.scalar_tensor_tensor(out=oo[:, :], in0=ya[:, :], scalar=GAB[:, n, 0:1], in1=x2c[:, :], op0=ALU.mult, op1=ALU.add), reads=[ya, GAB, x2c], writes=[oo])
        c.op("dve", lambda e: e.scalar_tensor_tensor(out=oo[:, :], in0=yb[:, :], scalar=GAB[:, n, 1:2], in1=oo[:, :], op0=ALU.mult, op1=ALU.add), reads=[yb, GAB, oo], writes=[oo])
        c.dma("act", lambda e: e.dma_start(out=y_d[n * 128:(n + 1) * 128, :], in_=oo[:, :]), owner=oo, reads=[oo], writes=[y_d], merge_w=True)
    phases["M3"] = (c.ninst, c.nwait)
    c.finish([y_d])
    c.pop_all()
    return nc, c, phases


def prep_core(inp, cfg, b, j, S):
    LS, NA, NF = cfg.LS, cfg.NA, cfg.NF
    t0 = j * LS
    x = inp["x"][b]
    xa = np.zeros((NA * 128, D), np.float32)
    lo, hi = t0 - 1024, t0 + LS + 1024
    slo, shi = max(lo, 0), min(hi, S)
    xa[slo - lo:shi - lo] = x[slo:shi]
    far_idx = np.concatenate([np.arange(0, t0), np.arange(t0 + LS, S)]).astype(np.int64)
    assert far_idx.size == NF * 128
    xf = np.ascontiguousarray(x[far_idx])
    posf = np.ascontiguousarray(far_idx.astype(np.float32).reshape(NF, 128).T)
    t01 = np.zeros((128, 2), np.float32)
    t01[:, 0] = t0
    t01[:, 1] = t0 + LS
    valt = np.zeros((128, cfg.NCHT), np.float32)
    kk = np.arange(128)
    for pi, d in enumerate(PATTERNS):
        for r in range(d):
            for ch in range(cfg.nch[pi]):
                T = d * (128 * ch - 64 + kk) + r + t0
                valt[:, cfg.chbase[pi] + r * cfg.nch[pi] + ch] = ((T >= 0) & (T < S)).astype(np.float32)

    def kc_layout(v):
        return np.ascontiguousarray(v.reshape(8, 128).T)

    def f32(a):
        return np.ascontiguousarray(a, dtype=np.float32)
    m = {
        "xa": xa, "xf": xf, "posf": posf, "t01": t01, "valt": valt,
        "memx": f32(inp["mem"][b]),
        "nw_mix": kc_layout(inp["norm_mix_w"][0]),
        "nw_mem": kc_layout(inp["norm_mem_w"][0]),
        "nw_memkv": kc_layout(inp["norm_memkv_w"][0]),
        "nw_moe": f32(inp["norm_moe_w"][0].reshape(1, D)),
        "aq_w": f32(np.tile(inp["attn_q_norm_w"][0], 2).reshape(128, 1)),
        "ak_w": f32(np.tile(inp["attn_k_norm_w"][0], 2).reshape(128, 1)),
        "mq_w": f32(inp["mem_q_norm_w"][0].reshape(2, 128).T),
        "mk_w": f32(inp["mem_k_norm_w"][0].reshape(2, 128).T),
        "dec": f32(np.concatenate([inp["ret_decay_f"][0], inp["ret_decay_b"][0]]).reshape(1, 8)),
        "gn_w": f32(inp["ret_gn_w"][0].reshape(1, 512)),
        "w_in": f32(inp["w_in"][0]), "w_out": f32(inp["w_out"][0]),
        "w_mq": f32(inp["w_mq"][0]), "w_mkv": f32(inp["w_mkv"][0]), "w_mo": f32(inp["w_mo"][0]),
        "w_rt": f32(np.concatenate([inp["w_group"][0], inp["w_router"][0].transpose(1, 0, 2).reshape(D, 32)], axis=1)),
        "w1": f32(inp["w_exp_gate"][0]), "w3": f32(inp["w_exp_up"][0]), "w2": f32(inp["w_exp_down"][0]),
    }
    return m


_CACHE = {}


def kernel(**inputs):
    inp = {k: np.asarray(v) for k, v in inputs.items()}
    B, S, _ = inp["x"].shape
    cfg = Cfg(NT=32, NF=96)
    assert S == 4 * cfg.LS and B == 2
    if "nc" not in _CACHE:
        _CACHE["nc"] = build(cfg)[0]
    nc = _CACHE["nc"]
    in_maps = [prep_core(inp, cfg, cidx // 4, cidx % 4, S) for cidx in range(8)]
    res = run_bass_kernel_spmd(nc, in_maps, core_ids=list(range(8)))
    out = np.empty((B, S, D), np.float32)
    for cidx in range(8):
        b, j = cidx // 4, cidx % 4
        out[b, j * cfg.LS:(j + 1) * cfg.LS] = np.asarray(res.results[cidx]["y"], dtype=np.float32)
    return out
```

```python
import contextlib
import numpy as np
import concourse.bass as bass
import concourse.mybir as mybir
from concourse.bass_utils import run_bass_kernel_spmd

F32 = mybir.dt.float32
BF16 = mybir.dt.bfloat16
I32 = mybir.dt.int32
AF = mybir.ActivationFunctionType
ALU = mybir.AluOpType
AX = mybir.AxisListType

D = 1024
KC = 8
NEG = -30000.0
EPS = 1e-6
GN_EPS = 1e-5
PATTERNS = (1, 4, 16)
SLOPES = [2.0 ** (-8.0 * (h + 1) / 8.0) for h in range(8)]


class Buf:
    def __init__(self, name, t=None):
        self.name = name
        self.t = t
        self.w = {}
        self.r = {}
        self.dsem = {}
        self.dcnt = {}
        self.excl = False

    def __getitem__(self, idx):
        return self.t[idx]


class Ring:
    def __init__(self, bufs):
        self.bufs = bufs
        self.i = 0

    def next(self):
        b = self.bufs[self.i % len(self.bufs)]
        self.i += 1
        return b


class Ctx:
    def __init__(self, nc):
        self.nc = nc
        self.e = {"pe": nc.tensor, "act": nc.scalar, "dve": nc.vector,
                  "pool": nc.gpsimd, "sp": nc.sync}
        self.psem = {}
        self.pcnt = {}
        for k in ("pe", "act", "dve", "pool"):
            self.psem[k] = nc.alloc_semaphore("prog_" + k)
            self.pcnt[k] = 0
        self.seen = {k: {} for k in self.e}
        self.nwait = 0
        self.ninst = 0
        self.nsem = 4
        self.owners = []
        self.stacks = []
        self.bar_sem = nc.alloc_semaphore("bar")
        self.bar_cnt = 0

    def sb(self, name, shape, dt=F32):
        if self.stacks:
            t = self.stacks[-1].enter_context(self.nc.sbuf_tensor(name, list(shape), dt, side="right"))
            return Buf(name, t)
        return Buf(name, self.nc.alloc_sbuf_tensor(name, list(shape), dt, side="left"))

    def barrier(self):
        need = {}
        for k in self.psem:
            if self.pcnt[k] > 0:
                need["prog_" + k] = (self.psem[k], self.pcnt[k])
        for o in self.owners:
            for qt, sm in o.dsem.items():
                need["d%s_%s" % (qt, o.name)] = (sm, o.dcnt[qt])
        self._wait("sp", need)
        self.bar_cnt += 1
        self.e["sp"].sem_inc(self.bar_sem, 1)
        for k in ("pe", "act", "dve", "pool"):
            self.e[k].wait_ge(self.bar_sem, self.bar_cnt)
            for kk, (sm, v) in need.items():
                if self.seen[k].get(kk, 0) < v:
                    self.seen[k][kk] = v

    def push(self):
        self.stacks.append(contextlib.ExitStack())

    def pop(self):
        self.barrier()
        self.stacks.pop().close()

    def pop_all(self):
        while self.stacks:
            self.pop()

    def ring(self, name, n, shape, dt=F32):
        return Ring([self.sb("%s%d" % (name, i), shape, dt) for i in range(n)])

    def ps(self, name):
        b = Buf(name, self.nc.alloc_psum_tensor(name, [128, 512], F32))
        b.excl = True
        return b

    def dram(self, name, shape, dt, kind="Internal"):
        return Buf(name, self.nc.dram_tensor(name, list(shape), dt, kind=kind))

    def _collect(self, eng, reads, writes):
        need = {}

        def add(d):
            for k, (s, v) in d.items():
                if k not in need or need[k][1] < v:
                    need[k] = (s, v)
        own = "prog_" + eng
        for b in reads:
            add(b.w)
            if b.excl:
                add({k: v for k, v in b.r.items() if k != own})
        for b in writes:
            add(b.w)
            add(b.r)
        if eng == "pe":
            need.pop("prog_pe", None)
        return need

    def _wait(self, eng, need):
        seen = self.seen[eng]
        for k, (s, v) in need.items():
            if seen.get(k, 0) < v:
                self.e[eng].wait_ge(s, v)
                seen[k] = v
                self.nwait += 1

    def _record(self, key, sem, val, reads, writes, merge_w):
        for b in reads:
            old = b.r.get(key)
            if old is None or old[1] < val:
                b.r[key] = (sem, val)
        for b in writes:
            if merge_w:
                b.w[key] = (sem, val)
            else:
                b.w = {key: (sem, val)}
                b.r = {}

    def op(self, eng, fn, reads=(), writes=(), merge_w=False):
        need = self._collect(eng, reads, writes)
        self._wait(eng, need)
        inst = fn(self.e[eng])
        self.pcnt[eng] += 1
        inst.then_inc(self.psem[eng], 1)
        self.ninst += 1
        self._record("prog_" + eng, self.psem[eng], self.pcnt[eng], reads, writes, merge_w)
        return inst

    def dma(self, q, fn, owner, reads=(), writes=(), merge_w=False, indep=False):
        need = self._collect(q, reads, writes)
        qt = "sw" if q == "pool" else "hw"
        if indep:
            need.pop("d%s_%s" % (qt, owner.name), None)
        self._wait(q, need)
        if qt not in owner.dsem:
            owner.dsem[qt] = self.nc.alloc_semaphore("d%s_%s" % (qt, owner.name))
            owner.dcnt[qt] = 0
            self.nsem += 1
            if owner not in self.owners:
                self.owners.append(owner)
        inst = fn(self.e[q])
        owner.dcnt[qt] += 16
        inst.then_inc(owner.dsem[qt], 16)
        self.ninst += 1
        self._record("d%s_%s" % (qt, owner.name), owner.dsem[qt], owner.dcnt[qt], reads, writes, merge_w)
        return inst

    def finish(self, bufs=(), eng="sp"):
        need = {}
        for o in self.owners:
            for qt, s in o.dsem.items():
                need["d%s_%s" % (qt, o.name)] = (s, o.dcnt[qt])
        for b in bufs:
            for k, (s, v) in b.w.items():
                if k not in need or need[k][1] < v:
                    need[k] = (s, v)
        self._wait(eng, need)


class Cfg:
    def __init__(self, NT=32, NF=96, debug=False, stop=None, skip=True):
        self.skip = skip
        self.NT = NT
        self.NH = 8
        self.NF = NF
        self.NA = NT + 16
        self.LS = NT * 128
        self.NBLK = 2 * NT + 32
        self.debug = debug
        self.stop = stop
        self.nq = [self.LS // d // 128 for d in PATTERNS]
        self.nch = [q + 1 for q in self.nq]
        self.chbase = []
        o = 0
        for d, n in zip(PATTERNS, self.nch):
            self.chbase.append(o)
            o += d * n
        self.NCHT = o


class _Stop(Exception):
    pass


def build(cfg):
    nc = bass.Bass("TRN2", target_bir_lowering=False)
    c = Ctx(nc)
    phases = {}
    try:
        return _build_body(cfg, nc, c, phases)
    except _Stop:
        c.finish()
        c.pop_all()
        return nc, c, phases


def _build_body(cfg, nc, c, phases):
    NT, NF, NA, LS, NBLK = cfg.NT, cfg.NF, cfg.NA, cfg.LS, cfg.NBLK
    NTOK = LS
    dbg = "ExternalOutput" if cfg.debug else "Internal"

    def din(name, shape, dt=F32):
        return c.dram(name, shape, dt, kind="ExternalInput")

    xa = din("xa", [NA * 128, D])
    xf = din("xf", [NF * 128, D])
    posf = din("posf", [128, NF])
    t01 = din("t01", [128, 2])
    valt_d = din("valt", [128, cfg.NCHT])
    memx = din("memx", [256, D])
    nw_mix = din("nw_mix", [128, 8])
    nw_mem = din("nw_mem", [128, 8])
    nw_memkv = din("nw_memkv", [128, 8])
    nw_moe = din("nw_moe", [1, D])
    aq_w = din("aq_w", [128, 1])
    ak_w = din("ak_w", [128, 1])
    mq_w = din("mq_w", [128, 2])
    mk_w = din("mk_w", [128, 2])
    dec_d = din("dec", [1, 8])
    gn_w = din("gn_w", [1, 512])
    w_in = din("w_in", [D, 3072])
    w_out = din("w_out", [D, D])
    w_mq = din("w_mq", [D, D])
    w_mkv = din("w_mkv", [D, 2 * D])
    w_mo = din("w_mo", [D, D])
    w_rt = din("w_rt", [D, 36])
    w1 = din("w1", [32, D, 512])
    w3 = din("w3", [32, D, 512])
    w2 = din("w2", [32, 512, D])
    y_d = c.dram("y", [NTOK, D], F32, kind="ExternalOutput")

    QT_d = c.dram("QT_d", [4, 128, LS], BF16, kind=dbg)
    KT_d = c.dram("KT_d", [4, 128, NA * 128], BF16, kind=dbg)
    VT_d = c.dram("VT_d", [4, 128, NA * 128], BF16, kind=dbg)
    MIX_d = c.dram("MIX_d", [8, 128, LS], BF16, kind=dbg)
    X2_d = c.dram("X2_d", [NTOK, D], F32, kind=dbg)
    H3_d = c.dram("H3_d", [NTOK + 128, D], BF16, kind=dbg)
    YS_d = c.dram("YS_d", [NBLK * 128, D], F32, kind=dbg)
    SLOT_d = c.dram("SLOT_d", [NBLK * 128, 16], I32, kind=dbg)
    if cfg.debug:
        DBG_d = c.dram("DBG_d", [128, 4096], F32, kind="ExternalOutput")

    P = [c.ps("pb%d" % i) for i in range(8)]

    def pbf(b):
        return b.t.bitcast(BF16)

    ident = c.sb("ident", [128, 128], BF16)
    identf = c.sb("identf", [128, 128], F32)
    ones_bf = c.sb("ones_bf", [128, 128], BF16)
    zeros_bf = c.sb("zeros_bf", [128, 128], BF16)
    blk1 = c.sb("blk1", [128, 128], BF16)
    kq = c.sb("kq", [128, 128], F32)
    kqi = c.sb("kqi", [128, 128], I32)
    tmpA = c.sb("tmpA", [128, 128], F32)
    tmpB = c.sb("tmpB", [128, 128], F32)
    tmpC = c.sb("tmpC", [128, 128], F32)
    tmpD = c.sb("tmpD", [128, 128], F32)

    c.op("pool", lambda e: e.iota(kqi[:, :], pattern=[[-1, 128]], base=0, channel_multiplier=1), writes=[kqi])
    c.op("dve", lambda e: e.tensor_copy(out=kq[:, :], in_=kqi[:, :]), reads=[kqi], writes=[kq])
    c.op("dve", lambda e: e.tensor_scalar(out=identf[:, :], in0=kq[:, :], scalar1=0.0, scalar2=None, op0=ALU.is_equal), reads=[kq], writes=[identf])
    c.op("dve", lambda e: e.tensor_copy(out=ident[:, :], in_=identf[:, :]), reads=[identf], writes=[ident])
    c.op("pool", lambda e: e.memset(ones_bf[:, :], 1.0), writes=[ones_bf])
    c.op("pool", lambda e: e.memset(zeros_bf[:, :], 0.0), writes=[zeros_bf])
    zrow = c.sb("zrow", [128, 512], BF16)
    c.op("pool", lambda e: e.memset(zrow[:, :], 0.0), writes=[zrow])
    c.op("pool", lambda e: e.memset(blk1[:, :], 0.0), writes=[blk1])
    c.op("pool", lambda e: e.memset(blk1[0:64, 0:64], 1.0), writes=[blk1], merge_w=True)
    c.op("pool", lambda e: e.memset(blk1[64:128, 64:128], 1.0), writes=[blk1], merge_w=True)

    def load_small(name, src_ap, shape, dt=F32):
        b = c.sb(name, shape, dt)
        c.dma("sp", lambda e: e.dma_start(out=b.t[tuple(slice(None) for _ in shape)], in_=src_ap), owner=b, writes=[b])
        return b

    nwmix = load_small("nwmix", nw_mix[:, :], [128, 8])
    nwmem = load_small("nwmem", nw_mem[:, :], [128, 8])
    nwmemkv = load_small("nwmemkv", nw_memkv[:, :], [128, 8])
    aqw = load_small("aqw", aq_w[:, :], [128, 1])
    akw = load_small("akw", ak_w[:, :], [128, 1])
    mqw = load_small("mqw", mq_w[:, :], [128, 2])
    mkw = load_small("mkw", mk_w[:, :], [128, 2])
    decb = load_small("decb", dec_d[0:1, :].to_broadcast([128, 8]), [128, 8])
    posf_s = load_small("posf_s", posf[:, :], [128, NF])
    t01_s = load_small("t01_s", t01[:, :], [128, 2])
    c.op("dve", lambda e: e.tensor_scalar(out=aqw[:, :], in0=aqw[:, :], scalar1=0.125, scalar2=None, op0=ALU.mult), reads=[aqw], writes=[aqw])
    c.op("dve", lambda e: e.tensor_scalar(out=mqw[:, :], in0=mqw[:, :], scalar1=1.0 / 16.0, scalar2=None, op0=ALU.mult), reads=[mqw], writes=[mqw])

    xs_ring = c.ring("xs", 3, [128, D], F32)
    junk_ring = c.ring("junk", 2, [128, D], BF16)
    xn_ring = c.ring("xn", 2, [128, D], BF16)
    ss_ring = c.ring("ss", 4, [128, 1], F32)
    rstd_ring = c.ring("rstd", 4, [128, 1], F32)
    wst_ring = c.ring("wst", 2, [128, D], F32)
    cast_rr = [0]

    def wcast(dst_ap, src_ap, ncols, col_ap=None, mul=None, dstbuf=None):
        st = wst_ring.next()
        c.dma("sp", lambda e: e.dma_start(out=st[:, 0:ncols], in_=src_ap), owner=st, writes=[st])
        eng = ("dve", "pool")[cast_rr[0] % 2]
        cast_rr[0] += 1
        rd = [st]
        if col_ap is not None:
            rd.append(col_ap[0])
        if col_ap is None and mul is None:
            c.op(eng, lambda e: e.tensor_copy(out=dst_ap, in_=st[:, 0:ncols]), reads=rd, writes=[dstbuf], merge_w=True)
        elif col_ap is None:
            c.op(eng, lambda e: e.tensor_scalar(out=dst_ap, in0=st[:, 0:ncols], scalar1=float(mul), scalar2=1.0, op0=ALU.mult, op1=ALU.mult), reads=rd, writes=[dstbuf], merge_w=True)
        elif mul is None:
            c.op(eng, lambda e: e.tensor_scalar(out=dst_ap, in0=st[:, 0:ncols], scalar1=col_ap[1], scalar2=1.0, op0=ALU.mult, op1=ALU.mult), reads=rd, writes=[dstbuf], merge_w=True)
        else:
            c.op(eng, lambda e: e.tensor_scalar(out=dst_ap, in0=st[:, 0:ncols], scalar1=col_ap[1], scalar2=float(mul), op0=ALU.mult, op1=ALU.mult), reads=rd, writes=[dstbuf], merge_w=True)

    def norm_rstd(xbuf, xap):
        jk = junk_ring.next()
        ss = ss_ring.next()
        rs = rstd_ring.next()
        c.op("act", lambda e: e.activation(out=jk[:, :], in_=xap, func=AF.Square, accum_out=ss[:, 0:1]), reads=[xbuf], writes=[jk, ss])
        c.op("act", lambda e: e.activation(out=rs[:, :], in_=ss[:, :], func=AF.Ln, scale=1.0 / D, bias=EPS), reads=[ss], writes=[rs])
        c.op("act", lambda e: e.activation(out=rs[:, :], in_=rs[:, :], func=AF.Exp, scale=-0.5), reads=[rs], writes=[rs])
        return rs

    def norm_T(xbuf, xap, ptb, dst_buf, dst_ap, eng_scale="dve"):
        rs = norm_rstd(xbuf, xap)
        xn = xn_ring.next()
        c.op(eng_scale, lambda e: e.tensor_scalar(out=xn[:, :], in0=xap, scalar1=rs[:, 0:1], scalar2=None, op0=ALU.mult), reads=[xbuf, rs], writes=[xn])
        pv = pbf(ptb)
        for kc in range(KC):
            c.op("pe", lambda e: e.transpose(out=pv[:, kc * 128:(kc + 1) * 128], in_=xn[:, kc * 128:(kc + 1) * 128], identity=ident[:, :]), reads=[xn, ident], writes=[ptb])
        c.op("act", lambda e: e.activation(out=dst_ap, in_=pv[:, 0:1024].rearrange("p (k t) -> p k t", k=8), func=AF.Identity), reads=[ptb], writes=[dst_buf], merge_w=True)
        return rs

    def load_x(src, row0):
        xs = xs_ring.next()
        c.dma("sp", lambda e: e.dma_start(out=xs[:, :], in_=src[row0:row0 + 128, :]), owner=xs, writes=[xs])
        return xs

    def stop_here(label):
        if cfg.stop == label:
            raise _Stop()

    c.push()
    STb = c.sb("STb", [128, NT, 512], BF16)
    lg = c.sb("lg", [128, 8])
    c.op("act", lambda e: e.activation(out=lg[:, :], in_=decb[:, :], func=AF.Exp, scale=-1.0), reads=[decb], writes=[lg])
    c.op("act", lambda e: e.activation(out=lg[:, :], in_=lg[:, :], func=AF.Ln, bias=1.0), reads=[lg], writes=[lg])
    c.op("dve", lambda e: e.tensor_scalar(out=lg[:, :], in0=lg[:, :], scalar1=-1.0, scalar2=None, op0=ALU.mult), reads=[lg], writes=[lg])
    lgsel = c.sb("lgsel", [128, 4])
    c.op("dve", lambda e: e.tensor_copy(out=lgsel[0:64, :], in_=lg[0:64, 0:4]), reads=[lg], writes=[lgsel], merge_w=True)
    c.op("dve", lambda e: e.tensor_copy(out=lgsel[64:128, :], in_=lg[64:128, 4:8]), reads=[lg], writes=[lgsel], merge_w=True)
    cdec = c.sb("cdec", [128, 4])
    c.op("act", lambda e: e.activation(out=cdec[:, :], in_=lgsel[:, :], func=AF.Exp, scale=128.0), reads=[lgsel], writes=[cdec])

    DT = c.sb("DT", [128, 4, 128])
    kqp = c.sb("kqp", [128, 128])
    kqn = c.sb("kqn", [128, 128])
    mle = c.sb("mle", [128, 128])
    mgt = c.sb("mgt", [128, 128])
    c.op("dve", lambda e: e.tensor_scalar(out=kqp[:, :], in0=kq[:, :], scalar1=0.0, scalar2=None, op0=ALU.max), reads=[kq], writes=[kqp])
    c.op("dve", lambda e: e.tensor_scalar(out=kqn[:, :], in0=kq[:, :], scalar1=-1.0, scalar2=0.0, op0=ALU.mult, op1=ALU.max), reads=[kq], writes=[kqn])
    c.op("dve", lambda e: e.tensor_scalar(out=mle[:, :], in0=kq[:, :], scalar1=0.0, scalar2=None, op0=ALU.is_le), reads=[kq], writes=[mle])
    c.op("dve", lambda e: e.tensor_scalar(out=mgt[:, :], in0=kq[:, :], scalar1=0.0, scalar2=None, op0=ALU.is_gt), reads=[kq], writes=[mgt])
    for h in range(4):
        c.op("act", lambda e: e.activation(out=tmpA[:, :], in_=kqn[:, :], func=AF.Exp, scale=lg[:, h:h + 1]), reads=[kqn, lg], writes=[tmpA])
        c.op("act", lambda e: e.activation(out=tmpB[:, :], in_=kqp[:, :], func=AF.Exp, scale=lg[:, 4 + h:5 + h]), reads=[kqp, lg], writes=[tmpB])
        c.op("dve", lambda e: e.tensor_tensor(out=tmpA[:, :], in0=tmpA[:, :], in1=mle[:, :], op=ALU.mult), reads=[tmpA, mle], writes=[tmpA])
        c.op("dve", lambda e: e.tensor_tensor(out=tmpB[:, :], in0=tmpB[:, :], in1=mgt[:, :], op=ALU.mult), reads=[tmpB, mgt], writes=[tmpB])
        c.op("dve", lambda e: e.tensor_tensor(out=DT[:, h, :], in0=tmpA[:, :], in1=tmpB[:, :], op=ALU.add), reads=[tmpA, tmpB], writes=[DT], merge_w=True)

    expo_i = c.sb("expo_i", [128, 128], I32)
    expo = c.sb("expo", [128, 128])
    DEC = c.sb("DEC", [128, 4, 128])
    c.op("pool", lambda e: e.iota(expo_i[0:64, :], pattern=[[1, 128]], base=1, channel_multiplier=0), writes=[expo_i], merge_w=True)
    c.op("pool", lambda e: e.iota(expo_i[64:128, :], pattern=[[-1, 128]], base=128, channel_multiplier=0), writes=[expo_i], merge_w=True)
    c.op("dve", lambda e: e.tensor_copy(out=expo[:, :], in_=expo_i[:, :]), reads=[expo_i], writes=[expo])
    for h in range(4):
        c.op("act", lambda e: e.activation(out=DEC[:, h, :], in_=expo[:, :], func=AF.Exp, scale=lgsel[:, h:h + 1]), reads=[expo, lgsel], writes=[DEC], merge_w=True)

    pcol_i = c.sb("pcol_i", [128, 2], I32)
    pcol = c.sb("pcol", [128, 2])
    c.op("pool", lambda e: e.iota(pcol_i[:, 0:1], pattern=[[0, 1]], base=127, channel_multiplier=-1), writes=[pcol_i], merge_w=True)
    c.op("pool", lambda e: e.iota(pcol_i[:, 1:2], pattern=[[0, 1]], base=0, channel_multiplier=1), writes=[pcol_i], merge_w=True)
    c.op("dve", lambda e: e.tensor_copy(out=pcol[:, :], in_=pcol_i[:, :]), reads=[pcol_i], writes=[pcol])
    kdec = c.sb("kdec", [128, 4, 2])
    for h in range(4):
        c.op("act", lambda e: e.activation(out=kdec[:, h, 0:1], in_=pcol[:, 0:1], func=AF.Exp, scale=lg[:, h:h + 1]), reads=[pcol, lg], writes=[kdec], merge_w=True)
        c.op("act", lambda e: e.activation(out=kdec[:, h, 1:2], in_=pcol[:, 1:2], func=AF.Exp, scale=lg[:, 4 + h:5 + h]), reads=[pcol, lg], writes=[kdec], merge_w=True)

    CFB = c.sb("CFB", [128, NF, 4, 2])
    dfd = c.sb("dfd", [128, NF])
    dbd = c.sb("dbd", [128, NF])
    mfd = c.sb("mfd", [128, NF])
    mbd = c.sb("mbd", [128, NF])
    tmpF = c.sb("tmpF", [128, NF])
    c.op("dve", lambda e: e.tensor_scalar(out=dfd[:, :], in0=posf_s[:, :], scalar1=-1.0, scalar2=t01_s[:, 0:1], op0=ALU.mult, op1=ALU.add), reads=[posf_s, t01_s], writes=[dfd])
    c.op("dve", lambda e: e.tensor_scalar(out=dfd[:, :], in0=dfd[:, :], scalar1=-1.0, scalar2=None, op0=ALU.add), reads=[dfd], writes=[dfd])
    c.op("dve", lambda e: e.tensor_scalar(out=dbd[:, :], in0=posf_s[:, :], scalar1=t01_s[:, 1:2], scalar2=None, op0=ALU.subtract), reads=[posf_s, t01_s], writes=[dbd])
    c.op("dve", lambda e: e.tensor_scalar(out=mfd[:, :], in0=dfd[:, :], scalar1=0.0, scalar2=None, op0=ALU.is_ge), reads=[dfd], writes=[mfd])
    c.op("dve", lambda e: e.tensor_scalar(out=mbd[:, :], in0=dbd[:, :], scalar1=0.0, scalar2=None, op0=ALU.is_ge), reads=[dbd], writes=[mbd])
    c.op("dve", lambda e: e.tensor_scalar(out=dfd[:, :], in0=dfd[:, :], scalar1=0.0, scalar2=None, op0=ALU.max), reads=[dfd], writes=[dfd])
    c.op("dve", lambda e: e.tensor_scalar(out=dbd[:, :], in0=dbd[:, :], scalar1=0.0, scalar2=None, op0=ALU.max), reads=[dbd], writes=[dbd])
    for h in range(4):
        c.op("act", lambda e: e.activation(out=tmpF[:, :], in_=dfd[:, :], func=AF.Exp, scale=lg[:, h:h + 1]), reads=[dfd, lg], writes=[tmpF])
        c.op("dve", lambda e: e.tensor_tensor(out=CFB[:, :, h, 0], in0=tmpF[:, :], in1=mfd[:, :], op=ALU.mult), reads=[tmpF, mfd], writes=[CFB], merge_w=True)
        c.op("act", lambda e: e.activation(out=tmpF[:, :], in_=dbd[:, :], func=AF.Exp, scale=lg[:, 4 + h:5 + h]), reads=[dbd, lg], writes=[tmpF])
        c.op("dve", lambda e: e.tensor_tensor(out=CFB[:, :, h, 1], in0=tmpF[:, :], in1=mbd[:, :], op=ALU.mult), reads=[tmpF, mbd], writes=[CFB], merge_w=True)

    c.push()
    Wf = c.sb("Wf", [128, 8, 768], BF16)
    for kc in range(KC):
        wcast(Wf[:, kc, 0:256], w_in[kc * 128:(kc + 1) * 128, 1792:2048], 256, col_ap=(nwmix, nwmix[:, kc:kc + 1]), mul=0.125, dstbuf=Wf)
        wcast(Wf[:, kc, 256:768], w_in[kc * 128:(kc + 1) * 128, 2048:2560], 512, col_ap=(nwmix, nwmix[:, kc:kc + 1]), dstbuf=Wf)
    hT_ring = c.ring("hTt", 3, [128, 8, 128], BF16)
    kfb_ring = c.ring("kfb", 2, [128, 4, 128], BF16)
    vb_ring = c.ring("vb", 2, [128, 512], BF16)
    CKV = c.sb("CKV", [128, NT, 512], F32)
    Sin = c.sb("Sin", [128, 512], F32)
    PT = [P[0], P[1]]
    PA = [P[2], P[3]]
    PB = [P[4], P[5]]
    PS = P[6]
    PC = [P[6], P[7]]
    c.op("pe", lambda e: e.matmul(PS[:, 0:512], lhsT=zeros_bf[:, :], rhs=zrow[:, :], start=True, stop=False), reads=[zeros_bf, zrow], writes=[PS])
    def f_front(it):
        far = it < NF
        if far:
            xs = load_x(xf, it * 128)
        else:
            xs = load_x(xa, (8 + it - NF) * 128)
        hT = hT_ring.next()
        norm_T(xs, xs[:, :], PT[it % 2], hT, hT[:, :, :])
        return hT

    def f_back(it, hT):
        far = it < NF
        if far:
            coef = CFB[:, it, :, :]
            coefb = CFB
        else:
            n = it - NF
            coef = kdec[:, :, :]
            coefb = kdec
        if it == NF:
            c.op("pe", lambda e: e.matmul(PS[:, 0:512], lhsT=zeros_bf[:, :], rhs=zrow[:, :], start=False, stop=True), reads=[zeros_bf, zrow], writes=[PS])
            c.op("dve", lambda e: e.tensor_copy(out=Sin[:, :], in_=PS[:, 0:512]), reads=[PS], writes=[Sin])
        pa, pb = PA[it % 2], PB[it % 2]
        for kc in range(KC):
            c.op("pe", lambda e: e.matmul(pa[:, 0:256], lhsT=hT[:, kc, :], rhs=Wf[:, kc, 0:256], start=(kc == 0), stop=(kc == KC - 1)), reads=[hT, Wf], writes=[pa])
        for kc in range(KC):
            c.op("pe", lambda e: e.matmul(pb[:, 0:512], lhsT=hT[:, kc, :], rhs=Wf[:, kc, 256:768], start=(kc == 0), stop=(kc == KC - 1)), reads=[hT, Wf], writes=[pb])
        kfb = kfb_ring.next()
        vb = vb_ring.next()
        kview = pa[:, 0:256].rearrange("p (h d) -> p h d", h=4)
        c.op("dve", lambda e: e.tensor_tensor(out=kfb[:, :, 0:64], in0=kview, in1=coef[:, :, 0:1].to_broadcast([128, 4, 64]), op=ALU.mult), reads=[pa, coefb], writes=[kfb], merge_w=True)
        c.op("dve", lambda e: e.tensor_tensor(out=kfb[:, :, 64:128], in0=kview, in1=coef[:, :, 1:2].to_broadcast([128, 4, 64]), op=ALU.mult), reads=[pa, coefb], writes=[kfb], merge_w=True)
        c.op("act", lambda e: e.activation(out=vb[:, :], in_=pb[:, 0:512], func=AF.Identity), reads=[pb], writes=[vb])
        if far:
            for h in range(4):
                c.op("pe", lambda e: e.matmul(PS[:, h * 128:(h + 1) * 128], lhsT=kfb[:, h, :], rhs=vb[:, h * 128:(h + 1) * 128], start=False, stop=False), reads=[kfb, vb], writes=[PS])
        else:
            pc = PC[n % 2]
            for h in range(4):
                c.op("pe", lambda e: e.matmul(pc[:, h * 128:(h + 1) * 128], lhsT=kfb[:, h, :], rhs=vb[:, h * 128:(h + 1) * 128], start=True, stop=True), reads=[kfb, vb], writes=[pc])
            c.op("dve", lambda e: e.tensor_copy(out=CKV[:, n, :], in_=pc[:, 0:512]), reads=[pc], writes=[CKV], merge_w=True)

    f_hts = {0: f_front(0)}
    for it in range(NF + NT):
        if it + 1 < NF + NT:
            f_hts[it + 1] = f_front(it + 1)
        f_back(it, f_hts.pop(it))

    Rf = c.sb("Rf", [128, 512], F32)
    c.op("dve", lambda e: e.tensor_copy(out=Rf[:, :], in_=Sin[:, :]), reads=[Sin], writes=[Rf])
    Rlo = Buf("Rlo")
    Rhi = Buf("Rhi")
    Rlo.w = dict(Rf.w)
    Rhi.w = dict(Rf.w)
    for s in range(NT):
        nf_, nb_ = s, NT - 1 - s
        c.op("act", lambda e: e.activation(out=STb[0:64, nf_, :], in_=Rf[0:64, :], func=AF.Identity), reads=[Rlo], writes=[STb], merge_w=True)
        c.op("pool", lambda e: e.tensor_copy(out=STb[64:128, nb_, :], in_=Rf[64:128, :]), reads=[Rhi], writes=[STb], merge_w=True)
        for h in range(4):
            sl = slice(h * 128, (h + 1) * 128)
            c.op("dve", lambda e: e.scalar_tensor_tensor(out=Rf[0:64, sl], in0=Rf[0:64, sl], scalar=cdec[0:64, h:h + 1], in1=CKV[0:64, nf_, sl], op0=ALU.mult, op1=ALU.add), reads=[Rlo, cdec, CKV], writes=[Rlo])
            c.op("dve", lambda e: e.scalar_tensor_tensor(out=Rf[64:128, sl], in0=Rf[64:128, sl], scalar=cdec[64:128, h:h + 1], in1=CKV[64:128, nb_, sl], op0=ALU.mult, op1=ALU.add), reads=[Rhi, cdec, CKV], writes=[Rhi])
    phases["F"] = (c.ninst, c.nwait)

    if cfg.debug:
        c.dma("sp", lambda e: e.dma_start(out=DBG_d[:, 0:512], in_=Sin[:, :]), owner=Sin, reads=[Sin], writes=[DBG_d], merge_w=True)
        c.dma("sp", lambda e: e.dma_start(out=DBG_d[:, 512:1024], in_=CKV[:, 0, :]), owner=CKV, reads=[CKV], writes=[DBG_d], merge_w=True)
    if cfg.stop == "F":
        c.finish()
        c.pop_all()
        return nc, c, phases
    c.pop()

    NBO = NT // 4
    c.push()
    Wr = c.sb("Wr", [128, 8, 1792], BF16)
    for kc in range(KC):
        rows = slice(kc * 128, (kc + 1) * 128)
        st = wst_ring.next()
        c.dma("sp", lambda e: e.dma_start(out=st[:, 0:256], in_=w_in[rows, 1536:1792]), owner=st, writes=[st])
        dup = Wr[:, kc, 0:512].rearrange("p (h t d) -> p h t d", h=4, t=2)
        for two in range(2):
            c.op(("dve", "pool")[two], lambda e: e.tensor_scalar(out=dup[:, :, two, :], in0=st[:, 0:256].rearrange("p (h d) -> p h d", h=4), scalar1=nwmix[:, kc:kc + 1], scalar2=1.0, op0=ALU.mult, op1=ALU.mult), reads=[st, nwmix], writes=[Wr], merge_w=True)
        wcast(Wr[:, kc, 512:768], w_in[rows, 1792:2048], 256, col_ap=(nwmix, nwmix[:, kc:kc + 1]), mul=0.125, dstbuf=Wr)
        wcast(Wr[:, kc, 768:1280], w_in[rows, 2048:2560], 512, col_ap=(nwmix, nwmix[:, kc:kc + 1]), dstbuf=Wr)
        wcast(Wr[:, kc, 1280:1792], w_in[rows, 2560:3072], 512, col_ap=(nwmix, nwmix[:, kc:kc + 1]), dstbuf=Wr)
    GW = c.sb("GW", [128, 512], F32)
    c.dma("sp", lambda e: e.dma_start(out=GW[:, :], in_=gn_w[0:1, :].to_broadcast([128, 512])), owner=GW, writes=[GW])
    stop_here("R0")
    hTb_ring = c.ring("hTb", 2, [128, 8, 512], BF16)
    qfb_ring = c.ring("qfb", 2, [128, 4, 512], BF16)
    qpl_ring = c.ring("qpl", 2, [128, 4, 512], BF16)
    ktr_ring = c.ring("ktr", 2, [128, 2, 512], BF16)
    vb2_ring = c.ring("vb2", 2, [128, 512], BF16)
    gs_ring = c.ring("gs", 2, [128, 512], F32)
    gg_ring = c.ring("gg", 2, [128, 512], F32)
    pts_ring = c.ring("pts", 2, [128, 4, 128], BF16)
    ysq_ring = c.ring("ysq", 2, [128, 512], F32)
    yn_ring = c.ring("yn", 2, [128, 512], F32)
    ro_ring = c.ring("ro", 2, [128, 512], BF16)
    rts_ring = c.ring("rts", 2, [128, 4, 512], BF16)
    st_ring = c.ring("gst", 16, [128, 4], F32)
    pring = Ring([P[2], P[3], P[4]])
    PAt2, PY, PRT = [P[6], P[7]], P[5], P[0]
    def r_front(b):
        hTb = hTb_ring.next()
        for ti in range(4):
            xs = load_x(xa, (8 + 4 * b + ti) * 128)
            norm_T(xs, xs[:, :], PT[ti % 2], hTb, hTb[:, :, ti * 128:(ti + 1) * 128])
        return hTb

    r_pre = {0: r_front(0)}
    for b in range(NBO):
        if b + 1 < NBO:
            r_pre[b + 1] = r_front(b + 1)
        hTb = r_pre.pop(b)
        stop_here("R1")
        qfb, qpl, ktr = qfb_ring.next(), qpl_ring.next(), ktr_ring.next()
        for h in range(4):
            hb = 64 * (h % 2)
            pq = pring.next()
            for kc in range(KC):
                c.op("pe", lambda e: e.matmul(pq[:, 0:512], lhsT=Wr[:, kc, h * 128:(h + 1) * 128], rhs=hTb[:, kc, :], start=(kc == 0), stop=(kc == KC - 1)), reads=[Wr, hTb], writes=[pq])
            stop_here("R1a")
            c.op("dve", lambda e: e.tensor_tensor(out=qfb[:, h, :].rearrange("p (a i) -> p a i", a=4), in0=pq[:, 0:512].rearrange("p (a i) -> p a i", a=4), in1=DEC[:, h:h + 1, :].to_broadcast([128, 4, 128]), op=ALU.mult), reads=[pq, DEC], writes=[qfb], merge_w=True)
            stop_here("R1b")
            c.op("dve", lambda e: e.tensor_copy(out=qpl[hb:hb + 64, h, :], in_=pq[hb:hb + 64, 0:512]), reads=[pq], writes=[qpl], merge_w=True)
            stop_here("R1c")
        for pr in range(2):
            pk = pring.next()
            for kc in range(KC):
                c.op("pe", lambda e: e.matmul(pk[:, 0:512], lhsT=Wr[:, kc, 512 + pr * 128:512 + (pr + 1) * 128], rhs=hTb[:, kc, :], start=(kc == 0), stop=(kc == KC - 1)), reads=[Wr, hTb], writes=[pk])
            c.op("act", lambda e: e.activation(out=ktr[:, pr, :], in_=pk[:, 0:512], func=AF.Identity), reads=[pk], writes=[ktr], merge_w=True)
        stop_here("R2")
        rts = rts_ring.next()
        for ti in range(4):
            n = 4 * b + ti
            tsl = slice(ti * 128, (ti + 1) * 128)
            pv = pring.next()
            for kc in range(KC):
                c.op("pe", lambda e: e.matmul(pv[:, 0:512], lhsT=hTb[:, kc, tsl], rhs=Wr[:, kc, 768:1280], start=(kc == 0), stop=(kc == KC - 1)), reads=[Wr, hTb], writes=[pv])
            vb2 = vb2_ring.next()
            c.op("act", lambda e: e.activation(out=vb2[:, :], in_=pv[:, 0:512], func=AF.Identity), reads=[pv], writes=[vb2])
            pg = pring.next()
            for kc in range(KC):
                c.op("pe", lambda e: e.matmul(pg[:, 0:512], lhsT=hTb[:, kc, tsl], rhs=Wr[:, kc, 1280:1792], start=(kc == 0), stop=(kc == KC - 1)), reads=[Wr, hTb], writes=[pg])
            gs = gs_ring.next()
            c.op("act", lambda e: e.activation(out=gs[:, :], in_=pg[:, 0:512], func=AF.Exp, scale=-1.0), reads=[pg], writes=[gs])
            c.op("act", lambda e: e.activation(out=gs[:, :], in_=gs[:, :], func=AF.Ln, bias=1.0), reads=[gs], writes=[gs])
            c.op("act", lambda e: e.activation(out=gs[:, :], in_=gs[:, :], func=AF.Exp, scale=-1.0), reads=[gs], writes=[gs])
            c.op("pool", lambda e: e.tensor_tensor(out=gs[:, :], in0=gs[:, :], in1=GW[:, :], op=ALU.mult), reads=[gs, GW], writes=[gs])
            gg = gg_ring.next()
            c.op("dve", lambda e: e.tensor_tensor(out=gg[:, :], in0=pg[:, 0:512], in1=gs[:, :], op=ALU.mult), reads=[pg, gs], writes=[gg])
            stop_here("R3")
            for h in range(4):
                hb = 64 * (h % 2)
                pat = PAt2[h % 2]
                c.op("pe", lambda e: e.matmul(pat[:, (h // 2) * 128:(h // 2 + 1) * 128], lhsT=ktr[hb:hb + 64, h // 2, tsl], rhs=qpl[hb:hb + 64, h, tsl], start=True, stop=True), reads=[ktr, qpl], writes=[pat])
            stop_here("R3a")
            pts = pts_ring.next()
            ptv = pts[:, :, :].rearrange("p (a two) i -> p a two i", two=2)
            dtv = DT[:, :, :].rearrange("p (a two) i -> p a two i", two=2)
            for par in range(2):
                c.op("dve", lambda e: e.tensor_tensor(out=ptv[:, :, par, :], in0=PAt2[par][:, 0:256].rearrange("p (a i) -> p a i", a=2), in1=dtv[:, :, par, :], op=ALU.mult), reads=[PAt2[par], DT], writes=[pts], merge_w=(par == 1))
            stop_here("R3b")
            for h in range(4):
                hs = slice(h * 128, (h + 1) * 128)
                c.op("pe", lambda e: e.matmul(PY[:, hs], lhsT=pts[:, h, :], rhs=vb2[:, hs], start=True, stop=False), reads=[pts, vb2], writes=[PY])
                c.op("pe", lambda e: e.matmul(PY[:, hs], lhsT=qfb[:, h, tsl], rhs=STb[:, n, hs], start=False, stop=True), reads=[qfb, STb], writes=[PY])
            stop_here("R4")
            s1, s2, mu, msq, var, sd, nmr = [st_ring.next() for _ in range(7)]
            pyv = PY[:, 0:512].rearrange("p (h e) -> p h e", h=4)
            c.op("dve", lambda e: e.tensor_reduce(out=s1[:, :], in_=pyv, axis=AX.X, op=ALU.add), reads=[PY], writes=[s1])
            stop_here("R4a")
            ysq = ysq_ring.next()
            c.op("act", lambda e: e.activation(out=ysq[:, :], in_=PY[:, 0:512], func=AF.Square), reads=[PY], writes=[ysq])
            stop_here("R4a2")
            c.op("dve", lambda e: e.tensor_reduce(out=s2[:, :], in_=ysq[:, :].rearrange("p (h e) -> p h e", h=4), axis=AX.X, op=ALU.add), reads=[ysq], writes=[s2])
            stop_here("R4b")
            c.op("dve", lambda e: e.tensor_scalar(out=mu[:, :], in0=s1[:, :], scalar1=1.0 / 128.0, scalar2=None, op0=ALU.mult), reads=[s1], writes=[mu])
            c.op("dve", lambda e: e.tensor_tensor(out=msq[:, :], in0=mu[:, :], in1=mu[:, :], op=ALU.mult), reads=[mu], writes=[msq])
            c.op("dve", lambda e: e.scalar_tensor_tensor(out=var[:, :], in0=s2[:, :], scalar=1.0 / 128.0, in1=msq[:, :], op0=ALU.mult, op1=ALU.subtract), reads=[s2, msq], writes=[var])
            c.op("act", lambda e: e.activation(out=sd[:, :], in_=var[:, :], func=AF.Ln, bias=GN_EPS), reads=[var], writes=[sd])
            c.op("act", lambda e: e.activation(out=sd[:, :], in_=sd[:, :], func=AF.Exp, scale=-0.5), reads=[sd], writes=[sd])
            c.op("dve", lambda e: e.scalar_tensor_tensor(out=nmr[:, :], in0=mu[:, :], scalar=-1.0, in1=sd[:, :], op0=ALU.mult, op1=ALU.mult), reads=[mu, sd], writes=[nmr])
            stop_here("R4c")
            yn = yn_ring.next()
            stop_here("R4c2")
            for h in range(4):
                hs = slice(h * 128, (h + 1) * 128)
                c.op("act", lambda e: e.activation(out=yn[:, hs], in_=PY[:, hs], func=AF.Identity, scale=sd[:, h:h + 1], bias=nmr[:, h:h + 1]), reads=[PY, sd, nmr], writes=[yn], merge_w=True)
            ro = ro_ring.next()
            c.op("dve", lambda e: e.tensor_tensor(out=ro[:, :], in0=yn[:, :], in1=gg[:, :], op=ALU.mult), reads=[yn, gg], writes=[ro])
            stop_here("R5")
            prt = pbf(PRT)
            for h in range(4):
                hs = slice(h * 128, (h + 1) * 128)
                c.op("pe", lambda e: e.transpose(out=prt[:, hs], in_=ro[:, hs], identity=ident[:, :]), reads=[ro, ident], writes=[PRT])
            c.op("act", lambda e: e.activation(out=rts[:, :, tsl], in_=prt[:, 0:512].rearrange("p (h t) -> p h t", h=4), func=AF.Identity), reads=[PRT], writes=[rts], merge_w=True)
        stop_here("R6")
        c.dma("pool", lambda e: e.dma_start(out=MIX_d.t[4:8, :, b * 512:(b + 1) * 512].rearrange("h p t -> p h t"), in_=rts[:, :, :]), owner=rts, reads=[rts], writes=[MIX_d], merge_w=True)
        stop_here("R7")
    phases["R"] = (c.ninst, c.nwait)
    if cfg.stop == "R":
        c.finish([MIX_d])
        c.pop_all()
        return nc, c, phases
    c.pop()
    c.pop()

    c.push()
    Wa = c.sb("Wa", [128, 8, 1536], BF16)
    for kc in range(KC):
        rows = slice(kc * 128, (kc + 1) * 128)
        for part in range(3):
            wcast(Wa[:, kc, part * 512:(part + 1) * 512], w_in[rows, part * 512:(part + 1) * 512], 512, col_ap=(nwmix, nwmix[:, kc:kc + 1]), dstbuf=Wa)
    hTa_ring = c.ring("hTa", 2, [128, 8, 512], BF16)
    sq_ring = c.ring("sqa", 2, [128, 512], BF16)
    ln_ring = c.ring("lna", 2, [128, 512], F32)
    stg_ring = c.ring("stga", 3, [128, 512], BF16)
    pring = Ring([P[2], P[3], P[4]])
    pssr = Ring([P[5], P[6]])
    NBA = NA // 4
    cnt = 0
    def a_front(b):
        hTb = hTa_ring.next()
        for ti in range(4):
            xs = load_x(xa, (4 * b + ti) * 128)
            norm_T(xs, xs[:, :], PT[ti % 2], hTb, hTb[:, :, ti * 128:(ti + 1) * 128])
        return hTb

    a_pre = {0: a_front(0)}
    for b in range(NBA):
        own = 2 <= b < NBA - 2
        if b + 1 < NBA:
            a_pre[b + 1] = a_front(b + 1)
        hTb = a_pre.pop(b)
        kinds = [("k", pr) for pr in range(4)] + [("v", pr) for pr in range(4)]
        if own:
            kinds += [("q", pr) for pr in range(4)]
        for kind, pr in kinds:
            col0 = {"q": 0, "k": 512, "v": 1024}[kind] + pr * 128
            pq = pring.next()
            for kc in range(KC):
                c.op("pe", lambda e: e.matmul(pq[:, 0:512], lhsT=Wa[:, kc, col0:col0 + 128], rhs=hTb[:, kc, :], start=(kc == 0), stop=(kc == KC - 1)), reads=[Wa, hTb], writes=[pq])
            stg = stg_ring.next()
            if kind == "v":
                eng = ("act", "dve")[cnt % 2]
                cnt += 1
                if eng == "act":
                    c.op("act", lambda e: e.activation(out=stg[:, :], in_=pq[:, 0:512], func=AF.Identity), reads=[pq], writes=[stg])
                else:
                    c.op("dve", lambda e: e.tensor_copy(out=stg[:, :], in_=pq[:, 0:512]), reads=[pq], writes=[stg])
                dst = VT_d.t[pr, :, b * 512:(b + 1) * 512]
                dbuf = VT_d
            else:
                sq = sq_ring.next()
                c.op("act", lambda e: e.activation(out=sq[:, :], in_=pq[:, 0:512], func=AF.Square), reads=[pq], writes=[sq])
                pss = pssr.next()
                c.op("pe", lambda e: e.matmul(pss[:, 0:512], lhsT=blk1[:, :], rhs=sq[:, :], start=True, stop=True), reads=[blk1, sq], writes=[pss])
                ln = ln_ring.next()
                c.op("act", lambda e: e.activation(out=ln[:, :], in_=pss[:, 0:512], func=AF.Ln, scale=1.0 / 64.0, bias=EPS), reads=[pss], writes=[ln])
                c.op("act", lambda e: e.activation(out=ln[:, :], in_=ln[:, :], func=AF.Exp, scale=-0.5), reads=[ln], writes=[ln])
                wcol = aqw if kind == "q" else akw
                c.op("dve", lambda e: e.scalar_tensor_tensor(out=stg[:, :], in0=pq[:, 0:512], scalar=wcol[:, 0:1], in1=ln[:, :], op0=ALU.mult, op1=ALU.mult), reads=[pq, wcol, ln], writes=[stg])
                if kind == "q":
                    dst = QT_d.t[pr, :, (b - 2) * 512:(b - 1) * 512]
                    dbuf = QT_d
                else:
                    dst = KT_d.t[pr, :, b * 512:(b + 1) * 512]
                    dbuf = KT_d
            c.dma("pool", lambda e: e.dma_start(out=dst, in_=stg[:, :]), owner=stg, reads=[stg], writes=[dbuf], merge_w=True)
    phases["A1"] = (c.ninst, c.nwait)
    if cfg.stop == "A1":
        c.finish([QT_d, KT_d, VT_d])
        c.pop_all()
        return nc, c, phases
    c.pop()

    c.push()
    BIAS = c.sb("BIAS", [128, 24, 256], BF16)
    c.op("act", lambda e: e.activation(out=tmpA[:, :], in_=kq[:, :], func=AF.Abs, bias=-64.0), reads=[kq], writes=[tmpA])
    c.op("act", lambda e: e.activation(out=tmpB[:, :], in_=kq[:, :], func=AF.Abs, bias=64.0), reads=[kq], writes=[tmpB])
    c.op("dve", lambda e: e.tensor_scalar(out=tmpC[:, :], in0=kq[:, :], scalar1=0.0, scalar2=None, op0=ALU.is_ge), reads=[kq], writes=[tmpC])
    c.op("dve", lambda e: e.tensor_scalar(out=tmpC[:, :], in0=tmpC[:, :], scalar1=-1.0, scalar2=-NEG, op0=ALU.add, op1=ALU.mult), reads=[tmpC], writes=[tmpC])
    c.op("dve", lambda e: e.tensor_scalar(out=tmpD[:, :], in0=kq[:, :], scalar1=0.0, scalar2=None, op0=ALU.is_le), reads=[kq], writes=[tmpD])
    c.op("dve", lambda e: e.tensor_scalar(out=tmpD[:, :], in0=tmpD[:, :], scalar1=-1.0, scalar2=-NEG, op0=ALU.add, op1=ALU.mult), reads=[tmpD], writes=[tmpD])
    for h in range(8):
        for pi, d in enumerate(PATTERNS):
            sc = -SLOPES[h] * d
            ix = h * 3 + pi
            c.op("dve", lambda e: e.scalar_tensor_tensor(out=BIAS[:, ix, 0:128], in0=tmpA[:, :], scalar=sc, in1=tmpC[:, :], op0=ALU.mult, op1=ALU.add), reads=[tmpA, tmpC], writes=[BIAS], merge_w=True)
            c.op("dve", lambda e: e.scalar_tensor_tensor(out=BIAS[:, ix, 128:256], in0=tmpB[:, :], scalar=sc, in1=tmpD[:, :], op0=ALU.mult, op1=ALU.add), reads=[tmpB, tmpD], writes=[BIAS], merge_w=True)

    valt_f = c.sb("valt_f", [128, cfg.NCHT], F32)
    VALT = c.sb("VALT", [128, cfg.NCHT, 64], BF16)
    c.dma("sp", lambda e: e.dma_start(out=valt_f[:, :], in_=valt_d[:, :]), owner=valt_f, writes=[valt_f])
    c.op("dve", lambda e: e.tensor_copy(out=VALT[:, :, :], in_=valt_f[:, :].unsqueeze(2).to_broadcast([128, cfg.NCHT, 64])), reads=[valt_f], writes=[VALT])
    qts_ring = c.ring("qts", 2, [128, LS], BF16)
    kts_ring = c.ring("kts", 2, [128, NA * 128], BF16)
    vts_ring = c.ring("vts", 2, [128, NA * 128], BF16)
    NUM = c.sb("NUM", [128, LS], F32)
    DEN = c.sb("DEN", [128, LS], F32)
    ATS = c.sb("ATS", [128, LS], BF16)
    es_ring = c.ring("es", 4, [128, 512], BF16)
    vpc_ring = c.ring("vpc", 4, [128, 128], BF16)
    Sring = Ring([P[0], P[1], P[2]])
    ONr = Ring([P[3], P[4]])
    ODr = Ring([P[5], P[6]])
    PVT = P[7]
    for pr in range(4):
        qts, kts, vts = qts_ring.next(), kts_ring.next(), vts_ring.next()
        c.dma("sp", lambda e: e.dma_start(out=qts[:, :], in_=QT_d.t[pr, :, :]), owner=qts, reads=[QT_d], writes=[qts])
        c.dma("sp", lambda e: e.dma_start(out=kts[:, :], in_=KT_d.t[pr, :, :]), owner=kts, reads=[KT_d], writes=[kts])
        c.dma("sp", lambda e: e.dma_start(out=vts[:, :], in_=VT_d.t[pr, :, :]), owner=vts, reads=[VT_d], writes=[vts])
        c.op("pool", lambda e: e.memset(NUM[:, :], 0.0), writes=[NUM])
        c.op("pool", lambda e: e.memset(DEN[:, :], 0.0), writes=[DEN])
        for pi, d in enumerate(PATTERNS):
            nq = cfg.nq[pi]
            nch = cfg.nch[pi]
            for r in range(d):
                vcache = {}

                def kslice(ch):
                    k0 = d * (128 * ch - 64) + r + 1024
                    return slice(k0, k0 + 127 * d + 1, d)

                def getv(ch):
                    if ch not in vcache:
                        vc = vpc_ring.next()
                        pv = pbf(PVT)
                        c.op("pe", lambda e: e.transpose(out=pv[:, 0:128], in_=vts[:, kslice(ch)], identity=ident[:, :]), reads=[vts, ident], writes=[PVT])
                        c.op("dve", lambda e: e.tensor_copy(out=vc[:, :], in_=pv[:, 0:128]), reads=[PVT], writes=[vc])
                        vcache[ch] = vc
                    return vcache[ch]
                for mm in range(nq // 2):
                    m0 = 2 * mm
                    E = []
                    for h2 in range(2):
                        hb = 64 * h2
                        hg = 2 * pr + h2
                        ps = Sring.next()
                        ix = hg * 3 + pi
                        c.op("pe", lambda e: e.matmul(ps[:, 0:512].rearrange("p (a n) -> p a n", a=2), lhsT=ident[:, :], rhs=BIAS[:, ix:ix + 1, :].to_broadcast([128, 2, 256]), start=True, stop=False), reads=[ident, BIAS], writes=[ps])
                        for blk, (tq, ch) in enumerate([(m0, m0), (m0, m0 + 1), (m0 + 1, m0 + 1), (m0 + 1, m0 + 2)]):
                            q0 = d * 128 * tq + r
                            c.op("pe", lambda e: e.matmul(ps[:, blk * 128:(blk + 1) * 128], lhsT=kts[hb:hb + 64, kslice(ch)], rhs=qts[hb:hb + 64, q0:q0 + 127 * d + 1:d], start=False, stop=(blk == 3)), reads=[kts, qts], writes=[ps])
                        es = es_ring.next()
                        c.op("act", lambda e: e.activation(out=es[:, :], in_=ps[:, 0:512], func=AF.Exp), reads=[ps], writes=[es])
                        E.append(es)
                    on, od = ONr.next(), ODr.next()
                    for h2 in range(2):
                        hb = 64 * h2
                        for tqi in range(2):
                            osl = slice(tqi * 128, (tqi + 1) * 128)
                            for half, ch in enumerate((m0 + tqi, m0 + tqi + 1)):
                                blk = 2 * tqi + half
                                vc = getv(ch)
                                c.op("pe", lambda e: e.matmul(on[hb:hb + 64, osl], lhsT=vc[:, hb:hb + 64], rhs=E[h2][:, blk * 128:(blk + 1) * 128], start=(half == 0), stop=(half == 1)), reads=[vc, E[h2]], writes=[on])
                            for half, ch in enumerate((m0 + tqi, m0 + tqi + 1)):
                                blk = 2 * tqi + half
                                vix = cfg.chbase[pi] + r * nch + ch
                                c.op("pe", lambda e: e.matmul(od[hb:hb + 64, osl], lhsT=VALT[:, vix, :], rhs=E[h2][:, blk * 128:(blk + 1) * 128], start=(half == 0), stop=(half == 1)), reads=[VALT, E[h2]], writes=[od])
                    p0 = d * 128 * m0 + r
                    psl = slice(p0, p0 + 255 * d + 1, d)
                    c.op("dve", lambda e: e.tensor_tensor(out=NUM[:, psl], in0=NUM[:, psl], in1=on[:, 0:256], op=ALU.add), reads=[NUM, on], writes=[NUM])
                    c.op("dve", lambda e: e.tensor_tensor(out=DEN[:, psl], in0=DEN[:, psl], in1=od[:, 0:256], op=ALU.add), reads=[DEN, od], writes=[DEN])
        c.op("act", lambda e: e.activation(out=DEN[:, :], in_=DEN[:, :], func=AF.Ln), reads=[DEN], writes=[DEN])
        c.op("act", lambda e: e.activation(out=DEN[:, :], in_=DEN[:, :], func=AF.Exp, scale=-1.0), reads=[DEN], writes=[DEN])
        for q4 in range(LS // 1024):
            qs = slice(q4 * 1024, (q4 + 1) * 1024)
            c.op(("dve", "pool")[q4 % 2], lambda e: e.tensor_tensor(out=ATS[:, qs], in0=NUM[:, qs], in1=DEN[:, qs], op=ALU.mult), reads=[NUM, DEN], writes=[ATS], merge_w=True)
        c.dma("pool", lambda e: e.dma_start(out=MIX_d.t[pr, :, :], in_=ATS[:, :]), owner=ATS, reads=[ATS], writes=[MIX_d], merge_w=True)
    phases["A2"] = (c.ninst, c.nwait)
    if cfg.stop == "A2":
        c.finish([MIX_d])
        c.pop_all()
        return nc, c, phases
    c.pop()

    OHs = c.sb("OHs", [128, NT, 32], F32)
    GTs = c.sb("GTs", [128, NT, 32], F32)
    c.push()
    Wout = c.sb("Wout", [128, 8, 1024], BF16)
    Wmq = c.sb("Wmq", [128, 8, 1024], BF16)
    Wmo = c.sb("Wmo", [128, 8, 1024], BF16)
    for kc in range(KC):
        rows = slice(kc * 128, (kc + 1) * 128)
        wcast(Wout[:, kc, :], w_out[rows, :], 1024, dstbuf=Wout)
        wcast(Wmq[:, kc, :], w_mq[rows, :], 1024, col_ap=(nwmem, nwmem[:, kc:kc + 1]), dstbuf=Wmq)
        wcast(Wmo[:, kc, :], w_mo[rows, :], 1024, dstbuf=Wmo)
    Wrt = c.sb("Wrt", [128, 8, 36], F32)
    c.dma("sp", lambda e: e.dma_start(out=Wrt[:, :, :], in_=w_rt.t.ap().rearrange("(kc p) n -> p kc n", p=128)), owner=Wrt, writes=[Wrt])
    WMOE = c.sb("WMOE", [128, D], F32)
    c.dma("sp", lambda e: e.dma_start(out=WMOE[:, :], in_=nw_moe[0:1, :].to_broadcast([128, D])), owner=WMOE, writes=[WMOE])
    KmT = c.sb("KmT", [128, 8, 256], BF16)
    Vm = c.sb("Vm", [128, 2, 1024], BF16)
    sqm_ring = c.ring("sqm", 2, [128, 512], BF16)
    rsm_ring = c.ring("rsm", 2, [128, 512], F32)
    c.push()
    Wmk = c.sb("Wmk", [128, 8, 2048], BF16)
    for kc in range(KC):
        rows = slice(kc * 128, (kc + 1) * 128)
        for half in range(2):
            wcast(Wmk[:, kc, half * 1024:(half + 1) * 1024], w_mkv[rows, half * 1024:(half + 1) * 1024], 1024, col_ap=(nwmemkv, nwmemkv[:, kc:kc + 1]), dstbuf=Wmk)
    mhT = c.sb("mhT", [128, 8, 256], BF16)
    for mt in range(2):
        xs = load_x(memx, mt * 128)
        norm_T(xs, xs[:, :], PT[mt], mhT, mhT[:, :, mt * 128:(mt + 1) * 128])
    for h in range(4):
        for dc in range(2):
            cidx = 2 * h + dc
            pk = P[2 + dc]
            for kc in range(KC):
                c.op("pe", lambda e: e.matmul(pk[:, 0:256], lhsT=Wmk[:, kc, cidx * 128:(cidx + 1) * 128], rhs=mhT[:, kc, :], start=(kc == 0), stop=(kc == KC - 1)), reads=[Wmk, mhT], writes=[pk])
            sqm = sqm_ring.next()
            c.op("act", lambda e: e.activation(out=sqm[:, 0:256], in_=pk[:, 0:256], func=AF.Square), reads=[pk], writes=[sqm])
            c.op("pe", lambda e: e.matmul(P[4][:, 0:256], lhsT=ones_bf[:, :], rhs=sqm[:, 0:256], start=(dc == 0), stop=(dc == 1)), reads=[ones_bf, sqm], writes=[P[4]])
        rsm = rsm_ring.next()
        c.op("act", lambda e: e.activation(out=rsm[:, 0:256], in_=P[4][:, 0:256], func=AF.Ln, scale=1.0 / 256.0, bias=EPS), reads=[P[4]], writes=[rsm])
        c.op("act", lambda e: e.activation(out=rsm[:, 0:256], in_=rsm[:, 0:256], func=AF.Exp, scale=-0.5), reads=[rsm], writes=[rsm])
        for dc in range(2):
            c.op("dve", lambda e: e.scalar_tensor_tensor(out=KmT[:, 2 * h + dc, :], in0=P[2 + dc][:, 0:256], scalar=mkw[:, dc:dc + 1], in1=rsm[:, 0:256], op0=ALU.mult, op1=ALU.mult), reads=[P[2 + dc], mkw, rsm], writes=[KmT], merge_w=True)
    for mc in range(2):
        for half in range(2):
            pv = P[5 + half]
            for kc in range(KC):
                c.op("pe", lambda e: e.matmul(pv[:, 0:512], lhsT=mhT[:, kc, mc * 128:(mc + 1) * 128], rhs=Wmk[:, kc, 1024 + half * 512:1024 + (half + 1) * 512], start=(kc == 0), stop=(kc == KC - 1)), reads=[Wmk, mhT], writes=[pv])
            c.op("act", lambda e: e.activation(out=Vm[:, mc, half * 512:(half + 1) * 512], in_=pv[:, 0:512], func=AF.Identity), reads=[pv], writes=[Vm], merge_w=True)
    if cfg.debug:
        dbk = c.sb("dbk", [128, 2048], F32)
        c.op("dve", lambda e: e.tensor_copy(out=dbk[:, :], in_=KmT[:, :, :].rearrange("p a b -> p (a b)")), reads=[KmT], writes=[dbk])
        c.dma("sp", lambda e: e.dma_start(out=DBG_d[:, 1024:3072], in_=dbk[:, :]), owner=dbk, reads=[dbk], writes=[DBG_d], merge_w=True)
    c.pop()
    mixb = c.sb("mixb", [128, 8, 512], BF16)
    x1b = c.sb("x1b", [128, 4, D], F32)
    h2T = c.sb("h2T", [128, 8, 512], BF16)
    QmT = c.sb("QmT", [128, 8, 512], BF16)
    OmT = c.sb("OmT", [128, 8, 512], BF16)
    et_ring = c.ring("et", 2, [128, 2, 512], BF16)
    rd_ring = c.ring("rd", 2, [128, 512], F32)
    x2_ring = c.ring("x2t", 2, [128, D], F32)
    h3_ring = c.ring("h3", 2, [128, D], F32)
    h3b_ring = c.ring("h3b", 2, [128, D], BF16)
    h3T_ring = c.ring("h3T", 2, [128, 8, 128], F32)
    sm_ring = c.ring("sm", 32, [128, 36], F32)
    for b in range(NBO):
        c.dma("sp", lambda e: e.dma_start(out=mixb[:, :, :], in_=MIX_d.t[:, :, b * 512:(b + 1) * 512].rearrange("c p t -> p c t")), owner=mixb, reads=[MIX_d], writes=[mixb])
        for ti in range(4):
            tsl = slice(ti * 128, (ti + 1) * 128)
            xs = load_x(xa, (8 + 4 * b + ti) * 128)
            for half in range(2):
                px = P[5 + half]
                hs = slice(half * 512, (half + 1) * 512)
                for kc in range(KC):
                    c.op("pe", lambda e: e.matmul(px[:, 0:512], lhsT=mixb[:, kc, tsl], rhs=Wout[:, kc, hs], start=(kc == 0), stop=(kc == KC - 1)), reads=[mixb, Wout], writes=[px])
                c.op("dve", lambda e: e.tensor_tensor(out=x1b[:, ti, hs], in0=xs[:, hs], in1=px[:, 0:512], op=ALU.add), reads=[xs, px], writes=[x1b], merge_w=True)
            norm_T(x1b, x1b[:, ti, :], PT[ti % 2], h2T, h2T[:, :, tsl])
        for h in range(4):
            for dc in range(2):
                cidx = 2 * h + dc
                pq = P[2 + dc]
                for kc in range(KC):
                    c.op("pe", lambda e: e.matmul(pq[:, 0:512], lhsT=Wmq[:, kc, cidx * 128:(cidx + 1) * 128], rhs=h2T[:, kc, :], start=(kc == 0), stop=(kc == KC - 1)), reads=[Wmq, h2T], writes=[pq])
                sqm = sqm_ring.next()
                c.op("act", lambda e: e.activation(out=sqm[:, :], in_=pq[:, 0:512], func=AF.Square), reads=[pq], writes=[sqm])
                c.op("pe", lambda e: e.matmul(P[4][:, 0:512], lhsT=ones_bf[:, :], rhs=sqm[:, :], start=(dc == 0), stop=(dc == 1)), reads=[ones_bf, sqm], writes=[P[4]])
            rsm = rsm_ring.next()
            c.op("act", lambda e: e.activation(out=rsm[:, :], in_=P[4][:, 0:512], func=AF.Ln, scale=1.0 / 256.0, bias=EPS), reads=[P[4]], writes=[rsm])
            c.op("act", lambda e: e.activation(out=rsm[:, :], in_=rsm[:, :], func=AF.Exp, scale=-0.5), reads=[rsm], writes=[rsm])
            for dc in range(2):
                c.op("dve", lambda e: e.scalar_tensor_tensor(out=QmT[:, 2 * h + dc, :], in0=P[2 + dc][:, 0:512], scalar=mqw[:, dc:dc + 1], in1=rsm[:, :], op0=ALU.mult, op1=ALU.mult), reads=[P[2 + dc], mqw, rsm], writes=[QmT], merge_w=True)
        for h in range(4):
            et = et_ring.next()
            for mc in range(2):
                ps = P[2 + mc]
                for dc in range(2):
                    c.op("pe", lambda e: e.matmul(ps[:, 0:512], lhsT=KmT[:, 2 * h + dc, mc * 128:(mc + 1) * 128], rhs=QmT[:, 2 * h + dc, :], start=(dc == 0), stop=(dc == 1)), reads=[KmT, QmT], writes=[ps])
                c.op("act", lambda e: e.activation(out=et[:, mc, :], in_=ps[:, 0:512], func=AF.Exp), reads=[ps], writes=[et], merge_w=True)
            for mc in range(2):
                c.op("pe", lambda e: e.matmul(P[4][:, 0:512], lhsT=ones_bf[:, :], rhs=et[:, mc, :], start=(mc == 0), stop=(mc == 1)), reads=[ones_bf, et], writes=[P[4]])
            rd = rd_ring.next()
            c.op("act", lambda e: e.activation(out=rd[:, :], in_=P[4][:, 0:512], func=AF.Ln), reads=[P[4]], writes=[rd])
            c.op("act", lambda e: e.activation(out=rd[:, :], in_=rd[:, :], func=AF.Exp, scale=-1.0), reads=[rd], writes=[rd])
            for dvc in range(2):
                po = P[5 + dvc]
                for mc in range(2):
                    c.op("pe", lambda e: e.matmul(po[:, 0:512], lhsT=Vm[:, mc, h * 256 + dvc * 128:h * 256 + (dvc + 1) * 128], rhs=et[:, mc, :], start=(mc == 0), stop=(mc == 1)), reads=[Vm, et], writes=[po])
                c.op("dve", lambda e: e.tensor_tensor(out=OmT[:, 2 * h + dvc, :], in0=po[:, 0:512], in1=rd[:, :], op=ALU.mult), reads=[po, rd], writes=[OmT], merge_w=True)
        for ti in range(4):
            n = 4 * b + ti
            tsl = slice(ti * 128, (ti + 1) * 128)
            x2 = x2_ring.next()
            for half in range(2):
                px = P[5 + half]
                hs = slice(half * 512, (half + 1) * 512)
                for cc in range(8):
                    c.op("pe", lambda e: e.matmul(px[:, 0:512], lhsT=OmT[:, cc, tsl], rhs=Wmo[:, cc, hs], start=(cc == 0), stop=(cc == 7)), reads=[OmT, Wmo], writes=[px])
                c.op("dve", lambda e: e.tensor_tensor(out=x2[:, hs], in0=x1b[:, ti, hs], in1=px[:, 0:512], op=ALU.add), reads=[x1b, px], writes=[x2], merge_w=True)
            c.dma("pool", lambda e: e.dma_start(out=X2_d[n * 128:(n + 1) * 128, :], in_=x2[:, :]), owner=x2, reads=[x2], writes=[X2_d], merge_w=True)
            rs3 = norm_rstd(x2, x2[:, :])
            h3 = h3_ring.next()
            c.op("dve", lambda e: e.scalar_tensor_tensor(out=h3[:, :], in0=x2[:, :], scalar=rs3[:, 0:1], in1=WMOE[:, :], op0=ALU.mult, op1=ALU.mult), reads=[x2, rs3, WMOE], writes=[h3])
            h3b = h3b_ring.next()
            c.op("pool", lambda e: e.tensor_copy(out=h3b[:, :], in_=h3[:, :]), reads=[h3], writes=[h3b])
            c.dma("pool", lambda e: e.dma_start(out=H3_d[n * 128:(n + 1) * 128, :], in_=h3b[:, :]), owner=h3b, reads=[h3b], writes=[H3_d], merge_w=True)
            for kc in range(KC):
                pt = P[2 + kc // 4]
                c.op("pe", lambda e: e.transpose(out=pt[:, (kc % 4) * 128:(kc % 4 + 1) * 128], in_=h3[:, kc * 128:(kc + 1) * 128], identity=identf[:, :]), reads=[h3, identf], writes=[pt])
            h3T = h3T_ring.next()
            c.op("act", lambda e: e.activation(out=h3T[:, 0:4, :], in_=P[2][:, 0:512].rearrange("p (k t) -> p k t", k=4), func=AF.Identity), reads=[P[2]], writes=[h3T], merge_w=True)
            c.op("dve", lambda e: e.tensor_copy(out=h3T[:, 4:8, :], in_=P[3][:, 0:512].rearrange("p (k t) -> p k t", k=4)), reads=[P[3]], writes=[h3T], merge_w=True)
            for kc in range(KC):
                c.op("pe", lambda e: e.matmul(P[4][:, 0:36], lhsT=h3T[:, kc, :], rhs=Wrt[:, kc, :], start=(kc == 0), stop=(kc == KC - 1)), reads=[h3T, Wrt], writes=[P[4]])
            Ls, gmax, gm, ngm, ge, gsum, pen, ML, t8, nv1, ex, gtu, s12, dn = [sm_ring.next() for _ in range(14)]
            c.op("dve", lambda e: e.tensor_copy(out=Ls[:, 0:36], in_=P[4][:, 0:36]), reads=[P[4]], writes=[Ls])
            c.op("dve", lambda e: e.tensor_reduce(out=gmax[:, 0:1], in_=Ls[:, 0:4], axis=AX.X, op=ALU.max), reads=[Ls], writes=[gmax])
            c.op("dve", lambda e: e.tensor_scalar(out=gm[:, 0:4], in0=Ls[:, 0:4], scalar1=gmax[:, 0:1], scalar2=None, op0=ALU.is_equal), reads=[Ls, gmax], writes=[gm])
            c.op("dve", lambda e: e.tensor_scalar(out=ngm[:, 0:1], in0=gmax[:, 0:1], scalar1=-1.0, scalar2=None, op0=ALU.mult), reads=[gmax], writes=[ngm])
            c.op("act", lambda e: e.activation(out=ge[:, 0:4], in_=Ls[:, 0:4], func=AF.Exp, bias=ngm[:, 0:1], accum_out=gsum[:, 0:1]), reads=[Ls, ngm], writes=[ge, gsum])
            c.op("dve", lambda e: e.tensor_scalar(out=pen[:, 0:4], in0=gm[:, 0:4], scalar1=-1.0, scalar2=1e30, op0=ALU.add, op1=ALU.mult), reads=[gm], writes=[pen])
            c.op("dve", lambda e: e.tensor_tensor(out=ML[:, 0:32].rearrange("p (g x) -> p g x", g=4), in0=Ls[:, 4:36].rearrange("p (g x) -> p g x", g=4), in1=pen[:, 0:4].unsqueeze(2).to_broadcast([128, 4, 8]), op=ALU.add), reads=[Ls, pen], writes=[ML])
            c.op("dve", lambda e: e.max(out=t8[:, 0:8], in_=ML[:, 0:32]), reads=[ML], writes=[t8])
            c.op("dve", lambda e: e.tensor_scalar(out=OHs[:, n, :], in0=ML[:, 0:32], scalar1=t8[:, 1:2], scalar2=None, op0=ALU.is_ge), reads=[ML, t8], writes=[OHs], merge_w=True)
            c.op("dve", lambda e: e.tensor_scalar(out=nv1[:, 0:1], in0=t8[:, 0:1], scalar1=-1.0, scalar2=None, op0=ALU.mult), reads=[t8], writes=[nv1])
            c.op("act", lambda e: e.activation(out=ex[:, 0:32], in_=ML[:, 0:32], func=AF.Exp, bias=nv1[:, 0:1]), reads=[ML, nv1], writes=[ex])
            c.op("dve", lambda e: e.scalar_tensor_tensor(out=gtu[:, 0:32], in0=OHs[:, n, :], scalar=1.0, in1=ex[:, 0:32], op0=ALU.mult, op1=ALU.mult, accum_out=s12[:, 0:1]), reads=[OHs, ex], writes=[gtu, s12])
            c.op("dve", lambda e: e.tensor_tensor(out=dn[:, 0:1], in0=gsum[:, 0:1], in1=s12[:, 0:1], op=ALU.mult), reads=[gsum, s12], writes=[dn])
            c.op("dve", lambda e: e.reciprocal(out=dn[:, 0:1], in_=dn[:, 0:1]), reads=[dn], writes=[dn])
            c.op("dve", lambda e: e.tensor_scalar(out=GTs[:, n, :], in0=gtu[:, 0:32], scalar1=dn[:, 0:1], scalar2=None, op0=ALU.mult), reads=[gtu, dn], writes=[GTs], merge_w=True)
    phases["T"] = (c.ninst, c.nwait)
    if cfg.debug:
        c.dma("sp", lambda e: e.dma_start(out=DBG_d[0:128, 3072:3072 + 32], in_=GTs[:, 0, :]), owner=GTs, reads=[GTs], writes=[DBG_d], merge_w=True)
    if cfg.stop == "T":
        c.finish([X2_d, H3_d])
        c.pop_all()
        return nc, c, phases
    c.pop()

    c.push()
    TRI = c.sb("TRI", [128, 128], BF16)
    c.op("dve", lambda e: e.tensor_scalar(out=TRI[:, :], in0=kq[:, :], scalar1=0.0, scalar2=None, op0=ALU.is_lt), reads=[kq], writes=[TRI])
    RK = c.sb("RK", [128, NT, 32], F32)
    acc32 = c.sb("acc32", [128, 32], F32)
    accb_ring = c.ring("accb", 2, [128, 32], BF16)
    ohb_ring = c.ring("ohb", 2, [128, 32], BF16)
    accb = None
    for n in range(NT):
        ohb = ohb_ring.next()
        c.op("pool", lambda e: e.tensor_copy(out=ohb[:, :], in_=OHs[:, n, :]), reads=[OHs], writes=[ohb])
        pr = P[n % 2]
        c.op("pe", lambda e: e.matmul(pr[:, 0:32], lhsT=TRI[:, :], rhs=ohb[:, :], start=True, stop=(n == 0)), reads=[TRI, ohb], writes=[pr])
        if n > 0:
            c.op("pe", lambda e: e.matmul(pr[:, 0:32], lhsT=ones_bf[:, :], rhs=accb[:, :], start=False, stop=True), reads=[ones_bf, accb], writes=[pr])
        c.op("act", lambda e: e.activation(out=RK[:, n, :], in_=pr[:, 0:32], func=AF.Identity), reads=[pr], writes=[RK], merge_w=True)
        if n == 0:
            c.op("dve", lambda e: e.tensor_copy(out=acc32[:, :], in_=OHs[:, 0, :]), reads=[OHs], writes=[acc32])
        else:
            c.op("dve", lambda e: e.tensor_tensor(out=acc32[:, :], in0=acc32[:, :], in1=OHs[:, n, :], op=ALU.add), reads=[acc32, OHs], writes=[acc32])
        accb = accb_ring.next()
        c.op("dve", lambda e: e.tensor_copy(out=accb[:, :], in_=acc32[:, :]), reads=[acc32], writes=[accb])
    c.op("pe", lambda e: e.matmul(P[2][:, 0:32], lhsT=ones_bf[:, :], rhs=accb[:, :], start=True, stop=True), reads=[ones_bf, accb], writes=[P[2]])
    cnt_i = c.sb("cnt_i", [128, 32], I32)
    pcf = c.sb("pcf", [128, 32], F32)
    pend = c.sb("pend", [128, 32], F32)
    pst1 = c.sb("pst1", [128, 32], F32)
    ones32 = c.sb("ones32", [128, 32], F32)
    c.op("pool", lambda e: e.memset(ones32[:, :], 1.0), writes=[ones32])
    c.op("dve", lambda e: e.tensor_scalar(out=cnt_i[:, :], in0=P[2][:, 0:32], scalar1=127.0, scalar2=None, op0=ALU.add), reads=[P[2]], writes=[cnt_i])
    c.op("dve", lambda e: e.tensor_scalar(out=cnt_i[:, :], in0=cnt_i[:, :], scalar1=7, scalar2=7, op0=ALU.logical_shift_right, op1=ALU.logical_shift_left), reads=[cnt_i], writes=[cnt_i])
    c.op("dve", lambda e: e.tensor_copy(out=pcf[:, :], in_=cnt_i[:, :]), reads=[cnt_i], writes=[pcf])
    c.op("dve", lambda e: e.tensor_tensor_scan(out=pend[:, :], data0=ones32[:, :], data1=pcf[:, :], initial=0.0, op0=ALU.mult, op1=ALU.add), reads=[ones32, pcf], writes=[pend])
    c.op("dve", lambda e: e.tensor_tensor(out=pst1[:, :], in0=pend[:, :], in1=pcf[:, :], op=ALU.subtract), reads=[pend, pcf], writes=[pst1])
    c.op("dve", lambda e: e.tensor_scalar(out=pst1[:, :], in0=pst1[:, :], scalar1=1.0, scalar2=None, op0=ALU.add), reads=[pst1], writes=[pst1])
    thr_i = c.sb("thr_i", [128, NBLK], I32)
    thr = c.sb("thr", [128, NBLK], F32)
    cmpb = c.sb("cmpb", [128, NBLK, 32], F32)
    bef = c.sb("bef", [128, NBLK], F32)
    pidx_i = c.sb("pidx_i", [128, 1], I32)
    pidx = c.sb("pidx", [128, 1], F32)
    widx_f = c.sb("widx_f", [128, NBLK, 2], F32)
    WIDX = c.sb("WIDX", [128, NBLK, 2], I32)
    c.op("pool", lambda e: e.iota(thr_i[:, :], pattern=[[128, NBLK]], base=0, channel_multiplier=0), writes=[thr_i])
    c.op("dve", lambda e: e.tensor_copy(out=thr[:, :], in_=thr_i[:, :]), reads=[thr_i], writes=[thr])
    c.op("dve", lambda e: e.tensor_tensor(out=cmpb[:, :, :], in0=pend[:, :].unsqueeze(1).to_broadcast([128, NBLK, 32]), in1=thr[:, :].unsqueeze(2).to_broadcast([128, NBLK, 32]), op=ALU.is_le), reads=[pend, thr], writes=[cmpb])
    c.op("dve", lambda e: e.tensor_reduce(out=bef[:, :], in_=cmpb[:, :, :], axis=AX.X, op=ALU.add), reads=[cmpb], writes=[bef])
    c.op("dve", lambda e: e.tensor_scalar(out=bef[:, :], in0=bef[:, :], scalar1=31.0, scalar2=None, op0=ALU.min), reads=[bef], writes=[bef])
    c.op("pool", lambda e: e.iota(pidx_i[:, :], pattern=[[0, 1]], base=0, channel_multiplier=1), writes=[pidx_i])
    c.op("dve", lambda e: e.tensor_copy(out=pidx[:, :], in_=pidx_i[:, :]), reads=[pidx_i], writes=[pidx])
    c.op("dve", lambda e: e.tensor_scalar(out=widx_f[:, :, 0], in0=bef[:, :], scalar1=128.0, scalar2=pidx[:, 0:1], op0=ALU.mult, op1=ALU.add), reads=[bef, pidx], writes=[widx_f], merge_w=True)
    c.op("dve", lambda e: e.tensor_scalar(out=widx_f[:, :, 0], in0=widx_f[:, :, 0], scalar1=2.0, scalar2=None, op0=ALU.mult), reads=[widx_f], writes=[widx_f], merge_w=True)
    c.op("dve", lambda e: e.tensor_scalar(out=widx_f[:, :, 1], in0=widx_f[:, :, 0], scalar1=1.0, scalar2=None, op0=ALU.add), reads=[widx_f], writes=[widx_f], merge_w=True)
    skp = c.sb("skp", [128, NBLK], F32)
    c.op("dve", lambda e: e.tensor_tensor(out=skp[:, 2:NBLK], in0=bef[:, 2:NBLK], in1=bef[:, 0:NBLK - 2], op=ALU.is_equal), reads=[bef], writes=[skp])
    c.op("dve", lambda e: e.tensor_scalar(out=skp[:, 2:NBLK], in0=skp[:, 2:NBLK], scalar1=(1.0e6 if cfg.skip else 0.0), scalar2=None, op0=ALU.mult), reads=[skp], writes=[skp])
    c.op("dve", lambda e: e.tensor_tensor(out=widx_f[:, 2:NBLK, :], in0=widx_f[:, 2:NBLK, :], in1=skp[:, 2:NBLK].unsqueeze(2).to_broadcast([128, NBLK - 2, 2]), op=ALU.add), reads=[widx_f, skp], writes=[widx_f])
    c.op("dve", lambda e: e.tensor_copy(out=WIDX[:, :, :], in_=widx_f[:, :, :]), reads=[widx_f], writes=[WIDX])
    DSTf = c.sb("DSTf", [128, NT, 2], F32)
    DSTi = c.sb("DSTi", [128, NT, 2], I32)
    GAB = c.sb("GAB", [128, NT, 2], F32)
    m1_ring = c.ring("m1r", 4, [128, 32], F32)
    sc_ring = c.ring("scr", 8, [128, 2], F32)
    for n in range(NT):
        d1, m1, mb, jk2 = [m1_ring.next() for _ in range(4)]
        db1, sm, gs = [sc_ring.next() for _ in range(3)]
        c.op("dve", lambda e: e.tensor_tensor(out=d1[:, :], in0=RK[:, n, :], in1=pst1[:, :], op=ALU.add), reads=[RK, pst1], writes=[d1])
        c.op("dve", lambda e: e.tensor_tensor(out=m1[:, :], in0=d1[:, :], in1=OHs[:, n, :], op=ALU.mult), reads=[d1, OHs], writes=[m1])
        c.op("dve", lambda e: e.tensor_reduce(out=db1[:, 0:1], in_=m1[:, :], axis=AX.X, op=ALU.max), reads=[m1], writes=[db1])
        c.op("dve", lambda e: e.tensor_reduce(out=sm[:, 0:1], in_=m1[:, :], axis=AX.X, op=ALU.add), reads=[m1], writes=[sm])
        c.op("dve", lambda e: e.scalar_tensor_tensor(out=DSTf[:, n, 0:1], in0=sm[:, 0:1], scalar=-1.0, in1=db1[:, 0:1], op0=ALU.add, op1=ALU.subtract), reads=[sm, db1], writes=[DSTf], merge_w=True)
        c.op("dve", lambda e: e.tensor_scalar(out=DSTf[:, n, 1:2], in0=db1[:, 0:1], scalar1=-1.0, scalar2=None, op0=ALU.add), reads=[db1], writes=[DSTf], merge_w=True)
        c.op("dve", lambda e: e.tensor_scalar(out=mb[:, :], in0=m1[:, :], scalar1=db1[:, 0:1], scalar2=None, op0=ALU.is_ge), reads=[m1, db1], writes=[mb])
        c.op("dve", lambda e: e.scalar_tensor_tensor(out=jk2[:, :], in0=GTs[:, n, :], scalar=1.0, in1=mb[:, :], op0=ALU.mult, op1=ALU.mult, accum_out=GAB[:, n, 1:2]), reads=[GTs, mb], writes=[jk2, GAB], merge_w=True)
        c.op("dve", lambda e: e.tensor_reduce(out=gs[:, 0:1], in_=GTs[:, n, :], axis=AX.X, op=ALU.add), reads=[GTs], writes=[gs])
        c.op("dve", lambda e: e.tensor_tensor(out=GAB[:, n, 0:1], in0=gs[:, 0:1], in1=GAB[:, n, 1:2], op=ALU.subtract), reads=[gs, GAB], writes=[GAB], merge_w=True)
    c.op("dve", lambda e: e.tensor_copy(out=DSTi[:, :, :], in_=DSTf[:, :, :]), reads=[DSTf], writes=[DSTi])
    fillb = c.sb("fillb", [128, NBLK, 16], I32)
    TOK16 = c.sb("TOK16", [128, NT, 16], I32)
    zpad = c.sb("zpad", [128, D], BF16)
    c.op("pool", lambda e: e.iota(fillb[:, :, :], pattern=[[0, NBLK], [0, 16]], base=NTOK, channel_multiplier=0), writes=[fillb])
    c.op("pool", lambda e: e.iota(TOK16[:, :, :], pattern=[[128, NT], [0, 16]], base=0, channel_multiplier=1), writes=[TOK16])
    c.op("pool", lambda e: e.memset(zpad[:, :], 0.0), writes=[zpad])
    c.dma("sp", lambda e: e.dma_start(out=SLOT_d.t.ap().rearrange("(b p) x -> p b x", p=128), in_=fillb[:, :, :]), owner=fillb, reads=[fillb], writes=[SLOT_d])
    c.dma("sp", lambda e: e.dma_start(out=H3_d[NTOK:NTOK + 128, :], in_=zpad[:, :]), owner=zpad, reads=[zpad], writes=[H3_d], merge_w=True)
    for n in range(NT):
        for k2 in range(2):
            c.dma("pool", lambda e: e.indirect_dma_start(out=SLOT_d.t.ap(), out_offset=bass.IndirectOffsetOnAxis(ap=DSTi[:, n, k2:k2 + 1], axis=0), in_=TOK16[:, n, :], in_offset=None), owner=TOK16, reads=[TOK16, DSTi], writes=[SLOT_d], merge_w=True)
    phases["M1"] = (c.ninst, c.nwait)
    if cfg.debug:
        c.dma("sp", lambda e: e.dma_start(out=DBG_d[:, 3200:3200 + 2 * NT], in_=DSTf[:, :, :].rearrange("p n k -> p (n k)")), owner=DSTf, reads=[DSTf], writes=[DBG_d], merge_w=True)
        c.dma("sp", lambda e: e.dma_start(out=DBG_d[:, 3400:3400 + 2 * NT], in_=GAB[:, :, :].rearrange("p n k -> p (n k)")), owner=GAB, reads=[GAB], writes=[DBG_d], merge_w=True)
        c.dma("sp", lambda e: e.dma_start(out=DBG_d[:, 3600:3600 + NBLK], in_=bef[:, :]), owner=bef, reads=[bef], writes=[DBG_d], merge_w=True)
    if cfg.stop == "M1":
        c.finish([SLOT_d])
        c.pop_all()
        return nc, c, phases

    w1v = w1.t.ap().rearrange("e (p k) f -> (e p) (k f)", p=128).rearrange("r (two x) -> (r two) x", two=2)
    w3v = w3.t.ap().rearrange("e (p k) f -> (e p) (k f)", p=128).rearrange("r (two x) -> (r two) x", two=2)
    w2v = w2.t.ap().rearrange("e (p k) f -> (e p) (k f)", p=128).rearrange("r (two x) -> (r two) x", two=2)
    bc_reg = nc.gpsimd.alloc_register("bc_reg")
    nc.gpsimd.reg_mov(bc_reg, 32 * 128 * 2 - 1)
    sidx_ring = c.ring("sidx", 3, [128, 16], I32)
    xg_ring = c.ring("xg", 2, [128, D], BF16)
    xT_ring = c.ring("xTm", 2, [128, 8, 128], BF16)
    w1b_ring = c.ring("w1b", 2, [128, 4096], BF16)
    w3b_ring = c.ring("w3b", 2, [128, 4096], BF16)
    w2b_ring = c.ring("w2b", 2, [128, 4096], BF16)
    g1_ring = c.ring("g1", 2, [128, 512], F32)
    ggb_ring = c.ring("ggb", 2, [128, 512], BF16)
    gT_ring = c.ring("gTm", 2, [128, 4, 128], BF16)
    ys_ring = c.ring("ys", 2, [128, D], F32)
    SLT = c.sb("SLT", [128, NBLK, 16], I32)
    c.dma("sp", lambda e: e.dma_start(out=SLT[:, :, :], in_=SLOT_d.t.ap().rearrange("(b p) x -> p b x", p=128)), owner=SLT, reads=[SLOT_d], writes=[SLT])
    for b in range(NBLK):
        xg = xg_ring.next()
        c.dma("pool", lambda e: e.indirect_dma_start(out=xg[:, :], out_offset=None, in_=H3_d.t.ap(), in_offset=bass.IndirectOffsetOnAxis(ap=SLT[:, b, 0:1], axis=0)), owner=xg, reads=[H3_d, SLT], writes=[xg])
        w1b, w3b, w2b = w1b_ring.next(), w3b_ring.next(), w2b_ring.next()
        for wv, wb in ((w1v, w1b), (w3v, w3b), (w2v, w2b)):
            for half in range(2):
                c.dma("pool", lambda e: e.indirect_dma_start(out=wb[:, half * 2048:(half + 1) * 2048], out_offset=None, in_=wv, in_offset=bass.IndirectOffsetOnAxis(ap=WIDX[:, b, half:half + 1], axis=0), bounds_check=bc_reg, oob_is_err=False), owner=wb, reads=[WIDX], writes=[wb], merge_w=(half == 1), indep=(half == 1))
        pxt = P[b % 2]
        pv = pbf(pxt)
        for kc in range(KC):
            c.op("pe", lambda e: e.transpose(out=pv[:, kc * 128:(kc + 1) * 128], in_=xg[:, kc:kc + 8 * 127 + 1:8], identity=ident[:, :]), reads=[xg, ident], writes=[pxt])
        xT = xT_ring.next()
        c.op("act", lambda e: e.activation(out=xT[:, :, :], in_=pv[:, 0:1024].rearrange("p (k t) -> p k t", k=8), func=AF.Identity), reads=[pxt], writes=[xT])
        ph1, ph3 = P[2 + 2 * (b % 2)], P[3 + 2 * (b % 2)]
        w1k = w1b[:, :].rearrange("p (k f) -> p k f", k=8)
        w3k = w3b[:, :].rearrange("p (k f) -> p k f", k=8)
        w2k = w2b[:, :].rearrange("p (k f) -> p k f", k=4)
        for kc in range(KC):
            c.op("pe", lambda e: e.matmul(ph1[:, 0:512], lhsT=xT[:, kc, :], rhs=w1k[:, kc, :], start=(kc == 0), stop=(kc == KC - 1)), reads=[xT, w1b], writes=[ph1])
        for kc in range(KC):
            c.op("pe", lambda e: e.matmul(ph3[:, 0:512], lhsT=xT[:, kc, :], rhs=w3k[:, kc, :], start=(kc == 0), stop=(kc == KC - 1)), reads=[xT, w3b], writes=[ph3])
        g1 = g1_ring.next()
        c.op("act", lambda e: e.activation(out=g1[:, :], in_=ph1[:, 0:512], func=AF.Exp, scale=-1.0), reads=[ph1], writes=[g1])
        c.op("act", lambda e: e.activation(out=g1[:, :], in_=g1[:, :], func=AF.Ln, bias=1.0), reads=[g1], writes=[g1])
        c.op("act", lambda e: e.activation(out=g1[:, :], in_=g1[:, :], func=AF.Exp, scale=-1.0), reads=[g1], writes=[g1])
        c.op("dve", lambda e: e.tensor_tensor(out=g1[:, :], in0=g1[:, :], in1=ph1[:, 0:512], op=ALU.mult), reads=[g1, ph1], writes=[g1])
        ggb = ggb_ring.next()
        c.op("dve", lambda e: e.tensor_tensor(out=ggb[:, :], in0=g1[:, :], in1=ph3[:, 0:512], op=ALU.mult), reads=[g1, ph3], writes=[ggb])
        for fc in range(4):
            c.op("pe", lambda e: e.transpose(out=pv[:, fc * 128:(fc + 1) * 128], in_=ggb[:, fc:fc + 4 * 127 + 1:4], identity=ident[:, :]), reads=[ggb, ident], writes=[pxt])
        gT = gT_ring.next()
        c.op("act", lambda e: e.activation(out=gT[:, :, :], in_=pv[:, 0:512].rearrange("p (k t) -> p k t", k=4), func=AF.Identity), reads=[pxt], writes=[gT])
        ys = ys_ring.next()
        for half in range(2):
            py = P[6 + half]
            for fc in range(4):
                c.op("pe", lambda e: e.matmul(py[:, 0:512], lhsT=gT[:, fc, :], rhs=w2k[:, fc, half * 512:(half + 1) * 512], start=(fc == 0), stop=(fc == 3)), reads=[gT, w2b], writes=[py])
            if half == 0:
                c.op("act", lambda e: e.activation(out=ys[:, 0:512], in_=py[:, 0:512], func=AF.Identity), reads=[py], writes=[ys], merge_w=True)
            else:
                c.op("dve", lambda e: e.tensor_copy(out=ys[:, 512:1024], in_=py[:, 0:512]), reads=[py], writes=[ys], merge_w=True)
        c.dma("sp", lambda e: e.dma_start(out=YS_d[b * 128:(b + 1) * 128, :], in_=ys[:, :]), owner=ys, reads=[ys], writes=[YS_d], merge_w=True)
    phases["M2"] = (c.ninst, c.nwait)

    ya_ring = c.ring("ya", 2, [128, D], F32)
    yb_ring = c.ring("yb", 2, [128, D], F32)
    x2c_ring = c.ring("x2c", 2, [128, D], F32)
    oo_ring = c.ring("oo", 2, [128, D], F32)
    for n in range(NT):
        ya, yb, x2c, oo = ya_ring.next(), yb_ring.next(), x2c_ring.next(), oo_ring.next()
        c.dma("pool", lambda e: e.indirect_dma_start(out=ya[:, :], out_offset=None, in_=YS_d.t.ap(), in_offset=bass.IndirectOffsetOnAxis(ap=DSTi[:, n, 0:1], axis=0)), owner=ya, reads=[YS_d, DSTi], writes=[ya])
        c.dma("pool", lambda e: e.indirect_dma_start(out=yb[:, :], out_offset=None, in_=YS_d.t.ap(), in_offset=bass.IndirectOffsetOnAxis(ap=DSTi[:, n, 1:2], axis=0)), owner=yb, reads=[YS_d, DSTi], writes=[yb])
        c.dma("sp", lambda e: e.dma_start(out=x2c[:, :], in_=X2_d[n * 128:(n + 1) * 128, :]), owner=x2c, reads=[X2_d], writes=[x2c])
        c.op("dve", lambda e: e.scalar_tensor_tensor(out=oo[:, :], in0=ya[:, :], scalar=GAB[:, n, 0:1], in1=x2c[:, :], op0=ALU.mult, op1=ALU.add), reads=[ya, GAB, x2c], writes=[oo])
        c.op("dve", lambda e: e.scalar_tensor_tensor(out=oo[:, :], in0=yb[:, :], scalar=GAB[:, n, 1:2], in1=oo[:, :], op0=ALU.mult, op1=ALU.add), reads=[yb, GAB, oo], writes=[oo])
        c.dma("act", lambda e: e.dma_start(out=y_d[n * 128:(n + 1) * 128, :], in_=oo[:, :]), owner=oo, reads=[oo], writes=[y_d], merge_w=True)
    phases["M3"] = (c.ninst, c.nwait)
    c.finish([y_d])
    c.pop_all()
    return nc, c, phases


def prep_core(inp, cfg, b, j, S):
    LS, NA, NF = cfg.LS, cfg.NA, cfg.NF
    t0 = j * LS
    x = inp["x"][b]
    xa = np.zeros((NA * 128, D), np.float32)
    lo, hi = t0 - 1024, t0 + LS + 1024
    slo, shi = max(lo, 0), min(hi, S)
    xa[slo - lo:shi - lo] = x[slo:shi]
    far_idx = np.concatenate([np.arange(0, t0), np.arange(t0 + LS, S)]).astype(np.int64)
    assert far_idx.size == NF * 128
    xf = np.ascontiguousarray(x[far_idx])
    posf = np.ascontiguousarray(far_idx.astype(np.float32).reshape(NF, 128).T)
    t01 = np.zeros((128, 2), np.float32)
    t01[:, 0] = t0
    t01[:, 1] = t0 + LS
    valt = np.zeros((128, cfg.NCHT), np.float32)
    kk = np.arange(128)
    for pi, d in enumerate(PATTERNS):
        for r in range(d):
            for ch in range(cfg.nch[pi]):
                T = d * (128 * ch - 64 + kk) + r + t0
                valt[:, cfg.chbase[pi] + r * cfg.nch[pi] + ch] = ((T >= 0) & (T < S)).astype(np.float32)

    def kc_layout(v):
        return np.ascontiguousarray(v.reshape(8, 128).T)

    def f32(a):
        return np.ascontiguousarray(a, dtype=np.float32)
    m = {
        "xa": xa, "xf": xf, "posf": posf, "t01": t01, "valt": valt,
        "memx": f32(inp["mem"][b]),
        "nw_mix": kc_layout(inp["norm_mix_w"][0]),
        "nw_mem": kc_layout(inp["norm_mem_w"][0]),
        "nw_memkv": kc_layout(inp["norm_memkv_w"][0]),
        "nw_moe": f32(inp["norm_moe_w"][0].reshape(1, D)),
        "aq_w": f32(np.tile(inp["attn_q_norm_w"][0], 2).reshape(128, 1)),
        "ak_w": f32(np.tile(inp["attn_k_norm_w"][0], 2).reshape(128, 1)),
        "mq_w": f32(inp["mem_q_norm_w"][0].reshape(2, 128).T),
        "mk_w": f32(inp["mem_k_norm_w"][0].reshape(2, 128).T),
        "dec": f32(np.concatenate([inp["ret_decay_f"][0], inp["ret_decay_b"][0]]).reshape(1, 8)),
        "gn_w": f32(inp["ret_gn_w"][0].reshape(1, 512)),
        "w_in": f32(inp["w_in"][0]), "w_out": f32(inp["w_out"][0]),
        "w_mq": f32(inp["w_mq"][0]), "w_mkv": f32(inp["w_mkv"][0]), "w_mo": f32(inp["w_mo"][0]),
        "w_rt": f32(np.concatenate([inp["w_group"][0], inp["w_router"][0].transpose(1, 0, 2).reshape(D, 32)], axis=1)),
        "w1": f32(inp["w_exp_gate"][0]), "w3": f32(inp["w_exp_up"][0]), "w2": f32(inp["w_exp_down"][0]),
    }
    return m


_CACHE = {}


def kernel(**inputs):
    inp = {k: np.asarray(v) for k, v in inputs.items()}
    B, S, _ = inp["x"].shape
    cfg = Cfg(NT=32, NF=96)
    assert S == 4 * cfg.LS and B == 2
    if "nc" not in _CACHE:
        _CACHE["nc"] = build(cfg)[0]
    nc = _CACHE["nc"]
    in_maps = [prep_core(inp, cfg, cidx // 4, cidx % 4, S) for cidx in range(8)]
    res = run_bass_kernel_spmd(nc, in_maps, core_ids=list(range(8)))
    out = np.empty((B, S, D), np.float32)
    for cidx in range(8):
        b, j = cidx // 4, cidx % 4
        out[b, j * cfg.LS:(j + 1) * cfg.LS] = np.asarray(res.results[cidx]["y"], dtype=np.float32)
    return out
```
